# Optimizing a Trainium2 kernel written in Bass

```python
import math
import jax, jax.numpy as jnp
from jax import lax
import numpy as np

D_MODEL = 1024
BATCH = 8
SEQ = 2048
DEPTH = 1

D_MIX = D_MODEL
ATT_HEADS = 4
ATT_QK_DIM = 64
ATT_V_DIM = 2 * ATT_QK_DIM
ATT_QK_WIDTH = ATT_HEADS * 2 * ATT_QK_DIM
ATT_WIDTH = ATT_HEADS * ATT_V_DIM
N_BUCKETS = 32
MAX_DISTANCE = 128
Q_BLOCK = 128
SSD_WIDTH = D_MIX - ATT_WIDTH
SSD_HEADDIM = 64
SSD_HEADS = SSD_WIDTH // SSD_HEADDIM
SSD_GROUPS = 2
SSD_STATE = 64
SSD_CONV = 5
SSD_CHUNK = 128
SSD_CONV_CH = SSD_WIDTH + 2 * SSD_GROUPS * SSD_STATE
IN_SPLITS = (ATT_QK_WIDTH, ATT_QK_WIDTH, ATT_WIDTH, SSD_WIDTH, SSD_CONV_CH, SSD_HEADS, SSD_HEADS)
IN_COLS = 2 * ATT_QK_WIDTH + ATT_WIDTH + SSD_WIDTH + SSD_CONV_CH + 2 * SSD_HEADS
N_EXPERTS = 16
CAPACITY_FACTOR = 2
EXPERT_FF = 2816
EPS = 1e-6

kernel_name = 'hybrid_diffattn_ssd_ec_moe_block'


def lambda_init(layer):
    return 0.8 - 0.6 * math.exp(-0.3 * layer)


def rmsnorm(x, g):
    xf = x.astype(jnp.float32)
    xf = xf * lax.rsqrt(jnp.mean(xf * xf, axis=-1, keepdims=True) + EPS)
    return (xf * g.astype(jnp.float32)).astype(x.dtype)


def modulate(h, shift, scale):
    return h * (1.0 + scale[:, None, :]) + shift[:, None, :]


def t5_bucket(rel):
    nb = N_BUCKETS // 2
    ret = jnp.where(rel > 0, nb, 0)
    n = jnp.abs(rel)
    max_exact = nb // 2
    nf = jnp.maximum(n, 1).astype(jnp.float32)
    large = max_exact + (jnp.log(nf / max_exact) / math.log(MAX_DISTANCE / max_exact)
                         * (nb - max_exact)).astype(jnp.int32)
    large = jnp.minimum(large, nb - 1)
    return ret + jnp.where(n < max_exact, n, large)


def diff_attention(q, k, v, lam, rel_table):
    b, s = q.shape[:2]
    nblk = s // Q_BLOCK
    scale = ATT_QK_DIM ** -0.5
    qb = q.reshape(b, nblk, Q_BLOCK, ATT_HEADS, 2, ATT_QK_DIM).transpose(1, 0, 2, 3, 4, 5)
    k_pos = jnp.arange(s)

    def block(args):
        i, qi = args
        q_pos = i * Q_BLOCK + jnp.arange(Q_BLOCK)
        bucket = t5_bucket(k_pos[None, :] - q_pos[:, None])
        bias = rel_table[bucket].transpose(2, 0, 1).astype(jnp.float32)
        logits = jnp.einsum('bqhmd,bkhmd->bhmqk', qi, k).astype(jnp.float32) * scale
        probs = jax.nn.softmax(logits + bias[None, :, None], axis=-1)
        w = probs[:, :, 0] - lam * probs[:, :, 1]
        return jnp.einsum('bhqk,bkhd->bqhd', w.astype(v.dtype), v)

    out = lax.map(block, (jnp.arange(nblk), qb))
    return out.transpose(1, 0, 2, 3, 4).reshape(b, s, ATT_HEADS, ATT_V_DIM)


def segsum(x):
    t = x.shape[-1]
    cs = jnp.cumsum(x, axis=-1)
    seg = cs[..., :, None] - cs[..., None, :]
    mask = jnp.tril(jnp.ones((t, t), dtype=bool))
    return jnp.where(mask, seg, -jnp.inf)


def ssd_chunked(xs, dt, A, Bm, Cm):
    b, s = xs.shape[:2]
    nc = s // SSD_CHUNK
    r = SSD_HEADS // SSD_GROUPS
    X = (xs * dt[..., None]).reshape(b, nc, SSD_CHUNK, SSD_GROUPS, r, SSD_HEADDIM)
    dA = (dt * A).reshape(b, nc, SSD_CHUNK, SSD_GROUPS, r).transpose(0, 3, 4, 1, 2)
    Bc = Bm.reshape(b, nc, SSD_CHUNK, SSD_GROUPS, SSD_STATE)
    Cc = Cm.reshape(b, nc, SSD_CHUNK, SSD_GROUPS, SSD_STATE)
    A_cs = jnp.cumsum(dA, axis=-1)
    L = jnp.exp(segsum(dA))
    y_diag = jnp.einsum('bclgn,bcsgn,bgrcls,bcsgrp->bclgrp', Cc, Bc, L, X)
    decay_states = jnp.exp(A_cs[..., -1:] - A_cs)
    states = jnp.einsum('bclgn,bgrcl,bclgrp->bcgrpn', Bc, decay_states, X)
    states = jnp.concatenate([jnp.zeros_like(states[:, :1]), states], axis=1)
    decay_chunk = jnp.exp(segsum(jnp.pad(A_cs[..., -1], ((0, 0), (0, 0), (0, 0), (1, 0)))))
    new_states = jnp.einsum('bgrzc,bcgrpn->bzgrpn', decay_chunk, states)[:, :-1]
    y_off = jnp.einsum('bclgn,bcgrpn,bgrcl->bclgrp', Cc, new_states, jnp.exp(A_cs))
    return (y_diag + y_off).reshape(b, s, SSD_HEADS, SSD_HEADDIM)


def ssd_mixer(z, xbc, dt_f_raw, dt_b_raw, conv_w, conv_b, dt_bias_f, dt_bias_b,
              A_log_f, A_log_b, D_skip, norm_g):
    b, s = z.shape[:2]
    f32 = jnp.float32
    xbc = lax.conv_general_dilated(xbc, conv_w, window_strides=(1,),
                                   padding=((SSD_CONV // 2, SSD_CONV // 2),),
                                   dimension_numbers=('NWC', 'WIO', 'NWC'),
                                   feature_group_count=SSD_CONV_CH) + conv_b
    xbc = jax.nn.silu(xbc.astype(f32))
    xs, Bm, Cm = jnp.split(xbc, [SSD_WIDTH, SSD_WIDTH + SSD_GROUPS * SSD_STATE], axis=-1)
    xs = xs.reshape(b, s, SSD_HEADS, SSD_HEADDIM)
    Bm = Bm.reshape(b, s, SSD_GROUPS, SSD_STATE)
    Cm = Cm.reshape(b, s, SSD_GROUPS, SSD_STATE)
    dt_f = jax.nn.softplus(dt_f_raw.astype(f32) + dt_bias_f.astype(f32))
    dt_b = jax.nn.softplus(dt_b_raw.astype(f32) + dt_bias_b.astype(f32))
    A_f = -jnp.exp(A_log_f.astype(f32))
    A_b = -jnp.exp(A_log_b.astype(f32))
    rev = lambda t: jnp.flip(t, axis=1)
    y_f = ssd_chunked(xs, dt_f, A_f, Bm, Cm)
    y_b = rev(ssd_chunked(rev(xs), rev(dt_b), A_b, rev(Bm), rev(Cm)))
    y = y_f + y_b + D_skip.astype(f32)[:, None] * xs
    y = y.reshape(b, s, SSD_WIDTH) * jax.nn.silu(z.astype(f32))
    yg = y.reshape(b, s, SSD_GROUPS, SSD_WIDTH // SSD_GROUPS)
    yg = yg * lax.rsqrt(jnp.mean(yg * yg, axis=-1, keepdims=True) + EPS)
    return (yg.reshape(b, s, SSD_WIDTH) * norm_g.astype(f32)).astype(z.dtype)


def expert_choice_ffn(h, router_w, w_gate, w_up, w_down):
    b, s, d = h.shape
    cap = CAPACITY_FACTOR * s // N_EXPERTS
    aff = jax.nn.softmax(jnp.einsum('bsd,de->bse', h, router_w).astype(jnp.float32), axis=-1)
    g, idx = lax.top_k(aff.transpose(0, 2, 1), cap)
    bidx = jnp.arange(b)[:, None, None]
    xg = h[bidx, idx]
    hid = jax.nn.silu(jnp.einsum('becd,edf->becf', xg, w_gate)) * jnp.einsum('becd,edf->becf', xg, w_up)
    y = jnp.einsum('becf,efd->becd', hid, w_down) * g[..., None].astype(h.dtype)
    return jnp.zeros_like(h).at[bidx, idx].add(y)


def setup_inputs(seed: int = 0) -> dict:
    key = jax.random.key(seed)
    ks = jax.random.split(key, 32)
    f32 = jnp.float32

    def nrm(k, shape, s):
        return jax.random.normal(k, shape, f32) * s

    def gain(k, shape):
        return 1.0 + 0.02 * jax.random.normal(k, shape, f32)

    def dt_bias(k):
        dt = jnp.exp(jax.random.uniform(k, (DEPTH, SSD_HEADS), f32, math.log(1e-3), math.log(1e-1)))
        return dt + jnp.log(-jnp.expm1(-dt))

    return {
        'x': nrm(ks[0], (BATCH, SEQ, D_MODEL), 1.0),
        'c': nrm(ks[1], (BATCH, D_MODEL), 1.0),
        'ada_w': nrm(ks[2], (DEPTH, D_MODEL, 6 * D_MODEL), D_MODEL ** -0.5),
        'ada_b': nrm(ks[3], (DEPTH, 6 * D_MODEL), 0.02),
        'norm_mix_g': gain(ks[4], (DEPTH, D_MODEL)),
        'norm_ffn_g': gain(ks[5], (DEPTH, D_MODEL)),
        'norm_final_g': gain(ks[6], (D_MODEL,)),
        'w_in': nrm(ks[7], (DEPTH, D_MODEL, IN_COLS), D_MODEL ** -0.5),
        'lambda_q1': nrm(ks[8], (DEPTH, ATT_QK_DIM), 0.1),
        'lambda_k1': nrm(ks[9], (DEPTH, ATT_QK_DIM), 0.1),
        'lambda_q2': nrm(ks[10], (DEPTH, ATT_QK_DIM), 0.1),
        'lambda_k2': nrm(ks[11], (DEPTH, ATT_QK_DIM), 0.1),
        'attn_subln_g': gain(ks[12], (DEPTH, ATT_V_DIM)),
        'rel_bias_table': nrm(ks[13], (N_BUCKETS, ATT_HEADS), 0.5),
        'conv_w': nrm(ks[14], (DEPTH, SSD_CONV, 1, SSD_CONV_CH), SSD_CONV ** -0.5),
        'conv_b': nrm(ks[15], (DEPTH, SSD_CONV_CH), 0.02),
        'dt_bias_f': dt_bias(ks[16]),
        'dt_bias_b': dt_bias(ks[17]),
        'A_log_f': jnp.log(jax.random.uniform(ks[18], (DEPTH, SSD_HEADS), f32, 1.0, 16.0)),
        'A_log_b': jnp.log(jax.random.uniform(ks[19], (DEPTH, SSD_HEADS), f32, 1.0, 16.0)),
        'D_skip': gain(ks[20], (DEPTH, SSD_HEADS)),
        'ssm_norm_g': gain(ks[21], (DEPTH, SSD_WIDTH)),
        'w_out': nrm(ks[22], (DEPTH, D_MIX, D_MODEL), D_MIX ** -0.5),
        'router_w': nrm(ks[23], (DEPTH, D_MODEL, N_EXPERTS), D_MODEL ** -0.5),
        'w_gate': nrm(ks[24], (DEPTH, N_EXPERTS, D_MODEL, EXPERT_FF), D_MODEL ** -0.5),
        'w_up': nrm(ks[25], (DEPTH, N_EXPERTS, D_MODEL, EXPERT_FF), D_MODEL ** -0.5),
        'w_down': nrm(ks[26], (DEPTH, N_EXPERTS, EXPERT_FF, D_MODEL), EXPERT_FF ** -0.5),
    }


def reference(x, c, ada_w, ada_b, norm_mix_g, norm_ffn_g, norm_final_g, w_in,
              lambda_q1, lambda_k1, lambda_q2, lambda_k2, attn_subln_g, rel_bias_table,
              conv_w, conv_b, dt_bias_f, dt_bias_b, A_log_f, A_log_b, D_skip, ssm_norm_g,
              w_out, router_w, w_gate, w_up, w_down):
    b, s, _ = x.shape
    offs = np.cumsum(IN_SPLITS)[:-1].tolist()
    for l in range(DEPTH):
        lam_init = lambda_init(l)
        mod = jnp.einsum('bd,de->be', jax.nn.silu(c), ada_w[l]) + ada_b[l]
        sh1, sc1, g1, sh2, sc2, g2 = jnp.split(mod, 6, axis=-1)
        h = modulate(rmsnorm(x, norm_mix_g[l]), sh1, sc1)
        p = jnp.einsum('bsd,de->bse', h, w_in[l])
        q, k, v, z, xbc, dt_f, dt_b = jnp.split(p, offs, axis=-1)
        lam = (jnp.exp(jnp.sum(lambda_q1[l] * lambda_k1[l]).astype(jnp.float32))
               - jnp.exp(jnp.sum(lambda_q2[l] * lambda_k2[l]).astype(jnp.float32)) + lam_init)
        att = diff_attention(q.reshape(b, s, ATT_HEADS, 2, ATT_QK_DIM),
                             k.reshape(b, s, ATT_HEADS, 2, ATT_QK_DIM),
                             v.reshape(b, s, ATT_HEADS, ATT_V_DIM), lam, rel_bias_table)
        att = (rmsnorm(att, attn_subln_g[l]) * (1.0 - lam_init)).reshape(b, s, ATT_WIDTH)
        ssd = ssd_mixer(z, xbc, dt_f, dt_b, conv_w[l], conv_b[l], dt_bias_f[l], dt_bias_b[l],
                        A_log_f[l], A_log_b[l], D_skip[l], ssm_norm_g[l])
        mix = jnp.einsum('bsm,md->bsd', jnp.concatenate([att, ssd], axis=-1), w_out[l])
        x = x + g1[:, None, :] * mix
        h2 = modulate(rmsnorm(x, norm_ffn_g[l]), sh2, sc2)
        x = x + g2[:, None, :] * expert_choice_ffn(h2, router_w[l], w_gate[l], w_up[l], w_down[l])
    return rmsnorm(x, norm_final_g)
```

```python
import contextlib
import math
import numpy as np
import concourse.bass as bass
import concourse.mybir as mybir
from concourse.bass_utils import run_bass_kernel_spmd

F32 = mybir.dt.float32
BF16 = mybir.dt.bfloat16
ALU = mybir.AluOpType
AF = mybir.ActivationFunctionType
AX = mybir.AxisListType

S = 2048
D = 1024
NT = 16
EPS = 1e-6
NEXP = 16
FF = 2816
NFC = 22
CAP = 256
LAM_INIT = 0.8 - 0.6 * math.exp(0.0)


class Prog:
    ENGS = ('pe', 'act', 'dve', 'pool', 'sp')

    def __init__(self, nc, esems, dma_sems):
        self.nc = nc
        self.esem = esems
        self.ecnt = {e: 0 for e in self.ENGS}
        self.dsem = {}
        self.free_dsems = list(dma_sems)
        self.ops = []
        self.lastw = {}
        self.readers = {}
        self.known = {e: {} for e in self.ENGS}

    def _dma_token(self, key):
        if key not in self.dsem:
            self.dsem[key] = [self.free_dsems.pop(), 0]
        ent = self.dsem[key]
        ent[1] += 16
        return ('d', key, ent[1])

    def op(self, eng, fn, reads=(), writes=(), dma=None):
        idx = len(self.ops)
        deps = set()
        for r in reads:
            w = self.lastw.get(r)
            if w is not None:
                deps.add(w)
            if isinstance(r, tuple) and r[0] == 'ps':
                for t in self.readers.get(r, ()):
                    if t[0] == 'c' and self.ops[t[1]]['eng'] != eng:
                        deps.add(t)
        for r in writes:
            w = self.lastw.get(r)
            if w is not None:
                deps.add(w)
            for t in self.readers.get(r, ()):
                if t[0] == 'c' and dma is None and self.ops[t[1]]['eng'] == eng and eng == 'pe':
                    continue
                deps.add(t)
        tok = self._dma_token(dma) if dma is not None else ('c', idx)
        fdeps = set()
        for t in deps:
            if t[0] == 'c' and dma is None and eng == 'pe' and self.ops[t[1]]['eng'] == 'pe':
                continue
            fdeps.add(t)
        self.ops.append(dict(eng=eng, fn=fn, deps=fdeps, dma=dma, tok=tok))
        for r in reads:
            self.readers.setdefault(r, []).append(tok)
        for r in writes:
            self.lastw[r] = tok
            self.readers[r] = []
        return tok

    def emit_phase(self):
        self.phase_no = getattr(self, 'phase_no', 0) + 1
        with self.nc.named_scope('ph%d' % self.phase_no):
            self._emit_phase()

    def _emit_phase(self):
        nc = self.nc
        ops = self.ops
        sig = set()
        for o in ops:
            for t in o['deps']:
                if t[0] == 'c':
                    sig.add(t[1])
        cnt = {}
        for i, o in enumerate(ops):
            if i in sig:
                self.ecnt[o['eng']] += 1
                cnt[i] = self.ecnt[o['eng']]
        per = {e: [] for e in self.ENGS}
        for i, o in enumerate(ops):
            per[o['eng']].append(i)

        def run(eng_name, engobj):
            kn = self.known[eng_name]
            for i in per[eng_name]:
                o = ops[i]
                need = {}
                for t in o['deps']:
                    if t[0] == 'c':
                        key = ('e', ops[t[1]]['eng'])
                        val = cnt[t[1]]
                    else:
                        key = ('d', t[1])
                        val = t[2]
                        if t[1] in ('cst', 'cstB'):
                            val = self.dsem[t[1]][1]
                    if kn.get(key, 0) >= val:
                        continue
                    need[key] = max(need.get(key, 0), val)
                for key, val in need.items():
                    sem = self.esem[key[1]] if key[0] == 'e' else self.dsem[key[1]][0]
                    engobj.wait_ge(sem, val)
                    kn[key] = val
                ins = o['fn'](engobj)
                if o['dma'] is not None:
                    ins.then_inc(self.dsem[o['dma']][0], 16)
                elif i in sig:
                    ins.then_inc(self.esem[o['eng']], 1)

        with nc.Block() as block:
            if per['pe']:
                @block.tensor
                def _(e):
                    run('pe', e)
            if per['act']:
                @block.scalar
                def _(e):
                    run('act', e)
            if per['dve']:
                @block.vector
                def _(e):
                    run('dve', e)
            if per['pool']:
                @block.gpsimd
                def _(e):
                    run('pool', e)
            if per['sp']:
                @block.sync
                def _(e):
                    run('sp', e)
        for r in list(self.lastw.keys()):
            if self.lastw[r] is not None and self.lastw[r][0] == 'c':
                self.lastw[r] = None
        for r in list(self.readers.keys()):
            self.readers[r] = [t for t in self.readers[r] if t[0] == 'd']
        self.ops = []

    def final_wait_all_dma(self):
        nc = self.nc
        with nc.Block() as block:
            @block.sync
            def _(e):
                for key, (sem, val) in self.dsem.items():
                    if val > 0:
                        e.wait_ge(sem, val)


def build(stop_after=None, dbg=None):
    dbg = dbg or {}
    nc = bass.Bass("TRN2", target_bir_lowering=False)

    def din(name, shape):
        return nc.dram_tensor(name, list(shape), F32, kind="ExternalInput").ap()

    x_d = din("x", [S, D])
    ccol_d = din("c_col", [128, 8])
    adaw_d = din("ada_w", [D, 6 * D])
    adabrow_d = din("ada_brow", [1, 6 * D])
    adabg_d = din("ada_bg", [128, 4096])
    gmixT_d = din("gmixT", [128, 8])
    gffn_d = din("gffn_bc", [128, D])
    gfin_d = din("gfin_bc", [128, D])
    win_d = din("w_in", [D, 2832])
    lamqk_d = din("lam_qk", [128, 256])
    subln_d = din("subln_bc", [128, 128])
    biasblk_d = din("biasblk", [128, 3, 4, 128])
    cfar_d = din("cfar", [128, 8])
    convw_d = din("conv_wT", [128, 6, 5])
    convb_d = din("conv_bT", [128, 6])
    dtb_d = din("dtb256", [128, 256])
    alog_d = din("alog256", [128, 256])
    dskip_d = din("dskip_bc", [128, 8])
    ssmg_d = din("ssmg_bc", [128, 512])
    wout_d = din("w_out", [D, D])
    rw_d = din("router_wT", [128, 8, 16])
    if stop_after in (None, 'E', 'E1'):
        wg_d = din("w_gate", [NEXP, D, FF])
        wu_d = din("w_up", [NEXP, D, FF])
        wd_d = din("w_down", [NEXP, FF, D])
    out_d = nc.dram_tensor("out", [S, D], F32, kind="ExternalOutput").ap()
    dbg_d = {k: nc.dram_tensor("dbg_" + k, list(shp), F32, kind="ExternalOutput").ap() for k, shp in dbg.items()}

    with contextlib.ExitStack() as top:
        def sbT(name, shape, dt, side='right'):
            return top.enter_context(nc.sbuf_tensor('s_' + name, list(shape), dt, side=side))

        esems = {e: top.enter_context(nc.semaphore('es_' + e)) for e in ('pe', 'act', 'dve', 'pool')}
        dsems = [top.enter_context(nc.semaphore('ds%d' % i)) for i in range(48)]
        P = Prog(nc, esems, dsems)
        PS = [top.enter_context(nc.psum_tensor('psb%d' % i, [128, 512], F32)) for i in range(8)]

        def psr(b):
            return ('ps', b)

        ident_f = sbT('ident_f', [128, 128], F32)
        ident_b = sbT('ident_b', [128, 128], BF16)
        ones_f = sbT('ones_f', [128, 128], F32)
        tri_ip = sbT('tri_ip', [128, 128], F32)
        tri_es = sbT('tri_es', [128, 128], F32)
        tri_is = sbT('tri_is', [128, 128], F32)
        tri_ep = sbT('tri_ep', [128, 128], F32)
        negm_f = sbT('negm_f', [128, 128], F32)
        negm_b = sbT('negm_b', [128, 128], F32)
        iota_j = sbT('iota_j', [128, 256], F32)
        iota_p = sbT('iota_p', [128, 2], F32)
        modT = sbT('modT', [128, 48], F32)
        a1 = sbT('a1', [128, 8], F32)
        g2bc = sbT('g2bc', [128, D], F32)
        WP = [sbT('wp%d' % i, [128, 8, 512], BF16) for i in range(3)]
        wp_ctr = [0]

        def wp_next():
            i = wp_ctr[0] % len(WP)
            wp_ctr[0] += 1
            return i

        def load_w(i, src_ap, ncols, nk=8):
            P.op('pool', lambda e: e.dma_start(out=WP[i][:, 0:nk, 0:ncols],
                                               in_=src_ap.rearrange("(c p) n -> p c n", p=128)),
                 writes=[('wp', i)], dma=('wp', i))


        NBLK = [(i * 512, min(512, FF - i * 512)) for i in range(6)]
        prefetched = {}

        def issue_w(ex, kind, fb):
            i = wp_next()
            if kind in ('g', 'u'):
                f0, fw = NBLK[fb]
                src = (wg_d if kind == 'g' else wu_d)[ex, :, f0:f0 + fw]
                load_w(i, src, fw)
            else:
                nk = 4 if fb < 5 else 2
                P.op('pool', lambda e: e.dma_start(out=WP[i][:, 0:2 * nk, :].rearrange("p (k h) n -> p k h n", h=2),
                                                   in_=wd_d[ex, fb * 512:fb * 512 + nk * 128, :].rearrange("(k p) (h n) -> p k h n", p=128, h=2)),
                     writes=[('wp', i)], dma=('wp', i))
            return i

        def get_w(ex, kind, fb):
            key = (ex, kind, fb)
            if key in prefetched:
                return prefetched.pop(key)
            return issue_w(ex, kind, fb)

        def dump(key, ap, reads):
            if key in dbg_d:
                P.op('sp', lambda e: e.dma_start(out=dbg_d[key], in_=ap), reads=reads, dma='dbg')

        P.op('pool', lambda e: e.memset(ident_f[:], 0.0), writes=['ident_f'])
        P.op('pool', lambda e: e.affine_select(ident_f[:], ident_f[:], [[-1, 128]], ALU.not_equal, 1.0, base=0, channel_multiplier=1),
             reads=['ident_f'], writes=['ident_f'])
        P.op('pool', lambda e: e.tensor_copy(ident_b[:], ident_f[:]), reads=['ident_f'], writes=['ident_b'])
        P.op('pool', lambda e: e.memset(ones_f[:], 1.0), writes=['ones_f'])
        for tl, key, cmp_ in ((tri_ip, 'tri_ip', ALU.is_ge), (tri_ep, 'tri_ep', ALU.is_gt)):
            P.op('pool', (lambda tl, cmp_: lambda e: e.affine_select(tl[:], ones_f[:], [[1, 128]], cmp_, 0.0, base=0, channel_multiplier=-1))(tl, cmp_),
                 reads=['ones_f'], writes=[key])
        for tl, key, cmp_ in ((tri_is, 'tri_is', ALU.is_ge), (tri_es, 'tri_es', ALU.is_gt)):
            P.op('pool', (lambda tl, cmp_: lambda e: e.affine_select(tl[:], ones_f[:], [[-1, 128]], cmp_, 0.0, base=0, channel_multiplier=1))(tl, cmp_),
                 reads=['ones_f'], writes=[key])
        zeros_f = sbT('zeros_f', [128, 128], F32)
        zeros_b = sbT('zeros_b', [128, 512], BF16)
        P.op('pool', lambda e: e.memset(zeros_b[:], 0.0), writes=['zeros_b'])
        P.op('pool', lambda e: e.memset(zeros_f[:], 0.0), writes=['zeros_f'])
        P.op('pool', lambda e: e.affine_select(negm_f[:], zeros_f[:], [[1, 128]], ALU.is_ge, -30000.0, base=0, channel_multiplier=-1),
             reads=['zeros_f'], writes=['negm_f'])
        P.op('pool', lambda e: e.affine_select(negm_b[:], zeros_f[:], [[-1, 128]], ALU.is_ge, -30000.0, base=0, channel_multiplier=1),
             reads=['zeros_f'], writes=['negm_b'])
        P.op('pool', lambda e: e.iota(iota_j[:], [[1, 256]], base=0, channel_multiplier=0, allow_small_or_imprecise_dtypes=True), writes=['iota_j'])
        P.op('pool', lambda e: e.iota(iota_p[:], [[128, 2]], base=0, channel_multiplier=1, allow_small_or_imprecise_dtypes=True), writes=['iota_p'])

        bcst = contextlib.ExitStack()
        bc3 = bcst.enter_context(nc.sbuf_tensor('s_bc3', [128, 3, D], F32, side='left'))

        def bcrow(i):
            return bc3[:, i, :] if i < 3 else g2bc[:, :]

        mixer = contextlib.ExitStack()
        mixT = mixer.enter_context(nc.sbuf_tensor('mixT', [128, 8, S], BF16, side='left'))
        hstack = contextlib.ExitStack()
        hT = hstack.enter_context(nc.sbuf_tensor('hT', [128, 8, S], BF16, side='left'))

        with contextlib.ExitStack() as ph:
            def sb(name, shape, dt):
                return ph.enter_context(nc.sbuf_tensor('s_' + name, list(shape), dt, side='left'))
            adaw = [sb('adaw%d' % i, [128, 8, 512], F32) for i in range(2)]
            xt = [sb('xt%d' % i, [128, D], F32) for i in range(2)]
            xn = [sb('xn%d' % i, [128, D], F32) for i in range(2)]
            ccol = sb('ccol', [128, 8], F32)
            scv = sb('scv', [128, 8], F32)
            scb = sb('scb', [128, 8, 128], F32)
            abr = [sb('abr%d' % i, [1, 512], F32) for i in range(2)]
            rowt = [sb('rowt%d' % i, [1, 512], F32) for i in range(2)]
            abg = sb('abg', [128, 4096], F32)
            gmixT = sb('gmixT', [128, 8], F32)
            gffn = sb('gffn', [128, D], F32)
            ss = sb('ss', [128, 16], F32)
            ms = sb('ms', [128, 16], F32)
            sq = sb('sq', [128, 16], F32)
            rstd = sb('rstd', [128, 16], F32)

            P.op('sp', lambda e: e.dma_start(out=ccol[:], in_=ccol_d), writes=['ccol'], dma='cst')
            P.op('sp', lambda e: e.dma_start(out=gmixT[:], in_=gmixT_d), writes=['gmixT'], dma='cst')
            P.op('sp', lambda e: e.dma_start(out=abg[:], in_=adabg_d), writes=['abg'], dma='cst')
            P.op('sp', lambda e: e.dma_start(out=gffn[:], in_=gffn_d), writes=['gffn'], dma='cst')
            P.op('act', lambda e: e.activation(out=scv[:], in_=ccol[:], func=AF.Silu), reads=['ccol'], writes=['scv'])
            P.op('dve', lambda e: e.tensor_copy(scb[:], scv[:].unsqueeze(2).to_broadcast([128, 8, 128])), reads=['scv'], writes=['scb'])
            adaw_v = adaw_d.rearrange("(c p) n -> p c n", p=128)
            gi_ctr = [0]

            def ada_block(blk):
                ab = blk % 2
                gi = gi_ctr[0]
                bk = 1 + ab
                P.op('sp', (lambda ab, blk: lambda e: e.dma_start(out=adaw[ab][:], in_=adaw_v[:, :, blk * 512:(blk + 1) * 512]))(ab, blk),
                     writes=[('adaw', ab)], dma=('adaw', ab))
                P.op('sp', (lambda ab, blk: lambda e: e.dma_start(out=abr[ab][:], in_=adabrow_d[:, blk * 512:(blk + 1) * 512]))(ab, blk),
                     writes=[('abr', ab)], dma=('abr', ab))
                for c in range(8):
                    P.op('pe', (lambda ab, bk, c: lambda e: e.matmul(PS[bk][:, :], scb[:, c, :], adaw[ab][:, c, :], start=(c == 0), stop=(c == 7)))(ab, bk, c),
                         reads=[('adaw', ab), 'scb'], writes=[psr(bk)])
                P.op('act', (lambda ab, bk, blk: lambda e: e.activation(out=rowt[ab][0:1, :], in_=PS[bk][0:1, :], func=AF.Copy))(ab, bk, blk), reads=[psr(bk)], writes=[('rowt', ab)])
                P.op('dve', (lambda ab, blk: lambda e: e.tensor_tensor(rowt[ab][0:1, :], rowt[ab][0:1, :], abr[ab][0:1, :], ALU.add))(ab, blk), reads=[('rowt', ab), ('abr', ab)], writes=[('rowt', ab)])
                if blk >= 4:
                    P.op('dve', (lambda bk, gi: lambda e: e.tensor_tensor(bcrow(gi // 2)[:, (gi % 2) * 512:(gi % 2 + 1) * 512], PS[bk][:, :], abg[:, gi * 512:(gi + 1) * 512], ALU.add))(bk, gi),
                         reads=[psr(bk), 'abg'], writes=[('bc4', gi // 2)])
                    gi_ctr[0] += 1
                for jj in range(4):
                    j = blk * 4 + jj
                    P.op('pe', (lambda ab, jj, j: lambda e: e.transpose(PS[0][:, j:j + 1], rowt[ab][0:1, jj * 128:(jj + 1) * 128], ident_f[0:1, 0:1]))(ab, jj, j),
                         reads=[('rowt', ab), 'ident_f'], writes=[psr(0)])
            for blk in range(4):
                ada_block(blk)
            P.op('dve', lambda e: e.tensor_copy(modT[:, 0:16], PS[0][:, 0:16]), reads=[psr(0)], writes=[('modT', 0)])
            P.op('dve', lambda e: e.scalar_tensor_tensor(a1[:], modT[:, 8:16], 1.0, gmixT[:], ALU.add, ALU.mult), reads=[('modT', 0), 'gmixT'], writes=['a1'])
            P.op('dve', lambda e: e.memset(ss[:], 0.0), writes=['ss'])
            def norm_tile(t):
                b = t % 2
                P.op('sp', (lambda b, t: lambda e: e.dma_start(out=xt[b][:], in_=x_d[t * 128:(t + 1) * 128, :]))(b, t), writes=[('xt', b)], dma=('xt', b))
                P.op('act', (lambda b, t: lambda e: e.activation(out=xn[b][:], in_=xt[b][:], func=AF.Square, accum_out=ss[:, t:t + 1]))(b, t),
                     reads=[('xt', b), 'ss'], writes=[('xn', b), ('ss', t)])
                P.op('dve', (lambda t: lambda e: e.tensor_scalar(ms[:, t:t + 1], ss[:, t:t + 1], 1.0 / D, EPS, ALU.mult, ALU.add))(t), reads=[('ss', t)], writes=[('ms', t)])
                P.op('act', (lambda t: lambda e: e.activation(out=sq[:, t:t + 1], in_=ms[:, t:t + 1], func=AF.Sqrt))(t), reads=[('ms', t)], writes=[('sq', t)])
                P.op('dve', (lambda t: lambda e: e.reciprocal(rstd[:, t:t + 1], sq[:, t:t + 1]))(t), reads=[('sq', t)], writes=[('rstd', t)])
                P.op('dve', (lambda b, t: lambda e: e.tensor_scalar(xn[b][:], xt[b][:], rstd[:, t:t + 1], None, ALU.mult))(b, t),
                     reads=[('xt', b), ('rstd', t)], writes=[('xn', b)])
                for c in range(8):
                    bk = 3 + 2 * b + c // 4
                    P.op('pe', (lambda b, bk, c: lambda e: e.transpose(PS[bk][:, (c % 4) * 128:(c % 4 + 1) * 128], xn[b][:, c * 128:(c + 1) * 128], ident_f[:]))(b, bk, c),
                         reads=[('xn', b), 'ident_f'], writes=[psr(bk)])
                for c in range(8):
                    bk = 3 + 2 * b + c // 4
                    if c // 4 == 0:
                        P.op('act', (lambda bk, c, t: lambda e: e.activation(out=hT[:, c, t * 128:(t + 1) * 128], in_=PS[bk][:, (c % 4) * 128:(c % 4 + 1) * 128], func=AF.Identity, scale=a1[:, c:c + 1], bias=modT[:, c:c + 1]))(bk, c, t),
                             reads=[psr(bk), 'a1', ('modT', 0)], writes=[('hT', c, t)])
                    else:
                        P.op('dve', (lambda bk, c, t: lambda e: e.tensor_scalar(hT[:, c, t * 128:(t + 1) * 128], PS[bk][:, (c % 4) * 128:(c % 4 + 1) * 128], a1[:, c:c + 1], modT[:, c:c + 1], ALU.mult, ALU.add))(bk, c, t),
                             reads=[psr(bk), 'a1', ('modT', 0)], writes=[('hT', c, t)])
            for i_ in range(8):
                ada_block(4 + i_)
                norm_tile(2 * i_)
                norm_tile(2 * i_ + 1)
            P.op('dve', lambda e: e.tensor_copy(modT[:, 16:48], PS[0][:, 16:48]), reads=[psr(0)], writes=[('modT', 1)])
            P.op('dve', lambda e: e.scalar_tensor_tensor(bc3[:, 2, :], bc3[:, 2, :], 1.0, gffn[:], ALU.add, ALU.mult), reads=[('bc4', 2), 'gffn'], writes=[('bc4', 2)])
            dump('modT', modT[:], [('modT', 0), ('modT', 1)])
            P.emit_phase()

        def hT_reads(c, Q):
            return [('hT', c, 4 * Q + i) for i in range(4)]

        if stop_after == 'A':
            with contextlib.ExitStack() as ph:
                tmp = ph.enter_context(nc.sbuf_tensor('dbgtmp', [128, S], F32, side='left'))
                for c in range(8):
                    P.op('dve', (lambda c: lambda e: e.tensor_copy(tmp[:], hT[:, c, :]))(c), reads=[('hT', c, t) for t in range(NT)], writes=['dbgtmp'])
                    P.op('sp', (lambda c: lambda e: e.dma_start(out=dbg_d['hT'][c], in_=tmp[:]))(c), reads=['dbgtmp'], dma='dbg')
                P.emit_phase()
            P.final_wait_all_dma()
            hstack.close()
            mixer.close()
            bcst.close()
            return nc

        with contextlib.ExitStack() as ph:
            def sb(name, shape, dt):
                return ph.enter_context(nc.sbuf_tensor('s_' + name, list(shape), dt, side='left'))
            qT = sb('qT', [128, 2, 4, S], BF16)
            kT = sb('kT', [128, 4, S], BF16)
            vaug = sb('vaug', [128, NT, 4, 130], BF16)
            biasb = sb('biasb', [128, 3, 4, 128], BF16)
            cfar = sb('cfar', [128, 8], F32)
            lamqk = sb('lamqk', [128, 256], F32)
            lprod = sb('lprod', [128, 256], F32)
            lsum = sb('lsum', [128, 4], F32)
            lamneg = sb('lamneg', [128, 1], F32)
            g08 = sb('g08', [128, 128], F32)
            PT = [sb('PT%d' % i, [128, 512], BF16) for i in range(4)]
            accsb = [sb('accsb%d' % i, [128, 8, 129], F32) for i in range(2)]
            rr = [sb('rr%d' % i, [128, 16], F32) for i in range(2)]
            t0b = [sb('t0b%d' % i, [128, 128], F32) for i in range(2)]
            attb = [sb('attb%d' % i, [128, 128], F32) for i in range(4)]
            attn = [sb('attn%d' % i, [128, 128], F32) for i in range(4)]
            junk = sb('junkb', [128, 128], F32)
            ssa = sb('ssa', [128, 64], F32)
            msa = sb('msa', [128, 64], F32)
            sqa = sb('sqa', [128, 64], F32)
            rsa = sb('rsa', [128, 64], F32)

            P.op('pool', lambda e: e.dma_start(out=biasb[:], in_=biasblk_d), writes=['biasb'], dma='cstB')
            P.op('sp', lambda e: e.dma_start(out=cfar[:], in_=cfar_d), writes=['cfar'], dma='cst')
            P.op('sp', lambda e: e.dma_start(out=lamqk[:], in_=lamqk_d), writes=['lamqk'], dma='cst')
            P.op('sp', lambda e: e.dma_start(out=g08[:], in_=subln_d), writes=['g08'], dma='cst')
            P.op('dve', lambda e: e.tensor_tensor(lprod[:, 0:64], lamqk[:, 0:64], lamqk[:, 64:128], ALU.mult), reads=['lamqk'], writes=['lprod'])
            P.op('dve', lambda e: e.tensor_tensor(lprod[:, 64:128], lamqk[:, 128:192], lamqk[:, 192:256], ALU.mult), reads=['lamqk', 'lprod'], writes=['lprod'])
            P.op('dve', lambda e: e.reduce_sum(lsum[:, 0:1], lprod[:, 0:64], axis=AX.X), reads=['lprod'], writes=['lsum'])
            P.op('dve', lambda e: e.reduce_sum(lsum[:, 1:2], lprod[:, 64:128], axis=AX.X), reads=['lprod', 'lsum'], writes=['lsum'])
            P.op('act', lambda e: e.activation(out=lsum[:, 2:4], in_=lsum[:, 0:2], func=AF.Exp), reads=['lsum'], writes=['lsum'])
            P.op('dve', lambda e: e.tensor_tensor(lamneg[:], lsum[:, 3:4], lsum[:, 2:3], ALU.subtract), reads=['lsum'], writes=['lamneg'])
            P.op('dve', lambda e: e.tensor_scalar(lamneg[:], lamneg[:], -LAM_INIT, None, ALU.add), reads=['lamneg'], writes=['lamneg'])
            P.op('dve', lambda e: e.tensor_scalar(g08[:], g08[:], 1.0 - LAM_INIT, None, ALU.mult), reads=['g08'], writes=['g08'])
            P.op('dve', lambda e: e.memset(vaug[:, :, :, 128:130], 1.0), writes=['vones'])
            P.op('dve', lambda e: e.memset(ssa[:], 0.0), writes=[('ssa', c) for c in range(64)])

            wq, wk, wv = wp_next(), wp_next(), wp_next()
            load_w(wq, win_d[:, 0:512], 512)
            load_w(wk, win_d[:, 512:1024], 512)
            load_w(wv, win_d[:, 1024:1536], 512)
            ev = [0]
            P.op('pool', lambda e: e.memset(qT[64:128, 0, :, :], 0.0), writes=['qTz0'])
            P.op('pool', lambda e: e.memset(qT[0:64, 1, :, :], 0.0), writes=['qTz1'])
            def proj_unit(dname, wi, h, Q, bk, eng):
                for c in range(8):
                    P.op('pe', (lambda c: lambda e: e.matmul(PS[bk][:, :], WP[wi][:, c, h * 128:(h + 1) * 128], hT[:, c, Q * 512:(Q + 1) * 512], start=(c == 0), stop=(c == 7)))(c),
                         reads=[('wp', wi)] + hT_reads(c, Q), writes=[psr(bk)])
                if dname == 'kT':
                    if eng == 'act':
                        P.op('act', lambda e: e.activation(out=kT[:, h, Q * 512:(Q + 1) * 512], in_=PS[bk][:, :], func=AF.Copy), reads=[psr(bk)], writes=[('kT', h, Q)])
                    else:
                        P.op('dve', lambda e: e.tensor_copy(kT[:, h, Q * 512:(Q + 1) * 512], PS[bk][:, :]), reads=[psr(bk)], writes=[('kT', h, Q)])
                else:
                    for m in range(2):
                        ps_ = slice(m * 64, (m + 1) * 64)
                        if eng == 'act':
                            P.op('act', (lambda m, ps_: lambda e: e.activation(out=qT[ps_, m, h, Q * 512:(Q + 1) * 512], in_=PS[bk][ps_, :], func=AF.Copy, scale=0.125))(m, ps_),
                                 reads=[psr(bk), 'qTz0', 'qTz1'], writes=[('qT', h, Q, m)])
                        else:
                            P.op('dve', (lambda m, ps_: lambda e: e.tensor_scalar(qT[ps_, m, h, Q * 512:(Q + 1) * 512], PS[bk][ps_, :], 0.125, None, ALU.mult))(m, ps_),
                                 reads=[psr(bk), 'qTz0', 'qTz1'], writes=[('qT', h, Q, m)])

            for t in range(NT):
                bk = ev[0] % 2
                for c in range(8):
                    P.op('pe', (lambda bk, c, t: lambda e: e.matmul(PS[bk][:, :], hT[:, c, t * 128:(t + 1) * 128], WP[wv][:, c, :], start=(c == 0), stop=(c == 7)))(bk, c, t),
                         reads=[('wp', wv), ('hT', c, t)], writes=[psr(bk)])
                eng = 'act' if ev[0] % 2 == 0 else 'dve'
                if eng == 'act':
                    P.op('act', (lambda bk, t: lambda e: e.activation(out=vaug[:, t, :, 0:128], in_=PS[bk][:, :].rearrange("p (h d) -> p h d", h=4), func=AF.Copy))(bk, t),
                         reads=[psr(bk)], writes=[('v', t)])
                else:
                    P.op('dve', (lambda bk, t: lambda e: e.tensor_copy(vaug[:, t, :, 0:128], PS[bk][:, :].rearrange("p (h d) -> p h d", h=4)))(bk, t),
                         reads=[psr(bk)], writes=[('v', t)])
                ev[0] += 1

            for Q in range(4):
                for dname, wi in (('qT', wq), ('kT', wk)):
                    proj_unit(dname, wi, 0, Q, ev[0] % 2, 'act' if ev[0] % 2 == 0 else 'dve')
                    ev[0] += 1
            pending_proj = {h_: [(dname, wi, h_, Q) for Q in range(4) for dname, wi in (('qT', wq), ('kT', wk))] for h_ in range(1, 4)}

            ACCB = (2, 3, 4)
            steps = [(h, Q, j, m) for h in range(4) for Q in range(4) for j in range(NT) for m in range(2)]
            LA = 3
            SBK = (1, 5, 6, 7)

            def cats_of(Q, j):
                cats = []
                for qi in range(4):
                    d = j - (4 * Q + qi)
                    cats.append('n' if abs(d) <= 1 else ('lo' if d < 0 else 'hi'))
                return cats

            def emit_qk(idx):
                h, Q, j, m = steps[idx]
                sbk = SBK[idx % 4]
                cats = cats_of(Q, j)
                nnear = sum(1 for cc in cats if cc == 'n')
                P.op('pe', (lambda sbk, m, h, j, Q, nnear: lambda e: e.matmul(PS[sbk][:, :], kT[:, h, j * 128:(j + 1) * 128], qT[:, m, h, Q * 512:(Q + 1) * 512], start=True, stop=(nnear == 0)))(sbk, m, h, j, Q, nnear),
                     reads=[('kT', h, j // 4), ('qT', h, Q, m), 'qTz0', 'qTz1'], writes=[psr(sbk)])
                k = 0
                for qi in range(4):
                    if cats[qi] == 'n':
                        k += 1
                        d = j - (4 * Q + qi)
                        P.op('pe', (lambda sbk, qi, d, h, last: lambda e: e.matmul(PS[sbk][:, qi * 128:(qi + 1) * 128], ident_b[:], biasb[:, d + 1, h, :], start=False, stop=last))(sbk, qi, d, h, k == nnear),
                             reads=['biasb', 'ident_b'], writes=[psr(sbk)])

            def emit_exp_pv(idx):
                h, Q, j, m = steps[idx]
                sbk = SBK[idx % 4]
                pb = idx % 4
                cats = cats_of(Q, j)
                r0 = 0
                ri = 0
                while r0 < 4:
                    r1 = r0
                    while r1 < 4 and cats[r1] == cats[r0]:
                        r1 += 1
                    cat = cats[r0]
                    if cat == 'n':
                        P.op('act', (lambda pb, sbk, r0, r1: lambda e: e.activation(out=PT[pb][:, r0 * 128:r1 * 128], in_=PS[sbk][:, r0 * 128:r1 * 128], func=AF.Exp))(pb, sbk, r0, r1),
                             reads=[psr(sbk)], writes=[('PT', pb, ri)])
                    else:
                        ci = h if cat == 'lo' else 4 + h
                        P.op('act', (lambda pb, sbk, r0, r1, ci: lambda e: e.activation(out=PT[pb][:, r0 * 128:r1 * 128], in_=PS[sbk][:, r0 * 128:r1 * 128], func=AF.Exp, bias=cfar[:, ci:ci + 1]))(pb, sbk, r0, r1, ci),
                             reads=[psr(sbk), 'cfar'], writes=[('PT', pb, ri)])
                    r0 = r1
                    ri += 1
                if j == 0 and m == 0:
                    for bi_, bkk_ in enumerate(ACCB):
                        n_ = 3 if bi_ < 2 else 2
                        P.op('pe', (lambda bkk_, n_: lambda e: e.matmul(PS[bkk_][:, 0:n_ * 129], zeros_b[:, 0:128], zeros_b[:, 0:n_ * 129], start=True, stop=False))(bkk_, n_),
                             reads=['zeros_b'], writes=[psr(bkk_)])
                for qi in range(4):
                    a = m * 4 + qi
                    ab_, ao = ACCB[a // 3], (a % 3) * 129
                    P.op('pe', (lambda ab_, ao, pb, qi, j, h, a: lambda e: e.matmul(PS[ab_][:, ao:ao + 129], PT[pb][:, qi * 128:(qi + 1) * 128], vaug[:, j, h, 0:129], start=False, stop=(j == NT - 1 and (a % 3 == 2 or a == 7))))(ab_, ao, pb, qi, j, h, a),
                         reads=[('PT', pb, 0), ('PT', pb, 1), ('PT', pb, 2), ('v', j), 'vones'], writes=[psr(ab_)])
                if j == NT - 1 and m == 1:
                    epilogue(h, Q, (h * 4 + Q))

            deferred = {}
            cur_idx = [0]

            def defer(at, fn):
                deferred.setdefault(at, []).append(fn)

            def epilogue(h, Q, it):
                ai = it % 2
                cur = cur_idx[0]
                accr = [('accsb', ai, bi) for bi in range(3)]
                for bi, bkk in enumerate(ACCB):
                    n = 3 if bi < 2 else 2
                    P.op('dve', (lambda ai, bi, bkk, n: lambda e: e.tensor_copy(accsb[ai][:, bi * 3:bi * 3 + n, :], PS[bkk][:, 0:n * 129].rearrange("p (a d) -> p a d", a=n)))(ai, bi, bkk, n),
                         reads=[psr(bkk)], writes=[('accsb', ai, bi)])
                P.op('dve', (lambda ai: lambda e: e.reciprocal(rr[ai][:, 0:8], accsb[ai][:, :, 128]))(ai), reads=accr, writes=[('rr', ai)])
                P.op('dve', (lambda ai: lambda e: e.tensor_scalar(rr[ai][:, 8:12], rr[ai][:, 4:8], lamneg[:, 0:1], None, ALU.mult))(ai), reads=[('rr', ai), 'lamneg'], writes=[('rr', ai)])
                for qi in range(4):
                    col = it * 4 + qi
                    P.op('dve', (lambda ai, qi: lambda e: e.tensor_scalar(t0b[qi % 2][:], accsb[ai][:, qi, 0:128], rr[ai][:, qi:qi + 1], None, ALU.mult))(ai, qi),
                         reads=accr + [('rr', ai)], writes=[('t0b', qi % 2)])
                    P.op('dve', (lambda ai, qi: lambda e: e.scalar_tensor_tensor(attb[qi][:], accsb[ai][:, 4 + qi, 0:128], rr[ai][:, 8 + qi:9 + qi], t0b[qi % 2][:], ALU.mult, ALU.add))(ai, qi),
                         reads=accr + [('rr', ai), ('t0b', qi % 2)], writes=[('attb', qi)])
                    P.op('dve', (lambda qi: lambda e: e.tensor_tensor(junk[:], attb[qi][:], attb[qi][:], ALU.mult))(qi), reads=[('attb', qi)], writes=['junkb'])
                    P.op('dve', (lambda col: lambda e: e.reduce_sum(ssa[:, col:col + 1], junk[:], axis=AX.X))(col), reads=['junkb'], writes=[('ssa', col)])
                sl = slice(it * 4, it * 4 + 4)
                P.op('dve', (lambda sl: lambda e: e.tensor_scalar(msa[:, sl], ssa[:, sl], 1.0 / 128, EPS, ALU.mult, ALU.add))(sl), reads=[('ssa', it * 4 + q_) for q_ in range(4)], writes=[('msa', it)])

                def st2():
                    P.op('act', (lambda sl: lambda e: e.activation(out=sqa[:, sl], in_=msa[:, sl], func=AF.Ln))(sl), reads=[('msa', it)], writes=[('sqa', it)])
                    P.op('act', (lambda sl: lambda e: e.activation(out=rsa[:, sl], in_=sqa[:, sl], func=AF.Exp, scale=-0.5))(sl), reads=[('sqa', it)], writes=[('rsa', it)])

                def st3():
                    for qi in range(4):
                        col = it * 4 + qi
                        P.op('dve', (lambda qi, col: lambda e: e.scalar_tensor_tensor(attn[qi][:], attb[qi][:], rsa[:, col:col + 1], g08[:], ALU.mult, ALU.mult))(qi, col),
                             reads=[('attb', qi), ('rsa', it), 'g08'], writes=[('attn', qi)])

                def st4():
                    tb = 0
                    for qi in range(4):
                        P.op('pe', (lambda tb, qi: lambda e: e.transpose(PS[tb][:, qi * 128:(qi + 1) * 128], attn[qi][:], ident_f[:]))(tb, qi),
                             reads=[('attn', qi), 'ident_f'], writes=[psr(tb)])

                def st5():
                    tb = 0
                    P.op('dve', (lambda tb, h, Q: lambda e: e.tensor_copy(mixT[:, h, Q * 512:(Q + 1) * 512], PS[tb][:, :]))(tb, h, Q),
                         reads=[psr(tb)], writes=[('mixT', h, 4 * Q + q_) for q_ in range(4)])
                defer(cur + 14, st2)
                defer(cur + 18, st3)
                defer(cur + 22, st4)
                defer(cur + 26, st5)

            for idx in range(len(steps) + LA + 32):
                cur_idx[0] = idx
                if idx < len(steps):
                    emit_qk(idx)
                    h_cur = steps[idx][0]
                    if (idx % 128) % 16 == 8 and h_cur + 1 < 4 and pending_proj[h_cur + 1]:
                        proj_unit(*pending_proj[h_cur + 1].pop(0), 0, 'dve')
                if LA <= idx < len(steps) + LA:
                    emit_exp_pv(idx - LA)
                for fn in deferred.pop(idx, []):
                    fn()
            assert not deferred
            P.emit_phase()

        if stop_after == 'B':
            with contextlib.ExitStack() as ph:
                tmp = ph.enter_context(nc.sbuf_tensor('dbgtmp', [128, S], F32, side='left'))
                for c in range(4):
                    P.op('dve', (lambda c: lambda e: e.tensor_copy(tmp[:], mixT[:, c, :]))(c), reads=[('mixT', c, t) for t in range(NT)], writes=['dbgtmp'])
                    P.op('sp', (lambda c: lambda e: e.dma_start(out=dbg_d['mixT'][c], in_=tmp[:]))(c), reads=['dbgtmp'], dma='dbg')
                P.emit_phase()
            P.final_wait_all_dma()
            hstack.close()
            mixer.close()
            bcst.close()
            return nc

        cst = contextlib.ExitStack()

        def sbC(name, shape, dt):
            return cst.enter_context(nc.sbuf_tensor('s_' + name, list(shape), dt, side='right'))
        BTm = sbC('BTm', [128, 2, S], BF16)
        CTm = sbC('CTm', [128, 2, S], BF16)
        xs_tok = sbC('xs_tok', [128, NT, 512], BF16)
        B_tok = sbC('B_tok', [128, NT, 128], BF16)
        zs = sbC('zs', [128, NT, 512], BF16)
        dt = sbC('dt', [128, 256], F32)
        lndt = sbC('lndt', [128, 256], F32)
        dA = sbC('dA', [128, 256], F32)
        csall = sbC('csall', [128, 512], F32)
        expT = sbC('expT', [128, 256], F32)
        bias_fb = sbC('bias_fb', [128, 256], F32)
        sdec = sbC('sdec', [128, 256], F32)
        eoff = sbC('eoff', [128, 256], F32)
        dskip = sbC('dskip', [128, 8], F32)
        ssmg = sbC('ssmg', [128, 512], F32)

        def v3(ap, n):
            return ap.rearrange("p (t k) -> p t k", t=NT)

        with contextlib.ExitStack() as ph:
            def sb(name, shape, dt_):
                return ph.enter_context(nc.sbuf_tensor('s_' + name, list(shape), dt_, side='left'))
            raw2 = [sb('raw%d' % i, [128, S + 4], F32) for i in range(2)]
            cv = sb('cv', [128, S], F32)
            scr4 = sb('scr4', [128, 1024], F32)
            xsT = [scr4[:, :].bitcast(BF16)] * 2
            convw = sb('convw', [128, 6, 5], F32)
            convb = sb('convb', [128, 6], F32)
            dtb = sb('dtb', [128, 256], F32)
            alog = sb('alog', [128, 256], F32)
            dtx = scr4[:, 0:256]
            tA = scr4[:, 256:512]
            tB = scr4[:, 512:768]
            tC = scr4[:, 768:1024]
            csx = cv[:, 0:512]

            for dst, src, key in ((convw, convw_d, 'convw'), (convb, convb_d, 'convb'), (dtb, dtb_d, 'dtb'), (alog, alog_d, 'alog'),
                                  (dskip, dskip_d, 'dskip'), (ssmg, ssmg_d, 'ssmg')):
                P.op('sp', (lambda dst, src: lambda e: e.dma_start(out=dst[:], in_=src))(dst, src), writes=[key], dma='cst')
            wz, wx, wb5 = wp_next(), wp_next(), wp_next()
            load_w(wz, win_d[:, 1536:2048], 512)
            load_w(wx, win_d[:, 2048:2560], 512)
            load_w(wb5, win_d[:, 2560:2832], 272)
            for rb_ in range(2):
                P.op('dve', (lambda rb_: lambda e: e.memset(raw2[rb_][:, 0:2], 0.0))(rb_), writes=[('rawpadL', rb_)])
                P.op('dve', (lambda rb_: lambda e: e.memset(raw2[rb_][:, S + 2:S + 4], 0.0))(rb_), writes=[('rawpadR', rb_)])
            ev = [0]
            for t in range(NT):
                bk = ev[0] % 2
                ev[0] += 1
                for c in range(8):
                    P.op('pe', (lambda bk, c, t: lambda e: e.matmul(PS[bk][:, :], hT[:, c, t * 128:(t + 1) * 128], WP[wz][:, c, :], start=(c == 0), stop=(c == 7)))(bk, c, t),
                         reads=[('wp', wz), ('hT', c, t)], writes=[psr(bk)])
                P.op('act', (lambda bk, t: lambda e: e.activation(out=zs[:, t, :], in_=PS[bk][:, :], func=AF.Silu))(bk, t), reads=[psr(bk)], writes=[('zs', t)])
            for t in range(NT):
                for c in range(8):
                    P.op('pe', (lambda c, t: lambda e: e.matmul(PS[2][:, t * 16:(t + 1) * 16], hT[:, c, t * 128:(t + 1) * 128], WP[wb5][:, c, 256:272], start=(c == 0), stop=(c == 7)))(c, t),
                         reads=[('wp', wb5), ('hT', c, t)], writes=[psr(2)])
            P.op('dve', lambda e: e.tensor_tensor(dtx[:], PS[2][:, 0:256], dtb[:], ALU.add), reads=[psr(2), 'dtb'], writes=['dtx'])
            P.op('act', lambda e: e.activation(out=tA[:], in_=dtx[:], func=AF.Abs), reads=['dtx'], writes=['tA'])
            P.op('act', lambda e: e.activation(out=tB[:], in_=tA[:], func=AF.Exp, scale=-1.0), reads=['tA'], writes=['tB'])
            P.op('act', lambda e: e.activation(out=tC[:], in_=tB[:], func=AF.Ln, bias=1.0), reads=['tB'], writes=['tC'])
            P.op('dve', lambda e: e.scalar_tensor_tensor(dt[:], dtx[:], 0.0, tC[:], ALU.max, ALU.add), reads=['dtx', 'tC'], writes=['dt'])
            P.op('act', lambda e: e.activation(out=lndt[:], in_=dt[:], func=AF.Ln), reads=['dt'], writes=['lndt'])
            P.op('act', lambda e: e.activation(out=tA[:], in_=alog[:], func=AF.Exp), reads=['alog', 'tA'], writes=['tA2'])
            P.op('dve', lambda e: e.scalar_tensor_tensor(dA[:], tA[:], -1.0, dt[:], ALU.mult, ALU.mult), reads=['tA2', 'dt'], writes=['dA'])
            dA3 = dA[:].rearrange("p (t k) -> p t k", t=NT)
            for t in range(NT):
                for kind, (tri, key, off) in enumerate(((tri_ip, 'tri_ip', 0), (tri_es, 'tri_es', 0), (tri_is, 'tri_is', 8), (tri_ep, 'tri_ep', 8))):
                    P.op('pe', (lambda t, kind, tri, off: lambda e: e.matmul(PS[3][:, t * 32 + kind * 8:t * 32 + kind * 8 + 8], tri[:], dA3[:, t, off:off + 8], start=True, stop=True))(t, kind, tri, off),
                         reads=['dA', key], writes=[psr(3)])
                P.op('pe', (lambda t: lambda e: e.matmul(PS[4][:, t * 16:(t + 1) * 16], ones_f[:], dA3[:, t, :], start=True, stop=True))(t),
                     reads=['dA', 'ones_f'], writes=[psr(4)])
            P.op('dve', lambda e: e.tensor_copy(csall[:], PS[3][:, :]), reads=[psr(3)], writes=['csall'])
            P.op('act', lambda e: e.activation(out=expT[:], in_=PS[4][:, 0:256], func=AF.Exp), reads=[psr(4)], writes=['expT'])
            cs4 = csall[:].rearrange("p (t k r) -> p t k r", t=NT, k=4)
            dt4 = dt[:].rearrange("p (t d r) -> p t d r", t=NT, d=2)
            ln4 = lndt[:].rearrange("p (t d r) -> p t d r", t=NT, d=2)
            bf4 = bias_fb[:].rearrange("p (t d r) -> p t d r", t=NT, d=2)
            sd4 = sdec[:].rearrange("p (t d r) -> p t d r", t=NT, d=2)
            eo4 = eoff[:].rearrange("p (t d r) -> p t d r", t=NT, d=2)
            cx4 = csx[:].rearrange("p (t k r) -> p t k r", t=NT, k=4)
            P.op('act', lambda e: e.activation(out=csx[:], in_=csall[:], func=AF.Exp), reads=['csall'], writes=['csx'])
            for d_, kcs, kst in ((0, 0, 1), (1, 2, 3)):
                P.op('dve', (lambda d_, kcs: lambda e: e.tensor_tensor(bf4[:, :, d_, :], ln4[:, :, d_, :], cs4[:, :, kcs, :], ALU.subtract))(d_, kcs), reads=['lndt', 'csall'], writes=[('bias_fb', d_)])
                P.op('dve', (lambda d_, kst: lambda e: e.tensor_tensor(sd4[:, :, d_, :], cx4[:, :, kst, :], dt4[:, :, d_, :], ALU.mult))(d_, kst), reads=['csx', 'dt'], writes=[('sdec', d_)])
                P.op('dve', (lambda d_, kcs: lambda e: e.tensor_copy(eo4[:, :, d_, :], cx4[:, :, kcs, :]))(d_, kcs), reads=['csx'], writes=[('eoff', d_)])
            def xbc_p(cc):
                raw = raw2[cc % 2]
                rkey = ('raw', cc % 2)
                wi = wx if cc < 4 else wb5
                off = (cc % 4) * 128 if cc < 4 else (cc - 4) * 128
                for Q in range(4):
                    bk = ev[0] % 2
                    ev[0] += 1
                    for c in range(8):
                        P.op('pe', (lambda bk, wi, off, c, Q: lambda e: e.matmul(PS[bk][:, :], WP[wi][:, c, off:off + 128], hT[:, c, Q * 512:(Q + 1) * 512], start=(c == 0), stop=(c == 7)))(bk, wi, off, c, Q),
                             reads=[('wp', wi)] + hT_reads(c, Q), writes=[psr(bk)])
                    P.op('act', (lambda bk, Q, raw: lambda e: e.activation(out=raw[:, 2 + Q * 512:2 + (Q + 1) * 512], in_=PS[bk][:, :], func=AF.Copy))(bk, Q, raw), reads=[psr(bk)], writes=[rkey])

            def xbc_c(cc):
                raw = raw2[cc % 2]
                rkey = ('raw', cc % 2)
                wi = wx if cc < 4 else wb5
                off = (cc % 4) * 128 if cc < 4 else (cc - 4) * 128
                P.op('dve', (lambda cc, raw: lambda e: e.tensor_scalar(cv[:], raw[:, 0:S], convw[:, cc, 0:1], None, ALU.mult))(cc, raw), reads=[rkey, ('rawpadL', cc % 2), ('rawpadR', cc % 2), 'convw'], writes=['cv', 'csx'])
                for j in range(1, 5):
                    P.op('dve', (lambda cc, j, raw: lambda e: e.scalar_tensor_tensor(cv[:], raw[:, j:j + S], convw[:, cc, j:j + 1], cv[:], ALU.mult, ALU.add))(cc, j, raw), reads=[rkey, 'cv', 'convw'], writes=['cv'])

            def xbc_s(cc):
                raw = raw2[cc % 2]
                rkey = ('raw', cc % 2)
                wi = wx if cc < 4 else wb5
                off = (cc % 4) * 128 if cc < 4 else (cc - 4) * 128
                if cc < 4:
                    xb = cc % 2
                    P.op('act', (lambda xb, cc: lambda e: e.activation(out=xsT[xb][:], in_=cv[:], func=AF.Silu, bias=convb[:, cc:cc + 1]))(xb, cc), reads=['cv', 'convb'], writes=[('xsT', 0), 'dtx', 'tA', 'tB', 'tC', 'tA2'])
                    for t in range(NT):
                        P.op('pe', (lambda xb, t, cc: lambda e: e.matmul(PS[5 + (t // 4) % 2][:, (t % 4) * 128:(t % 4 + 1) * 128], xsT[xb][:, t * 128:(t + 1) * 128], ident_b[:], start=True, stop=True))(xb, t, cc),
                             reads=[('xsT', 0), 'ident_b'], writes=[psr(5 + (t // 4) % 2)])
                        if t % 4 == 3:
                            t0_ = t - 3
                            P.op('dve', (lambda t0_, t, cc: lambda e: e.tensor_copy(xs_tok[:, t0_:t0_ + 4, cc * 128:(cc + 1) * 128], PS[5 + (t // 4) % 2][:, :].rearrange("p (a d) -> p a d", a=4)))(t0_, t, cc),
                                 reads=[psr(5 + (t // 4) % 2)], writes=[('xs_tok', cc)])
                else:
                    tgt, tkey = (BTm, 'BTm') if cc == 4 else (CTm, 'CTm')
                    P.op('act', (lambda cc, tgt: lambda e: e.activation(out=tgt[:, 0, :], in_=cv[:], func=AF.Silu, bias=convb[:, cc:cc + 1]))(cc, tgt), reads=['cv', 'convb'], writes=[(tkey, 0)])
                    if cc == 4:
                        for t in range(NT):
                            P.op('pe', (lambda t: lambda e: e.matmul(PS[5 + (t // 4) % 2][:, (t % 4) * 128:(t % 4 + 1) * 128], BTm[:, 0, t * 128:(t + 1) * 128], ident_b[:], start=True, stop=True))(t),
                                 reads=[('BTm', 0), 'ident_b'], writes=[psr(5 + (t // 4) % 2)])
                            if t % 4 == 3:
                                t0_ = t - 3
                                P.op('dve', (lambda t0_, t: lambda e: e.tensor_copy(B_tok[:, t0_:t0_ + 4, :], PS[5 + (t // 4) % 2][:, :].rearrange("p (a d) -> p a d", a=4)))(t0_, t),
                                     reads=[psr(5 + (t // 4) % 2)], writes=['B_tok'])
                    P.op('dve', (lambda tgt: lambda e: e.tensor_copy(tgt[64:128, 1, :], tgt[64:128, 0, :]))(tgt), reads=[(tkey, 0)], writes=[(tkey, 1)])
                    P.op('dve', (lambda tgt: lambda e: e.memset(tgt[0:64, 1, :], 0.0))(tgt), reads=[], writes=[(tkey, 1)])
                    P.op('dve', (lambda tgt: lambda e: e.memset(tgt[64:128, 0, :], 0.0))(tgt), reads=[], writes=[(tkey, 0)])

            xbc_p(0)
            for cc in range(6):
                if cc + 1 < 6:
                    xbc_p(cc + 1)
                xbc_c(cc)
                xbc_s(cc)
            P.emit_phase()
        hstack.close()
        if stop_after == 'C1':
            for nm, tl in (('dt', dt), ('dA', dA), ('csall', csall), ('bias_fb', bias_fb), ('eoff', eoff), ('sdec', sdec), ('expT', expT)):
                dump(nm, tl[:], [])
            P.emit_phase()
            P.final_wait_all_dma()
            cst.close()
            mixer.close()
            bcst.close()
            return nc

        with contextlib.ExitStack() as ph:
            def sb(name, shape, dt_):
                return ph.enter_context(nc.sbuf_tensor('s_' + name, list(shape), dt_, side='left'))
            Sin = sb('Sin', [128, 2, NT, 256], BF16)
            Srun = sb('Srun', [128, 2, 256], F32)
            Stmp = sb('Stmp', [128, 256], F32)
            Tdec = sb('Tdec', [128, NT, 2, 4], F32)
            Xd = sb('Xd', [128, 2, 512], BF16)
            DI = sb('DI', [128, 8, 128], BF16)
            dAbc = [sb('dAbc%d' % i, [128, 16, 128], F32) for i in range(2)]
            Lt = [sb('Lt%d' % i, [128, 16, 128], F32) for i in range(2)]
            Gsb = [sb('Gsb%d' % i, [128, 256], F32) for i in range(2)]
            Mt = Xd[:, :, :].rearrange("p a (b d) -> p (a b) d", b=4)
            ysb = sb('ysb', [128, 512], F32)
            ytmp = sb('ytmp', [128, 512], F32)
            junkc = sb('junkc', [128, 256], F32)
            ssdo = sb('ssdo', [128, 512], BF16)
            ssg = sb('ssg', [128, 32], F32)
            msg = sb('msg', [128, 32], F32)
            sqg = sb('sqg', [128, 32], F32)
            rsg = sb('rsg', [128, 32], F32)
            ex4 = expT[:].rearrange("p (t d r) -> p t d r", t=NT, d=2)
            sd3 = sdec[:].rearrange("p (t k) -> p t k", t=NT)
            bf3 = bias_fb[:].rearrange("p (t k) -> p t k", t=NT)
            eo3 = eoff[:].rearrange("p (t k) -> p t k", t=NT)
            dA3 = dA[:].rearrange("p (t k) -> p t k", t=NT)
            for d_ in range(2):
                P.op('dve', (lambda d_: lambda e: e.tensor_copy(Tdec[0:64, :, d_, :], ex4[0:64, :, d_, 0:4]))(d_), reads=['expT'], writes=['Tdec'])
                P.op('dve', (lambda d_: lambda e: e.tensor_copy(Tdec[64:128, :, d_, :], ex4[64:128, :, d_, 4:8]))(d_), reads=['expT'], writes=['Tdec'])
            for r in range(8):
                P.op('dve', (lambda r: lambda e: e.tensor_scalar(DI[:, r, :], ident_f[:], dskip[:, r:r + 1], None, ALU.mult))(r), reads=['ident_f', 'dskip'], writes=['DI'])
            P.op('dve', lambda e: e.memset(Srun[:], 0.0), writes=['Srun'])
            P.op('dve', lambda e: e.memset(Sin[:, 0, 0, :], 0.0), writes=[('Sin', 0, 0)])
            P.op('dve', lambda e: e.memset(Sin[:, 1, NT - 1, :], 0.0), writes=[('Sin', 1, NT - 1)])
            P.op('dve', lambda e: e.memset(ssg[:], 0.0), writes=['ssg'])
            for d_, order in ((0, range(0, NT - 1)), (1, range(NT - 1, 0, -1))):
                for t in order:
                    P.op('dve', (lambda d_, t: lambda e: e.tensor_tensor(Xd[:, d_, :].rearrange("p (r d) -> p r d", r=8), xs_tok[:, t, :].rearrange("p (r d) -> p r d", r=8),
                                                                      sd3[:, t, d_ * 8:(d_ + 1) * 8].unsqueeze(2).to_broadcast([128, 8, 64]), ALU.mult))(d_, t),
                         reads=[('xs_tok', c) for c in range(4)] + [('sdec', d_)], writes=[('Xd', d_)])
                    bk = 6 + d_
                    P.op('pe', (lambda bk, t, d_: lambda e: e.matmul(PS[bk][:, :], B_tok[:, t, :], Xd[:, d_, :], start=True, stop=True))(bk, t, d_),
                         reads=['B_tok', ('Xd', d_)], writes=[psr(bk)])
                    P.op('dve', (lambda d_, t: lambda e: e.tensor_tensor(Stmp[:].rearrange("p (r d) -> p r d", r=4), Srun[:, d_, :].rearrange("p (r d) -> p r d", r=4),
                                                                      Tdec[:, t, d_, :].unsqueeze(2).to_broadcast([128, 4, 64]), ALU.mult))(d_, t),
                         reads=['Srun', 'Tdec'], writes=['Stmp'])
                    P.op('dve', (lambda bk, d_: lambda e: e.tensor_tensor(Srun[0:64, d_, :], Stmp[0:64, :], PS[bk][0:64, 0:256], ALU.add))(bk, d_), reads=['Stmp', psr(bk)], writes=['Srun'])
                    P.op('dve', (lambda bk, d_: lambda e: e.tensor_tensor(Srun[64:128, d_, :], Stmp[64:128, :], PS[bk][64:128, 256:512], ALU.add))(bk, d_), reads=['Stmp', psr(bk)], writes=['Srun'])
                    tn = t + 1 if d_ == 0 else t - 1
                    P.op('act', (lambda d_, tn: lambda e: e.activation(out=Sin[:, d_, tn, :], in_=Srun[:, d_, :], func=AF.Copy))(d_, tn), reads=['Srun'], writes=[('Sin', d_, tn)])
            def stage_xa(t):
                db = t % 2
                P.op('dve', lambda e: e.tensor_copy(dAbc[db][:], dA3[:, t, :].unsqueeze(2).to_broadcast([128, 16, 128])), reads=['dA'], writes=[('dAbc', db)])

            def stage_xd(t, d_):
                db = t % 2
                if d_ == 0:
                    for g in range(2):
                        P.op('pe', (lambda g: lambda e: e.matmul(PS[0][:, g * 128:(g + 1) * 128], BTm[:, g, t * 128:(t + 1) * 128], CTm[:, g, t * 128:(t + 1) * 128], start=True, stop=True))(g),
                             reads=[('BTm', g), ('CTm', g)], writes=[psr(0)])
                    P.op('act', lambda e: e.activation(out=Gsb[db][:], in_=PS[0][:, 0:256], func=AF.Copy), reads=[psr(0)], writes=[('Gsb', db)])
                tri, tkey, ngm, nkey = (tri_ip, 'tri_ip', negm_f, 'negm_f') if d_ == 0 else (tri_is, 'tri_is', negm_b, 'negm_b')
                for r in range(8):
                    bk = 1 + d_ * 2 + r // 4
                    sl = slice((r % 4) * 128, (r % 4 + 1) * 128)
                    P.op('pe', (lambda bk, sl, r, tri: lambda e: e.matmul(PS[bk][:, sl], dAbc[db][:, d_ * 8 + r, :], tri[:], start=True, stop=False))(bk, sl, r, tri),
                         reads=[('dAbc', db), tkey], writes=[psr(bk)])
                    P.op('pe', (lambda bk, sl, ngm: lambda e: e.matmul(PS[bk][:, sl], ident_f[:], ngm[:], start=False, stop=True))(bk, sl, ngm),
                         reads=['ident_f', nkey], writes=[psr(bk)])
                for r in range(8):
                    bk = 1 + d_ * 2 + r // 4
                    sl = slice((r % 4) * 128, (r % 4 + 1) * 128)
                    P.op('act', (lambda bk, sl, r: lambda e: e.activation(out=Lt[db][:, d_ * 8 + r, :], in_=PS[bk][:, sl], func=AF.Exp, bias=bf3[:, t, d_ * 8 + r:d_ * 8 + r + 1]))(bk, sl, r),
                         reads=[psr(bk), ('bias_fb', d_)], writes=[('Lt', db, d_, r)])

            def stage_y1(t):
                db = t % 2
                P.op('dve', lambda e: e.tensor_tensor(Lt[db][:, 0:8, :], Lt[db][:, 0:8, :], Lt[db][:, 8:16, :], ALU.add), reads=[('Lt', db, dd, rr_) for dd in range(2) for rr_ in range(8)], writes=[('Lt', db, 0, rr_) for rr_ in range(8)])
                for g in range(2):
                    P.op('dve', (lambda g: lambda e: e.tensor_tensor(Mt[:, g * 4:(g + 1) * 4, :], Lt[db][:, g * 4:(g + 1) * 4, :], Gsb[db][:, g * 128:(g + 1) * 128].unsqueeze(1).to_broadcast([128, 4, 128]), ALU.mult))(g),
                         reads=[('Lt', db, 0, rr_) for rr_ in range(8)] + [('Gsb', db)], writes=[('Mt', g), ('Xd', 0), ('Xd', 1)])

            def stage_y2(t):
                for r in range(8):
                    P.op('pe', (lambda r: lambda e: e.matmul(PS[5][:, r * 64:(r + 1) * 64], Mt[:, r, :], xs_tok[:, t, r * 64:(r + 1) * 64], start=True, stop=False))(r),
                         reads=[('Mt', r // 4)] + [('xs_tok', c) for c in range(4)], writes=[psr(5)])
                    P.op('pe', (lambda r: lambda e: e.matmul(PS[5][:, r * 64:(r + 1) * 64], DI[:, r, :], xs_tok[:, t, r * 64:(r + 1) * 64], start=False, stop=True))(r),
                         reads=['DI'] + [('xs_tok', c) for c in range(4)], writes=[psr(5)])
                for d_ in range(2):
                    for g in range(2):
                        P.op('pe', (lambda d_, g: lambda e: e.matmul(PS[6 + d_][:, g * 256:(g + 1) * 256], CTm[:, g, t * 128:(t + 1) * 128], Sin[:, d_, t, :], start=True, stop=True))(d_, g),
                             reads=[('CTm', g), ('Sin', d_, t)], writes=[psr(6 + d_)])
                P.op('act', lambda e: e.activation(out=ysb[:], in_=PS[5][:, :], func=AF.Copy), reads=[psr(5)], writes=['ysb'])

            def stage_y3(t):
                for d_ in range(2):
                    P.op('dve', (lambda d_: lambda e: e.tensor_tensor(ytmp[:].rearrange("p (r d) -> p r d", r=8), PS[6 + d_][:, :].rearrange("p (r d) -> p r d", r=8),
                                                                   eo3[:, t, d_ * 8:(d_ + 1) * 8].unsqueeze(2).to_broadcast([128, 8, 64]), ALU.mult))(d_),
                         reads=[psr(6 + d_), ('eoff', d_)], writes=['ytmp'])
                    P.op('dve', lambda e: e.tensor_tensor(ysb[:], ysb[:], ytmp[:], ALU.add), reads=['ysb', 'ytmp'], writes=['ysb'])
                P.op('dve', lambda e: e.tensor_tensor(ysb[:], ysb[:], zs[:, t, :], ALU.mult), reads=['ysb', ('zs', t)], writes=['ysb'])
                for g in range(2):
                    col = t * 2 + g
                    P.op('dve', (lambda g: lambda e: e.tensor_tensor(junkc[:], ysb[:, g * 256:(g + 1) * 256], ysb[:, g * 256:(g + 1) * 256], ALU.mult))(g), reads=['ysb'], writes=['junkc'])
                    P.op('dve', (lambda col: lambda e: e.reduce_sum(ssg[:, col:col + 1], junkc[:], axis=AX.X))(col), reads=['junkc'], writes=[('ssg', col)])
                sl2 = slice(t * 2, t * 2 + 2)
                P.op('dve', lambda e: e.tensor_scalar(msg[:, sl2], ssg[:, sl2], 1.0 / 256, EPS, ALU.mult, ALU.add), reads=[('ssg', t * 2), ('ssg', t * 2 + 1)], writes=[('msg', t)])

            def stage_z1(t):
                sl2 = slice(t * 2, t * 2 + 2)
                P.op('act', lambda e: e.activation(out=sqg[:, sl2], in_=msg[:, sl2], func=AF.Ln), reads=[('msg', t)], writes=[('sqg', t)])
                P.op('act', lambda e: e.activation(out=rsg[:, sl2], in_=sqg[:, sl2], func=AF.Exp, scale=-0.5), reads=[('sqg', t)], writes=[('rsg', t)])
                for g in range(2):
                    col = t * 2 + g
                    P.op('dve', (lambda g, col: lambda e: e.scalar_tensor_tensor(ssdo[:, g * 256:(g + 1) * 256], ysb[:, g * 256:(g + 1) * 256], rsg[:, col:col + 1], ssmg[:, g * 256:(g + 1) * 256], ALU.mult, ALU.mult))(g, col),
                         reads=['ysb', ('rsg', t), 'ssmg'], writes=['ssdo'])

            def stage_z2(t):
                for cc in range(4):
                    P.op('pe', (lambda cc: lambda e: e.matmul(PS[0][:, cc * 128:(cc + 1) * 128], ssdo[:, cc * 128:(cc + 1) * 128], ident_b[:], start=True, stop=True))(cc),
                         reads=['ssdo', 'ident_b'], writes=[psr(0)])
                P.op('dve', lambda e: e.tensor_copy(mixT[:, 4:8, t * 128:(t + 1) * 128], PS[0][:, :].rearrange("p (a d) -> p a d", a=4)),
                     reads=[psr(0)], writes=[('mixT', 4 + c, t) for c in range(4)])

            NTc = NT if stop_after != 'C2a' else 0
            if NTc:
                stage_xa(0)
                stage_xd(0, 0)
                stage_xd(0, 1)
            for i in range(NTc + 1):
                if 1 <= i:
                    stage_z1(i - 1)
                if i + 1 < NTc:
                    stage_xa(i + 1)
                if i < NTc:
                    stage_y1(i)
                if i + 1 < NTc:
                    stage_xd(i + 1, 0)
                if 1 <= i:
                    stage_z2(i - 1)
                if i < NTc:
                    stage_y2(i)
                if i + 1 < NTc:
                    stage_xd(i + 1, 1)
                if i < NTc:
                    stage_y3(i)
            P.emit_phase()
        cst.close()
        if stop_after in ('C', 'C2a'):
            with contextlib.ExitStack() as ph:
                tmp = ph.enter_context(nc.sbuf_tensor('s_dbgtmp', [128, S], F32, side='left'))
                for c in range(8):
                    P.op('dve', (lambda c: lambda e: e.tensor_copy(tmp[:], mixT[:, c, :]))(c), reads=[('mixT', c, t) for t in range(NT)], writes=['dbgtmp'])
                    P.op('sp', (lambda c: lambda e: e.dma_start(out=dbg_d['mixT'][c], in_=tmp[:]))(c), reads=['dbgtmp'], dma='dbg')
                P.emit_phase()
            P.final_wait_all_dma()
            mixer.close()
            bcst.close()
            return nc

        xres = sbT('xres', [128, NT, D], F32)
        h2 = sbT('h2', [128, NT, D], BF16)
        aff = sbT('aff', [128, NT, 16], F32)
        sel = sbT('sel', [128, NT, 16], F32)
        pos = sbT('pos', [128, NT, 16], F32)
        with contextlib.ExitStack() as ph:
            def sb(name, shape, dt_):
                return ph.enter_context(nc.sbuf_tensor('s_' + name, list(shape), dt_, side='left'))
            tmpm = sb('tmpm', [128, 512], F32)
            xn2 = sb('xn2', [128, D], F32)
            h2f = [sb('h2f%d' % i, [128, D], F32) for i in range(2)]
            h2Tf = sb('h2Tf', [128, 8, 128], F32)
            rw = sb('rw', [128, 8, 16], F32)
            ss2 = sb('ss2', [128, 16], F32)
            ms2 = sb('ms2', [128, 16], F32)
            sq2 = sb('sq2', [128, 16], F32)
            rs2 = sb('rs2', [128, 16], F32)
            mx = sb('mx', [128, 16], F32)
            nmx = sb('nmx', [128, 16], F32)
            sme = sb('sme', [128, 16], F32)
            rsm = sb('rsm', [128, 16], F32)
            eaf = sb('eaf', [128, NT, 16], F32)
            P.op('sp', lambda e: e.dma_start(out=rw[:], in_=rw_d), writes=['rw'], dma='cst')
            wo = [wp_next(), wp_next()]
            for hf in range(2):
                load_w(wo[hf], wout_d[:, hf * 512:(hf + 1) * 512], 512)
            P.op('dve', lambda e: e.memset(ss2[:], 0.0), writes=['ss2'])
            P.op('dve', lambda e: e.memset(sme[:], 0.0), writes=['sme'])
            def d1_xpe(t):
                P.op('sp', (lambda t: lambda e: e.dma_start(out=xres[:, t, :], in_=x_d[t * 128:(t + 1) * 128, :]))(t), writes=[('xres', t)], dma=('xres', t))
                for hf in range(2):
                    bk = hf
                    for c in range(8):
                        P.op('pe', (lambda bk, hf, c: lambda e: e.matmul(PS[bk][:, :], mixT[:, c, t * 128:(t + 1) * 128], WP[wo[hf]][:, c, :], start=(c == 0), stop=(c == 7)))(bk, hf, c),
                             reads=[('wp', wo[hf]), ('mixT', c, t)], writes=[psr(bk)])

            def d1_xch(t):
                hb = t % 2
                for hf in range(2):
                    bk = hf
                    P.op('dve', (lambda bk, hf: lambda e: e.tensor_tensor(tmpm[:], PS[bk][:, :], bc3[:, 0, hf * 512:(hf + 1) * 512], ALU.mult))(bk, hf), reads=[psr(bk), ('bc4', 0)], writes=['tmpm'])
                    P.op('dve', (lambda hf: lambda e: e.tensor_tensor(xres[:, t, hf * 512:(hf + 1) * 512], xres[:, t, hf * 512:(hf + 1) * 512], tmpm[:], ALU.add))(hf), reads=['tmpm', ('xres', t)], writes=[('xres', t)])
                P.op('act', lambda e: e.activation(out=xn2[:], in_=xres[:, t, :], func=AF.Square, accum_out=ss2[:, t:t + 1]), reads=[('xres', t), 'ss2'], writes=['xn2', ('ss2', t)])
                P.op('dve', lambda e: e.tensor_scalar(ms2[:, t:t + 1], ss2[:, t:t + 1], 1.0 / D, EPS, ALU.mult, ALU.add), reads=[('ss2', t)], writes=[('ms2', t)])
                P.op('act', lambda e: e.activation(out=sq2[:, t:t + 1], in_=ms2[:, t:t + 1], func=AF.Sqrt), reads=[('ms2', t)], writes=[('sq2', t)])
                P.op('dve', lambda e: e.reciprocal(rs2[:, t:t + 1], sq2[:, t:t + 1]), reads=[('sq2', t)], writes=[('rs2', t)])
                P.op('dve', lambda e: e.scalar_tensor_tensor(xn2[:], xres[:, t, :], rs2[:, t:t + 1], bc3[:, 2, :], ALU.mult, ALU.mult), reads=[('xres', t), ('rs2', t), ('bc4', 2)], writes=['xn2'])
                P.op('dve', lambda e: e.tensor_tensor(h2f[hb][:], xn2[:], bc3[:, 1, :], ALU.add), reads=['xn2', ('bc4', 1)], writes=[('h2f', hb)])
                P.op('act', lambda e: e.activation(out=h2[:, t, :], in_=h2f[hb][:], func=AF.Copy), reads=[('h2f', hb)], writes=[('h2', t)])

            def d1_ytr(t):
                hb = t % 2
                for c in range(8):
                    bk = 2 + c // 4
                    P.op('pe', (lambda bk, c: lambda e: e.transpose(PS[bk][:, (c % 4) * 128:(c % 4 + 1) * 128], h2f[hb][:, c * 128:(c + 1) * 128], ident_f[:]))(bk, c), reads=[('h2f', hb), 'ident_f'], writes=[psr(bk)])
                P.op('act', lambda e: e.activation(out=h2Tf[:, 0:4, :], in_=PS[2][:, :].rearrange("p (a d) -> p a d", a=4), func=AF.Copy), reads=[psr(2)], writes=[('h2Tf', 0)])
                P.op('dve', lambda e: e.tensor_copy(h2Tf[:, 4:8, :], PS[3][:, :].rearrange("p (a d) -> p a d", a=4)), reads=[psr(3)], writes=[('h2Tf', 1)])

            def d1_yrt(t):
                for c in range(8):
                    P.op('pe', (lambda c: lambda e: e.matmul(PS[4][:, t * 16:(t + 1) * 16], h2Tf[:, c, :], rw[:, c, :], start=(c == 0), stop=(c == 7)))(c), reads=[('h2Tf', c // 4), 'rw'], writes=[psr(4)])

            d1_xpe(0)
            d1_xch(0)
            for t in range(NT):
                if t + 1 < NT:
                    d1_xpe(t + 1)
                d1_ytr(t)
                if t + 1 < NT:
                    d1_xch(t + 1)
                d1_yrt(t)
            lg3 = PS[4][:, 0:256].rearrange("p (t k) -> p t k", t=NT)
            P.op('dve', lambda e: e.reduce_max(mx[:], lg3, axis=AX.X), reads=[psr(4)], writes=['mx'])
            P.op('dve', lambda e: e.tensor_scalar(nmx[:], mx[:], -1.0, None, ALU.mult), reads=['mx'], writes=['nmx'])
            for t in range(NT):
                P.op('act', (lambda t: lambda e: e.activation(out=eaf[:, t, :], in_=PS[4][:, t * 16:(t + 1) * 16], func=AF.Exp, bias=nmx[:, t:t + 1], accum_out=sme[:, t:t + 1]))(t),
                     reads=[psr(4), 'nmx', 'sme'], writes=[('eaf', t), ('sme', t)])
            P.op('dve', lambda e: e.reciprocal(rsm[:], sme[:]), reads=[('sme', t) for t in range(NT)], writes=['rsm'])
            P.op('dve', lambda e: e.tensor_tensor(aff[:], eaf[:], rsm[:].unsqueeze(2).to_broadcast([128, NT, 16]), ALU.mult), reads=[('eaf', t) for t in range(NT)] + ['rsm'], writes=['aff'])
            dump('x2', xres[:, :, :], [('xres', t) for t in range(NT)])
            dump('aff', aff[:, :, :], ['aff'])
            P.emit_phase()
        mixer.close()
        bcst.close()

        gsel = sbT('gsel', [128, NT, 16], F32)
        wpx_stack = contextlib.ExitStack()
        NWX = 3
        for i_ in range(NWX):
            WP.append(wpx_stack.enter_context(nc.sbuf_tensor('s_wpx%d' % i_, [128, 8, 512], BF16, side='left')))
        with contextlib.ExitStack() as ph:
            def sb(name, shape, dt_):
                return ph.enter_context(nc.sbuf_tensor('s_' + name, list(shape), dt_, side='left'))
            if stop_after in (None, 'E', 'E1'):
                for fb_ in range(len(WP) // 2):
                    for kind_ in ('g', 'u'):
                        prefetched[(0, kind_, fb_)] = issue_w(0, kind_, fb_)
            affT = sb('affT', [16, S], F32)
            work = sb('work', [16, S], F32)
            m8 = sb('m8', [16, 8], F32)
            csel = sb('csel', [128, NT, 16], F32)
            for t in range(NT):
                bk = t // 4
                P.op('pe', (lambda bk, t: lambda e: e.transpose(PS[bk][0:16, (t % 4) * 128:(t % 4 + 1) * 128], aff[:, t, :], ident_f[:]))(bk, t), reads=['aff', 'ident_f'], writes=[psr(bk)])
            for bk in range(4):
                P.op('dve', (lambda bk: lambda e: e.tensor_copy(affT[:, bk * 512:(bk + 1) * 512], PS[bk][0:16, :]))(bk), reads=[psr(bk)], writes=['affT'])
            P.op('dve', lambda e: e.tensor_copy(work[:], affT[:]), reads=['affT'], writes=['work'])
            for it_ in range(CAP // 8):
                P.op('dve', lambda e: e.max(m8[:], work[:]), reads=['work'], writes=['m8'])
                if it_ < CAP // 8 - 1:
                    P.op('dve', lambda e: e.match_replace(work[:], m8[:], work[:], -1.0), reads=['work', 'm8'], writes=['work'])
            P.op('dve', lambda e: e.tensor_scalar(work[:], affT[:], m8[:, 7:8], None, ALU.is_ge), reads=['affT', 'm8', 'work'], writes=['work'])
            for t in range(NT):
                P.op('pe', (lambda t: lambda e: e.transpose(PS[4][:, t * 16:(t + 1) * 16], work[:, t * 128:(t + 1) * 128], ident_f[0:16, 0:16]))(t), reads=['work', 'ident_f'], writes=[psr(4)])
            P.op('dve', lambda e: e.tensor_copy(sel[:], PS[4][:, 0:256].rearrange("p (t k) -> p t k", t=NT)), reads=[psr(4)], writes=['sel'])
            P.op('dve', lambda e: e.memset(csel[:, 0, :], 0.0), writes=[('csel', 0)])
            for t in range(1, NT):
                P.op('dve', (lambda t: lambda e: e.tensor_tensor(csel[:, t, :], csel[:, t - 1, :], sel[:, t - 1, :], ALU.add))(t), reads=[('csel', t - 1), 'sel'], writes=[('csel', t)])
            for t in range(NT):
                P.op('pe', (lambda t: lambda e: e.matmul(PS[5][:, t * 16:(t + 1) * 16], tri_ep[:], sel[:, t, :], start=True, stop=False))(t), reads=['tri_ep', 'sel'], writes=[psr(5)])
                P.op('pe', (lambda t: lambda e: e.matmul(PS[5][:, t * 16:(t + 1) * 16], ones_f[:], csel[:, t, :], start=False, stop=True))(t), reads=['ones_f', ('csel', t)], writes=[psr(5)])
            P.op('dve', lambda e: e.tensor_copy(pos[:], PS[5][:, 0:256].rearrange("p (t k) -> p t k", t=NT)), reads=[psr(5)], writes=['pos'])
            P.op('dve', lambda e: e.tensor_tensor(gsel[:], aff[:], sel[:], ALU.mult), reads=['aff', 'sel'], writes=['gsel'])
            dump('sel', sel[:, :, :], ['sel'])
            dump('pos', pos[:, :, :], ['pos'])
            P.emit_phase()

        with contextlib.ExitStack() as ph:
            def sb(name, shape, dt_):
                return ph.enter_context(nc.sbuf_tensor('s_' + name, list(shape), dt_, side='left'))
            oh = [sb('oh%d' % i, [128, 256], BF16) for i in range(4)]
            ohg = [sb('ohg%d' % i, [128, 256], BF16) for i in range(4)]
            xgT = [sb('xgT0', [128, 8, 256], BF16)] * 2
            hact = sb('hact', [128, NFC, 256], BF16)
            ohT = [sb('ohT%d' % i, [128, 2, S], BF16) for i in range(2)]
            sg = [sb('sg%d' % i, [128, 256], F32) for i in range(2)]
            ye = sb('ye', [128, 2, D], BF16)
            print('E: sbuf bytes remaining after locals', nc.sbuf_bytes_remaining)
            nblk = [(i * 512, min(512, FF - i * 512)) for i in range(6)]
            ohc = [0]
            NE = NEXP if stop_after != 'E1' else 1

            def gather_units(ex):
                xb = ex % 2
                units = []
                obuf = {}

                def mk_oh(t):
                    ob = ohc[0] % 4
                    ohc[0] += 1
                    obuf[t] = ob
                    P.op('dve', (lambda ob: lambda e: e.tensor_scalar(oh[ob][:], iota_j[:], pos[:, t, ex:ex + 1], sel[:, t, ex:ex + 1], ALU.is_equal, ALU.mult))(ob),
                         reads=['iota_j', 'pos', 'sel'], writes=[('oh', ob)])
                    P.op('dve', (lambda ob: lambda e: e.tensor_scalar(ohg[ob][:], iota_j[:], pos[:, t, ex:ex + 1], gsel[:, t, ex:ex + 1], ALU.is_equal, ALU.mult))(ob),
                         reads=['iota_j', 'pos', 'gsel'], writes=[('ohg', ob)])

                def pre():
                    mk_oh(0)
                    mk_oh(1)
                    for b4_ in range(4):
                        P.op('pe', (lambda b4_: lambda e: e.matmul(PS[b4_][:, :], zeros_b[:, 0:128], zeros_b[:, :], start=True, stop=False))(b4_), reads=['zeros_b'], writes=[psr(b4_)])
                units.append(pre)
                for t in range(NT):
                    def u(t=t):
                        if t + 2 < NT:
                            mk_oh(t + 2)
                        ob = obuf[t]
                        for c in range(8):
                            P.op('pe', (lambda ob, c: lambda e: e.matmul(PS[c // 2][:, (c % 2) * 256:(c % 2 + 1) * 256], h2[:, t, c * 128:(c + 1) * 128], oh[ob][:], start=False, stop=(t == NT - 1 and c % 2 == 1)))(ob, c),
                                 reads=[('h2', t), ('oh', ob)], writes=[psr(c // 2)])
                        for jh in range(2):
                            P.op('pe', (lambda ob, jh: lambda e: e.matmul(PS[4 + jh][:, (t % 4) * 128:(t % 4 + 1) * 128], ohg[ob][:, jh * 128:(jh + 1) * 128], ident_b[:], start=True, stop=True))(ob, jh),
                                 reads=[('ohg', ob), 'ident_b'], writes=[psr(4 + jh)])
                        if t % 4 == 3:
                            t0_ = t - 3
                            P.op('act', (lambda t0_: lambda e: e.activation(out=ohT[xb][:, 0, t0_ * 128:(t0_ + 4) * 128], in_=PS[4][:, :], func=AF.Copy))(t0_),
                                 reads=[psr(4)], writes=[('ohT', xb, 0, t0_ // 4)])
                            P.op('dve', (lambda t0_: lambda e: e.tensor_copy(ohT[xb][:, 1, t0_ * 128:(t0_ + 4) * 128], PS[5][:, :]))(t0_),
                                 reads=[psr(5)], writes=[('ohT', xb, 1, t0_ // 4)])
                    units.append(u)

                def fin():
                    for b4 in range(4):
                        if b4 % 2 == 0:
                            P.op('act', (lambda b4: lambda e: e.activation(out=xgT[xb][:, 2 * b4:2 * b4 + 2, :], in_=PS[b4][:, :].rearrange("p (a d) -> p a d", a=2), func=AF.Copy))(b4), reads=[psr(b4)], writes=[('xgT', 0, b4)])
                        else:
                            P.op('dve', (lambda b4: lambda e: e.tensor_copy(xgT[xb][:, 2 * b4:2 * b4 + 2, :], PS[b4][:, :].rearrange("p (a d) -> p a d", a=2)))(b4), reads=[psr(b4)], writes=[('xgT', 0, b4)])
                units.append(fin)
                return units

            def scatter_units(ex):
                xb = ex % 2
                units = []
                for t in range(NT):
                    for dh in range(2):
                        def u(t=t, dh=dh):
                            bk = 4 + (t * 2 + dh) % 2
                            for jh in range(2):
                                P.op('pe', (lambda bk, jh: lambda e: e.matmul(PS[bk][:, :], ohT[xb][:, jh, t * 128:(t + 1) * 128], ye[:, jh, dh * 512:(dh + 1) * 512], start=(jh == 0), stop=(jh == 1)))(bk, jh),
                                     reads=[('ohT', xb, jh, t // 4), ('ye', jh, dh)], writes=[psr(bk)])
                            P.op('dve', (lambda bk: lambda e: e.tensor_tensor(xres[:, t, dh * 512:(dh + 1) * 512], xres[:, t, dh * 512:(dh + 1) * 512], PS[bk][:, :], ALU.add))(bk),
                                 reads=[psr(bk), ('xres', t)], writes=[('xres', t)])
                        units.append(u)
                return units

            def ffn(ex, fillers):
                xb = ex % 2
                fillers = list(fillers)
                nfc_done = [0]
                nfill_total = [len(fillers)]
                nfill_emitted = [0]
                for fb, (f0, fw) in enumerate(nblk):
                    wgi = get_w(ex, 'g', fb)
                    wui = get_w(ex, 'u', fb)
                    for k in range(fw // 128):
                        fi = fb * 4 + k
                        bk = 6 + fi % 2
                        for c in range(8):
                            P.op('pe', (lambda bk, wgi, c, k: lambda e: e.matmul(PS[bk][:, 0:256], WP[wgi][:, c, k * 128:(k + 1) * 128], xgT[xb][:, c, :], start=(c == 0), stop=(c == 7)))(bk, wgi, c, k),
                                 reads=[('wp', wgi), ('xgT', 0, c // 2)], writes=[psr(bk)])
                        for c in range(8):
                            P.op('pe', (lambda bk, wui, c, k: lambda e: e.matmul(PS[bk][:, 256:512], WP[wui][:, c, k * 128:(k + 1) * 128], xgT[xb][:, c, :], start=(c == 0), stop=(c == 7)))(bk, wui, c, k),
                                 reads=[('wp', wui), ('xgT', 0, c // 2)], writes=[psr(bk)])
                        sgi = fi % 2
                        P.op('act', (lambda bk, sgi: lambda e: e.activation(out=sg[sgi][:], in_=PS[bk][:, 0:256], func=AF.Silu))(bk, sgi), reads=[psr(bk)], writes=[('sg', sgi)])
                        P.op('dve', (lambda bk, sgi, fi: lambda e: e.tensor_tensor(hact[:, fi, :], sg[sgi][:], PS[bk][:, 256:512], ALU.mult))(bk, sgi, fi), reads=[psr(bk), ('sg', sgi)], writes=[('hact', fi)])
                        nfc_done[0] += 1
                        want = (len(fillers) * 0 + nfill_total[0] * nfc_done[0] + NFC - 1) // NFC
                        while nfill_emitted[0] < want and fillers:
                            fillers.pop(0)()
                            nfill_emitted[0] += 1
                for fb in range(6):
                    nk = 4 if fb < 5 else 2
                    wdi = get_w(ex, 'd', fb)
                    for k in range(nk):
                        fi = fb * 4 + k
                        for jh in range(2):
                            for dh in range(2):
                                bk = jh * 2 + dh
                                P.op('pe', (lambda bk, wdi, fi, k, jh, dh: lambda e: e.matmul(PS[bk][:, :], hact[:, fi, jh * 128:(jh + 1) * 128], WP[wdi][:, 2 * k + dh, :], start=(fi == 0), stop=(fi == NFC - 1)))(bk, wdi, fi, k, jh, dh),
                                     reads=[('wp', wdi), ('hact', fi)], writes=[psr(bk)])
                for jh in range(2):
                    for dh in range(2):
                        bk = jh * 2 + dh
                        P.op('dve', (lambda bk, jh, dh: lambda e: e.tensor_tensor(ye[:, jh, dh * 512:(dh + 1) * 512], PS[bk][:, :], g2bc[:, dh * 512:(dh + 1) * 512], ALU.mult))(bk, jh, dh),
                             reads=[psr(bk), ('g2bc' if False else ('bc4', 3))], writes=[('ye', jh, dh)])

            for u in gather_units(0):
                u()
            for ex in range(NE):
                ffn(ex, scatter_units(ex - 1) if ex >= 1 else [])
                if ex + 1 < NE:
                    for u in gather_units(ex + 1):
                        u()
            for u in scatter_units(NE - 1):
                u()
            P.emit_phase()

        del WP[3:]
        wpx_stack.close()
        with contextlib.ExitStack() as ph:
            def sb(name, shape, dt_):
                return ph.enter_context(nc.sbuf_tensor('s_' + name, list(shape), dt_, side='left'))
            gfin = sb('gfin', [128, D], F32)
            ob_ = [sb('ob%d' % i, [128, D], F32) for i in range(2)]
            junkf = sb('junkf', [128, D], F32)
            ssf = sb('ssf', [128, 16], F32)
            msf = sb('msf', [128, 16], F32)
            sqf = sb('sqf', [128, 16], F32)
            rsf = sb('rsf', [128, 16], F32)
            P.op('sp', lambda e: e.dma_start(out=gfin[:], in_=gfin_d), writes=['gfin'], dma='cst')
            P.op('dve', lambda e: e.memset(ssf[:], 0.0), writes=['ssf'])
            for t in range(NT):
                b = t % 2
                P.op('act', (lambda t: lambda e: e.activation(out=junkf[:], in_=xres[:, t, :], func=AF.Square, accum_out=ssf[:, t:t + 1]))(t), reads=[('xres', t), 'ssf'], writes=['junkf', ('ssf', t)])
                P.op('dve', (lambda t: lambda e: e.tensor_scalar(msf[:, t:t + 1], ssf[:, t:t + 1], 1.0 / D, EPS, ALU.mult, ALU.add))(t), reads=[('ssf', t)], writes=[('msf', t)])
                P.op('act', (lambda t: lambda e: e.activation(out=sqf[:, t:t + 1], in_=msf[:, t:t + 1], func=AF.Sqrt))(t), reads=[('msf', t)], writes=[('sqf', t)])
                P.op('dve', (lambda t: lambda e: e.reciprocal(rsf[:, t:t + 1], sqf[:, t:t + 1]))(t), reads=[('sqf', t)], writes=[('rsf', t)])
                P.op('dve', (lambda b, t: lambda e: e.scalar_tensor_tensor(ob_[b][:], xres[:, t, :], rsf[:, t:t + 1], gfin[:], ALU.mult, ALU.mult))(b, t), reads=[('xres', t), ('rsf', t), 'gfin'], writes=[('ob', b)])
                P.op('sp', (lambda b, t: lambda e: e.dma_start(out=out_d[t * 128:(t + 1) * 128, :], in_=ob_[b][:]))(b, t), reads=[('ob', b)], dma=('ob', b))
            P.emit_phase()
        P.final_wait_all_dma()
    return nc


def _t5_bucket_static(rel):
    nb = 16
    ret = np.where(rel > 0, nb, 0)
    n = np.abs(rel)
    max_exact = nb // 2
    nf = np.maximum(n, 1).astype(np.float32)
    large = max_exact + (np.log(nf / max_exact) / math.log(128 / max_exact) * (nb - max_exact)).astype(np.int32)
    large = np.minimum(large, nb - 1)
    return ret + np.where(n < max_exact, n, large)


def _col(v, n):
    return np.ascontiguousarray(np.asarray(v, np.float32).reshape(n, 128).T)


def _rep(v):
    v = np.asarray(v, np.float32).reshape(1, -1)
    return np.ascontiguousarray(np.broadcast_to(v, (128, v.shape[1])))


def prep_inputs(inp):
    f = lambda a: np.ascontiguousarray(np.asarray(a, np.float32))
    sh = {}
    sh['ada_w'] = f(inp['ada_w'][0])
    ada_b = f(inp['ada_b'][0])
    sh['ada_brow'] = np.ascontiguousarray(ada_b.reshape(1, -1))
    sh['ada_bg'] = _rep(ada_b[2048:6144])
    sh['gmixT'] = _col(inp['norm_mix_g'][0], 8)
    sh['gffn_bc'] = _rep(inp['norm_ffn_g'][0])
    sh['gfin_bc'] = _rep(inp['norm_final_g'])
    sh['w_in'] = f(inp['w_in'][0])
    sh['lam_qk'] = _rep(np.concatenate([f(inp['lambda_q1'][0]), f(inp['lambda_k1'][0]), f(inp['lambda_q2'][0]), f(inp['lambda_k2'][0])]))
    sh['subln_bc'] = _rep(inp['attn_subln_g'][0])
    tab = f(inp['rel_bias_table'])
    kl = np.arange(128)[:, None]
    ql = np.arange(128)[None, :]
    blk = np.zeros((128, 3, 4, 128), np.float32)
    for d in (-1, 0, 1):
        bidx = _t5_bucket_static(d * 128 + kl - ql)
        for h in range(4):
            blk[:, d + 1, h, :] = tab[bidx, h]
    sh['biasblk'] = blk
    sh['cfar'] = _rep(np.concatenate([tab[15, :], tab[31, :]]))
    cw = f(inp['conv_w'][0])[:, 0, :]
    sh['conv_wT'] = np.ascontiguousarray(cw.reshape(5, 6, 128).transpose(2, 1, 0))
    sh['conv_bT'] = _col(inp['conv_b'][0], 6)
    dtb = np.concatenate([f(inp['dt_bias_f'][0]), f(inp['dt_bias_b'][0])])
    sh['dtb256'] = _rep(np.tile(dtb, 16))
    alog = np.concatenate([f(inp['A_log_f'][0]), f(inp['A_log_b'][0])])
    sh['alog256'] = _rep(np.tile(alog, 16))
    sh['dskip_bc'] = _rep(inp['D_skip'][0])
    sh['ssmg_bc'] = _rep(inp['ssm_norm_g'][0])
    sh['w_out'] = f(inp['w_out'][0])
    sh['router_wT'] = np.ascontiguousarray(f(inp['router_w'][0]).reshape(8, 128, 16).transpose(1, 0, 2))
    sh['w_gate'] = f(inp['w_gate'][0])
    sh['w_up'] = f(inp['w_up'][0])
    sh['w_down'] = f(inp['w_down'][0])
    x = f(inp['x'])
    c = f(inp['c'])
    maps = []
    for b in range(x.shape[0]):
        m = dict(sh)
        m['x'] = x[b]
        m['c_col'] = _col(c[b], 8)
        maps.append(m)
    return maps


_NC_CACHE = {}


def kernel(**inputs):
    maps = prep_inputs(inputs)
    if 'nc' not in _NC_CACHE:
        _NC_CACHE['nc'] = build()
    nc = _NC_CACHE['nc']
    res = run_bass_kernel_spmd(nc, maps, core_ids=list(range(8)))
    return np.stack([np.asarray(r['out'], np.float32) for r in res.results], axis=0)
```

```python
import contextlib
import math
import numpy as np
import concourse.bass as bass
import concourse.mybir as mybir
from concourse.bass_utils import run_bass_kernel_spmd

F32 = mybir.dt.float32
BF16 = mybir.dt.bfloat16
ALU = mybir.AluOpType
AF = mybir.ActivationFunctionType
AX = mybir.AxisListType

S = 2048
D = 1024
NT = 16
EPS = 1e-6
NEXP = 16
FF = 2816
NFC = 22
CAP = 256
LAM_INIT = 0.8 - 0.6 * math.exp(0.0)


class Prog:
    ENGS = ('pe', 'act', 'dve', 'pool', 'sp')

    def __init__(self, nc, esems, dma_sems):
        self.nc = nc
        self.esem = esems
        self.ecnt = {e: 0 for e in self.ENGS}
        self.dsem = {}
        self.free_dsems = list(dma_sems)
        self.ops = []
        self.lastw = {}
        self.readers = {}
        self.known = {e: {} for e in self.ENGS}

    def _dma_token(self, key):
        if key not in self.dsem:
            self.dsem[key] = [self.free_dsems.pop(), 0]
        ent = self.dsem[key]
        ent[1] += 16
        return ('d', key, ent[1])

    def op(self, eng, fn, reads=(), writes=(), dma=None):
        idx = len(self.ops)
        deps = set()
        for r in reads:
            w = self.lastw.get(r)
            if w is not None:
                deps.add(w)
            if isinstance(r, tuple) and r[0] == 'ps':
                for t in self.readers.get(r, ()):
                    if t[0] == 'c' and self.ops[t[1]]['eng'] != eng:
                        deps.add(t)
        for r in writes:
            w = self.lastw.get(r)
            if w is not None:
                deps.add(w)
            for t in self.readers.get(r, ()):
                if t[0] == 'c' and dma is None and self.ops[t[1]]['eng'] == eng and eng == 'pe':
                    continue
                deps.add(t)
        tok = self._dma_token(dma) if dma is not None else ('c', idx)
        fdeps = set()
        for t in deps:
            if t[0] == 'c' and dma is None and eng == 'pe' and self.ops[t[1]]['eng'] == 'pe':
                continue
            fdeps.add(t)
        self.ops.append(dict(eng=eng, fn=fn, deps=fdeps, dma=dma, tok=tok))
        for r in reads:
            self.readers.setdefault(r, []).append(tok)
        for r in writes:
            self.lastw[r] = tok
            self.readers[r] = []
        return tok

    def emit_phase(self):
        self.phase_no = getattr(self, 'phase_no', 0) + 1
        with self.nc.named_scope('ph%d' % self.phase_no):
            self._emit_phase()

    def _emit_phase(self):
        nc = self.nc
        ops = self.ops
        sig = set()
        for o in ops:
            for t in o['deps']:
                if t[0] == 'c':
                    sig.add(t[1])
        cnt = {}
        for i, o in enumerate(ops):
            if i in sig:
                self.ecnt[o['eng']] += 1
                cnt[i] = self.ecnt[o['eng']]
        per = {e: [] for e in self.ENGS}
        for i, o in enumerate(ops):
            per[o['eng']].append(i)

        def run(eng_name, engobj):
            kn = self.known[eng_name]
            for i in per[eng_name]:
                o = ops[i]
                need = {}
                for t in o['deps']:
                    if t[0] == 'c':
                        key = ('e', ops[t[1]]['eng'])
                        val = cnt[t[1]]
                    else:
                        key = ('d', t[1])
                        val = t[2]
                        if t[1] in ('cst', 'cstB'):
                            val = self.dsem[t[1]][1]
                    if kn.get(key, 0) >= val:
                        continue
                    need[key] = max(need.get(key, 0), val)
                for key, val in need.items():
                    sem = self.esem[key[1]] if key[0] == 'e' else self.dsem[key[1]][0]
                    engobj.wait_ge(sem, val)
                    kn[key] = val
                ins = o['fn'](engobj)
                if o['dma'] is not None:
                    ins.then_inc(self.dsem[o['dma']][0], 16)
                elif i in sig:
                    ins.then_inc(self.esem[o['eng']], 1)

        with nc.Block() as block:
            if per['pe']:
                @block.tensor
                def _(e):
                    run('pe', e)
            if per['act']:
                @block.scalar
                def _(e):
                    run('act', e)
            if per['dve']:
                @block.vector
                def _(e):
                    run('dve', e)
            if per['pool']:
                @block.gpsimd
                def _(e):
                    run('pool', e)
            if per['sp']:
                @block.sync
                def _(e):
                    run('sp', e)
        for r in list(self.lastw.keys()):
            if self.lastw[r] is not None and self.lastw[r][0] == 'c':
                self.lastw[r] = None
        for r in list(self.readers.keys()):
            self.readers[r] = [t for t in self.readers[r] if t[0] == 'd']
        self.ops = []

    def final_wait_all_dma(self):
        nc = self.nc
        with nc.Block() as block:
            @block.sync
            def _(e):
                for key, (sem, val) in self.dsem.items():
                    if val > 0:
                        e.wait_ge(sem, val)


def build(stop_after=None, dbg=None):
    dbg = dbg or {}
    nc = bass.Bass("TRN2", target_bir_lowering=False)

    def din(name, shape):
        return nc.dram_tensor(name, list(shape), F32, kind="ExternalInput").ap()

    x_d = din("x", [S, D])
    ccol_d = din("c_col", [128, 8])
    adaw_d = din("ada_w", [D, 6 * D])
    adabrow_d = din("ada_brow", [1, 6 * D])
    adabg_d = din("ada_bg", [128, 4096])
    gmixT_d = din("gmixT", [128, 8])
    gffn_d = din("gffn_bc", [128, D])
    gfin_d = din("gfin_bc", [128, D])
    win_d = din("w_in", [D, 2832])
    lamqk_d = din("lam_qk", [128, 256])
    subln_d = din("subln_bc", [128, 128])
    biasblk_d = din("biasblk", [128, 3, 4, 128])
    cfar_d = din("cfar", [128, 8])
    convw_d = din("conv_wT", [128, 6, 5])
    convb_d = din("conv_bT", [128, 6])
    dtb_d = din("dtb256", [128, 256])
    alog_d = din("alog256", [128, 256])
    dskip_d = din("dskip_bc", [128, 8])
    ssmg_d = din("ssmg_bc", [128, 512])
    wout_d = din("w_out", [D, D])
    rw_d = din("router_wT", [128, 8, 16])
    if stop_after in (None, 'E', 'E1'):
        wg_d = din("w_gate", [NEXP, D, FF])
        wu_d = din("w_up", [NEXP, D, FF])
        wd_d = din("w_down", [NEXP, FF, D])
    out_d = nc.dram_tensor("out", [S, D], F32, kind="ExternalOutput").ap()
    dbg_d = {k: nc.dram_tensor("dbg_" + k, list(shp), F32, kind="ExternalOutput").ap() for k, shp in dbg.items()}

    with contextlib.ExitStack() as top:
        def sbT(name, shape, dt, side='right'):
            return top.enter_context(nc.sbuf_tensor('s_' + name, list(shape), dt, side=side))

        esems = {e: top.enter_context(nc.semaphore('es_' + e)) for e in ('pe', 'act', 'dve', 'pool')}
        dsems = [top.enter_context(nc.semaphore('ds%d' % i)) for i in range(48)]
        P = Prog(nc, esems, dsems)
        PS = [top.enter_context(nc.psum_tensor('psb%d' % i, [128, 512], F32)) for i in range(8)]

        def psr(b):
            return ('ps', b)

        ident_f = sbT('ident_f', [128, 128], F32)
        ident_b = sbT('ident_b', [128, 128], BF16)
        ones_f = sbT('ones_f', [128, 128], F32)
        tri_ip = sbT('tri_ip', [128, 128], F32)
        tri_es = sbT('tri_es', [128, 128], F32)
        tri_is = sbT('tri_is', [128, 128], F32)
        tri_ep = sbT('tri_ep', [128, 128], F32)
        negm_f = sbT('negm_f', [128, 128], F32)
        negm_b = sbT('negm_b', [128, 128], F32)
        iota_j = sbT('iota_j', [128, 256], F32)
        iota_p = sbT('iota_p', [128, 2], F32)
        modT = sbT('modT', [128, 48], F32)
        a1 = sbT('a1', [128, 8], F32)
        g2bc = sbT('g2bc', [128, D], F32)
        WP = [sbT('wp%d' % i, [128, 8, 512], BF16) for i in range(3)]
        wp_ctr = [0]

        def wp_next():
            i = wp_ctr[0] % len(WP)
            wp_ctr[0] += 1
            return i

        def load_w(i, src_ap, ncols, nk=8):
            P.op('pool', lambda e: e.dma_start(out=WP[i][:, 0:nk, 0:ncols],
                                               in_=src_ap.rearrange("(c p) n -> p c n", p=128)),
                 writes=[('wp', i)], dma=('wp', i))


        NBLK = [(i * 512, min(512, FF - i * 512)) for i in range(6)]
        prefetched = {}

        def issue_w(ex, kind, fb):
            i = wp_next()
            if kind in ('g', 'u'):
                f0, fw = NBLK[fb]
                src = (wg_d if kind == 'g' else wu_d)[ex, :, f0:f0 + fw]
                load_w(i, src, fw)
            else:
                nk = 4 if fb < 5 else 2
                P.op('pool', lambda e: e.dma_start(out=WP[i][:, 0:2 * nk, :].rearrange("p (k h) n -> p k h n", h=2),
                                                   in_=wd_d[ex, fb * 512:fb * 512 + nk * 128, :].rearrange("(k p) (h n) -> p k h n", p=128, h=2)),
                     writes=[('wp', i)], dma=('wp', i))
            return i

        def get_w(ex, kind, fb):
            key = (ex, kind, fb)
            if key in prefetched:
                return prefetched.pop(key)
            return issue_w(ex, kind, fb)

        def dump(key, ap, reads):
            if key in dbg_d:
                P.op('sp', lambda e: e.dma_start(out=dbg_d[key], in_=ap), reads=reads, dma='dbg')

        P.op('pool', lambda e: e.memset(ident_f[:], 0.0), writes=['ident_f'])
        P.op('pool', lambda e: e.affine_select(ident_f[:], ident_f[:], [[-1, 128]], ALU.not_equal, 1.0, base=0, channel_multiplier=1),
             reads=['ident_f'], writes=['ident_f'])
        P.op('pool', lambda e: e.tensor_copy(ident_b[:], ident_f[:]), reads=['ident_f'], writes=['ident_b'])
        P.op('pool', lambda e: e.memset(ones_f[:], 1.0), writes=['ones_f'])
        for tl, key, cmp_ in ((tri_ip, 'tri_ip', ALU.is_ge), (tri_ep, 'tri_ep', ALU.is_gt)):
            P.op('pool', (lambda tl, cmp_: lambda e: e.affine_select(tl[:], ones_f[:], [[1, 128]], cmp_, 0.0, base=0, channel_multiplier=-1))(tl, cmp_),
                 reads=['ones_f'], writes=[key])
        for tl, key, cmp_ in ((tri_is, 'tri_is', ALU.is_ge), (tri_es, 'tri_es', ALU.is_gt)):
            P.op('pool', (lambda tl, cmp_: lambda e: e.affine_select(tl[:], ones_f[:], [[-1, 128]], cmp_, 0.0, base=0, channel_multiplier=1))(tl, cmp_),
                 reads=['ones_f'], writes=[key])
        zeros_f = sbT('zeros_f', [128, 128], F32)
        zeros_b = sbT('zeros_b', [128, 512], BF16)
        P.op('pool', lambda e: e.memset(zeros_b[:], 0.0), writes=['zeros_b'])
        P.op('pool', lambda e: e.memset(zeros_f[:], 0.0), writes=['zeros_f'])
        P.op('pool', lambda e: e.affine_select(negm_f[:], zeros_f[:], [[1, 128]], ALU.is_ge, -30000.0, base=0, channel_multiplier=-1),
             reads=['zeros_f'], writes=['negm_f'])
        P.op('pool', lambda e: e.affine_select(negm_b[:], zeros_f[:], [[-1, 128]], ALU.is_ge, -30000.0, base=0, channel_multiplier=1),
             reads=['zeros_f'], writes=['negm_b'])
        P.op('pool', lambda e: e.iota(iota_j[:], [[1, 256]], base=0, channel_multiplier=0, allow_small_or_imprecise_dtypes=True), writes=['iota_j'])
        P.op('pool', lambda e: e.iota(iota_p[:], [[128, 2]], base=0, channel_multiplier=1, allow_small_or_imprecise_dtypes=True), writes=['iota_p'])

        bcst = contextlib.ExitStack()
        bc3 = bcst.enter_context(nc.sbuf_tensor('s_bc3', [128, 3, D], F32, side='left'))

        def bcrow(i):
            return bc3[:, i, :] if i < 3 else g2bc[:, :]

        mixer = contextlib.ExitStack()
        mixT = mixer.enter_context(nc.sbuf_tensor('mixT', [128, 8, S], BF16, side='left'))
        hstack = contextlib.ExitStack()
        hT = hstack.enter_context(nc.sbuf_tensor('hT', [128, 8, S], BF16, side='left'))

        with contextlib.ExitStack() as ph:
            def sb(name, shape, dt):
                return ph.enter_context(nc.sbuf_tensor('s_' + name, list(shape), dt, side='left'))
            adaw = [sb('adaw%d' % i, [128, 8, 512], F32) for i in range(2)]
            xt = [sb('xt%d' % i, [128, D], F32) for i in range(2)]
            xn = [sb('xn%d' % i, [128, D], F32) for i in range(2)]
            ccol = sb('ccol', [128, 8], F32)
            scv = sb('scv', [128, 8], F32)
            scb = sb('scb', [128, 8, 128], F32)
            abr = [sb('abr%d' % i, [1, 512], F32) for i in range(2)]
            rowt = [sb('rowt%d' % i, [1, 512], F32) for i in range(2)]
            abg = sb('abg', [128, 4096], F32)
            gmixT = sb('gmixT', [128, 8], F32)
            gffn = sb('gffn', [128, D], F32)
            ss = sb('ss', [128, 16], F32)
            ms = sb('ms', [128, 16], F32)
            sq = sb('sq', [128, 16], F32)
            rstd = sb('rstd', [128, 16], F32)

            P.op('sp', lambda e: e.dma_start(out=ccol[:], in_=ccol_d), writes=['ccol'], dma='cst')
            P.op('sp', lambda e: e.dma_start(out=gmixT[:], in_=gmixT_d), writes=['gmixT'], dma='cst')
            P.op('sp', lambda e: e.dma_start(out=abg[:], in_=adabg_d), writes=['abg'], dma='cst')
            P.op('sp', lambda e: e.dma_start(out=gffn[:], in_=gffn_d), writes=['gffn'], dma='cst')
            P.op('act', lambda e: e.activation(out=scv[:], in_=ccol[:], func=AF.Silu), reads=['ccol'], writes=['scv'])
            P.op('dve', lambda e: e.tensor_copy(scb[:], scv[:].unsqueeze(2).to_broadcast([128, 8, 128])), reads=['scv'], writes=['scb'])
            adaw_v = adaw_d.rearrange("(c p) n -> p c n", p=128)
            gi_ctr = [0]

            def ada_block(blk):
                ab = blk % 2
                gi = gi_ctr[0]
                bk = 1 + ab
                P.op('sp', (lambda ab, blk: lambda e: e.dma_start(out=adaw[ab][:], in_=adaw_v[:, :, blk * 512:(blk + 1) * 512]))(ab, blk),
                     writes=[('adaw', ab)], dma=('adaw', ab))
                P.op('sp', (lambda ab, blk: lambda e: e.dma_start(out=abr[ab][:], in_=adabrow_d[:, blk * 512:(blk + 1) * 512]))(ab, blk),
                     writes=[('abr', ab)], dma=('abr', ab))
                for c in range(8):
                    P.op('pe', (lambda ab, bk, c: lambda e: e.matmul(PS[bk][:, :], scb[:, c, :], adaw[ab][:, c, :], start=(c == 0), stop=(c == 7)))(ab, bk, c),
                         reads=[('adaw', ab), 'scb'], writes=[psr(bk)])
                P.op('act', (lambda ab, bk, blk: lambda e: e.activation(out=rowt[ab][0:1, :], in_=PS[bk][0:1, :], func=AF.Copy))(ab, bk, blk), reads=[psr(bk)], writes=[('rowt', ab)])
                P.op('dve', (lambda ab, blk: lambda e: e.tensor_tensor(rowt[ab][0:1, :], rowt[ab][0:1, :], abr[ab][0:1, :], ALU.add))(ab, blk), reads=[('rowt', ab), ('abr', ab)], writes=[('rowt', ab)])
                if blk >= 4:
                    P.op('dve', (lambda bk, gi: lambda e: e.tensor_tensor(bcrow(gi // 2)[:, (gi % 2) * 512:(gi % 2 + 1) * 512], PS[bk][:, :], abg[:, gi * 512:(gi + 1) * 512], ALU.add))(bk, gi),
                         reads=[psr(bk), 'abg'], writes=[('bc4', gi // 2)])
                    gi_ctr[0] += 1
                for jj in range(4):
                    j = blk * 4 + jj
                    P.op('pe', (lambda ab, jj, j: lambda e: e.transpose(PS[0][:, j:j + 1], rowt[ab][0:1, jj * 128:(jj + 1) * 128], ident_f[0:1, 0:1]))(ab, jj, j),
                         reads=[('rowt', ab), 'ident_f'], writes=[psr(0)])
            for blk in range(4):
                ada_block(blk)
            P.op('dve', lambda e: e.tensor_copy(modT[:, 0:16], PS[0][:, 0:16]), reads=[psr(0)], writes=[('modT', 0)])
            P.op('dve', lambda e: e.scalar_tensor_tensor(a1[:], modT[:, 8:16], 1.0, gmixT[:], ALU.add, ALU.mult), reads=[('modT', 0), 'gmixT'], writes=['a1'])
            P.op('dve', lambda e: e.memset(ss[:], 0.0), writes=['ss'])
            def norm_tile(t):
                b = t % 2
                P.op('sp', (lambda b, t: lambda e: e.dma_start(out=xt[b][:], in_=x_d[t * 128:(t + 1) * 128, :]))(b, t), writes=[('xt', b)], dma=('xt', b))
                P.op('act', (lambda b, t: lambda e: e.activation(out=xn[b][:], in_=xt[b][:], func=AF.Square, accum_out=ss[:, t:t + 1]))(b, t),
                     reads=[('xt', b), 'ss'], writes=[('xn', b), ('ss', t)])
                P.op('dve', (lambda t: lambda e: e.tensor_scalar(ms[:, t:t + 1], ss[:, t:t + 1], 1.0 / D, EPS, ALU.mult, ALU.add))(t), reads=[('ss', t)], writes=[('ms', t)])
                P.op('act', (lambda t: lambda e: e.activation(out=sq[:, t:t + 1], in_=ms[:, t:t + 1], func=AF.Sqrt))(t), reads=[('ms', t)], writes=[('sq', t)])
                P.op('dve', (lambda t: lambda e: e.reciprocal(rstd[:, t:t + 1], sq[:, t:t + 1]))(t), reads=[('sq', t)], writes=[('rstd', t)])
                P.op('dve', (lambda b, t: lambda e: e.tensor_scalar(xn[b][:], xt[b][:], rstd[:, t:t + 1], None, ALU.mult))(b, t),
                     reads=[('xt', b), ('rstd', t)], writes=[('xn', b)])
                for c in range(8):
                    bk = 3 + 2 * b + c // 4
                    P.op('pe', (lambda b, bk, c: lambda e: e.transpose(PS[bk][:, (c % 4) * 128:(c % 4 + 1) * 128], xn[b][:, c * 128:(c + 1) * 128], ident_f[:]))(b, bk, c),
                         reads=[('xn', b), 'ident_f'], writes=[psr(bk)])
                for c in range(8):
                    bk = 3 + 2 * b + c // 4
                    if c // 4 == 0:
                        P.op('act', (lambda bk, c, t: lambda e: e.activation(out=hT[:, c, t * 128:(t + 1) * 128], in_=PS[bk][:, (c % 4) * 128:(c % 4 + 1) * 128], func=AF.Identity, scale=a1[:, c:c + 1], bias=modT[:, c:c + 1]))(bk, c, t),
                             reads=[psr(bk), 'a1', ('modT', 0)], writes=[('hT', c, t)])
                    else:
                        P.op('dve', (lambda bk, c, t: lambda e: e.tensor_scalar(hT[:, c, t * 128:(t + 1) * 128], PS[bk][:, (c % 4) * 128:(c % 4 + 1) * 128], a1[:, c:c + 1], modT[:, c:c + 1], ALU.mult, ALU.add))(bk, c, t),
                             reads=[psr(bk), 'a1', ('modT', 0)], writes=[('hT', c, t)])
            for i_ in range(8):
                ada_block(4 + i_)
                norm_tile(2 * i_)
                norm_tile(2 * i_ + 1)
            P.op('dve', lambda e: e.tensor_copy(modT[:, 16:48], PS[0][:, 16:48]), reads=[psr(0)], writes=[('modT', 1)])
            P.op('dve', lambda e: e.scalar_tensor_tensor(bc3[:, 2, :], bc3[:, 2, :], 1.0, gffn[:], ALU.add, ALU.mult), reads=[('bc4', 2), 'gffn'], writes=[('bc4', 2)])
            dump('modT', modT[:], [('modT', 0), ('modT', 1)])
            P.emit_phase()

        def hT_reads(c, Q):
            return [('hT', c, 4 * Q + i) for i in range(4)]

        if stop_after == 'A':
            with contextlib.ExitStack() as ph:
                tmp = ph.enter_context(nc.sbuf_tensor('dbgtmp', [128, S], F32, side='left'))
                for c in range(8):
                    P.op('dve', (lambda c: lambda e: e.tensor_copy(tmp[:], hT[:, c, :]))(c), reads=[('hT', c, t) for t in range(NT)], writes=['dbgtmp'])
                    P.op('sp', (lambda c: lambda e: e.dma_start(out=dbg_d['hT'][c], in_=tmp[:]))(c), reads=['dbgtmp'], dma='dbg')
                P.emit_phase()
            P.final_wait_all_dma()
            hstack.close()
            mixer.close()
            bcst.close()
            return nc

        with contextlib.ExitStack() as ph:
            def sb(name, shape, dt):
                return ph.enter_context(nc.sbuf_tensor('s_' + name, list(shape), dt, side='left'))
            qT = sb('qT', [128, 2, 4, S], BF16)
            kT = sb('kT', [128, 4, S], BF16)
            vaug = sb('vaug', [128, NT, 4, 130], BF16)
            biasb = sb('biasb', [128, 3, 4, 128], BF16)
            cfar = sb('cfar', [128, 8], F32)
            lamqk = sb('lamqk', [128, 256], F32)
            lprod = sb('lprod', [128, 256], F32)
            lsum = sb('lsum', [128, 4], F32)
            lamneg = sb('lamneg', [128, 1], F32)
            g08 = sb('g08', [128, 128], F32)
            PT = [sb('PT%d' % i, [128, 512], BF16) for i in range(4)]
            accsb = [sb('accsb%d' % i, [128, 8, 129], F32) for i in range(2)]
            rr = [sb('rr%d' % i, [128, 16], F32) for i in range(2)]
            t0b = [sb('t0b%d' % i, [128, 128], F32) for i in range(2)]
            attb = [sb('attb%d' % i, [128, 128], F32) for i in range(4)]
            attn = [sb('attn%d' % i, [128, 128], F32) for i in range(4)]
            junk = sb('junkb', [128, 128], F32)
            ssa = sb('ssa', [128, 64], F32)
            msa = sb('msa', [128, 64], F32)
            sqa = sb('sqa', [128, 64], F32)
            rsa = sb('rsa', [128, 64], F32)

            P.op('pool', lambda e: e.dma_start(out=biasb[:], in_=biasblk_d), writes=['biasb'], dma='cstB')
            P.op('sp', lambda e: e.dma_start(out=cfar[:], in_=cfar_d), writes=['cfar'], dma='cst')
            P.op('sp', lambda e: e.dma_start(out=lamqk[:], in_=lamqk_d), writes=['lamqk'], dma='cst')
            P.op('sp', lambda e: e.dma_start(out=g08[:], in_=subln_d), writes=['g08'], dma='cst')
            P.op('dve', lambda e: e.tensor_tensor(lprod[:, 0:64], lamqk[:, 0:64], lamqk[:, 64:128], ALU.mult), reads=['lamqk'], writes=['lprod'])
            P.op('dve', lambda e: e.tensor_tensor(lprod[:, 64:128], lamqk[:, 128:192], lamqk[:, 192:256], ALU.mult), reads=['lamqk', 'lprod'], writes=['lprod'])
            P.op('dve', lambda e: e.reduce_sum(lsum[:, 0:1], lprod[:, 0:64], axis=AX.X), reads=['lprod'], writes=['lsum'])
            P.op('dve', lambda e: e.reduce_sum(lsum[:, 1:2], lprod[:, 64:128], axis=AX.X), reads=['lprod', 'lsum'], writes=['lsum'])
            P.op('act', lambda e: e.activation(out=lsum[:, 2:4], in_=lsum[:, 0:2], func=AF.Exp), reads=['lsum'], writes=['lsum'])
            P.op('dve', lambda e: e.tensor_tensor(lamneg[:], lsum[:, 3:4], lsum[:, 2:3], ALU.subtract), reads=['lsum'], writes=['lamneg'])
            P.op('dve', lambda e: e.tensor_scalar(lamneg[:], lamneg[:], -LAM_INIT, None, ALU.add), reads=['lamneg'], writes=['lamneg'])
            P.op('dve', lambda e: e.tensor_scalar(g08[:], g08[:], 1.0 - LAM_INIT, None, ALU.mult), reads=['g08'], writes=['g08'])
            P.op('dve', lambda e: e.memset(vaug[:, :, :, 128:130], 1.0), writes=['vones'])
            P.op('dve', lambda e: e.memset(ssa[:], 0.0), writes=[('ssa', c) for c in range(64)])

            wq, wk, wv = wp_next(), wp_next(), wp_next()
            load_w(wq, win_d[:, 0:512], 512)
            load_w(wk, win_d[:, 512:1024], 512)
            load_w(wv, win_d[:, 1024:1536], 512)
            ev = [0]
            P.op('pool', lambda e: e.memset(qT[64:128, 0, :, :], 0.0), writes=['qTz0'])
            P.op('pool', lambda e: e.memset(qT[0:64, 1, :, :], 0.0), writes=['qTz1'])
            def proj_unit(dname, wi, h, Q, bk, eng):
                for c in range(8):
                    P.op('pe', (lambda c: lambda e: e.matmul(PS[bk][:, :], WP[wi][:, c, h * 128:(h + 1) * 128], hT[:, c, Q * 512:(Q + 1) * 512], start=(c == 0), stop=(c == 7)))(c),
                         reads=[('wp', wi)] + hT_reads(c, Q), writes=[psr(bk)])
                if dname == 'kT':
                    if eng == 'act':
                        P.op('act', lambda e: e.activation(out=kT[:, h, Q * 512:(Q + 1) * 512], in_=PS[bk][:, :], func=AF.Copy), reads=[psr(bk)], writes=[('kT', h, Q)])
                    else:
                        P.op('dve', lambda e: e.tensor_copy(kT[:, h, Q * 512:(Q + 1) * 512], PS[bk][:, :]), reads=[psr(bk)], writes=[('kT', h, Q)])
                else:
                    for m in range(2):
                        ps_ = slice(m * 64, (m + 1) * 64)
                        if eng == 'act':
                            P.op('act', (lambda m, ps_: lambda e: e.activation(out=qT[ps_, m, h, Q * 512:(Q + 1) * 512], in_=PS[bk][ps_, :], func=AF.Copy, scale=0.125))(m, ps_),
                                 reads=[psr(bk), 'qTz0', 'qTz1'], writes=[('qT', h, Q, m)])
                        else:
                            P.op('dve', (lambda m, ps_: lambda e: e.tensor_scalar(qT[ps_, m, h, Q * 512:(Q + 1) * 512], PS[bk][ps_, :], 0.125, None, ALU.mult))(m, ps_),
                                 reads=[psr(bk), 'qTz0', 'qTz1'], writes=[('qT', h, Q, m)])

            for t in range(NT):
                bk = ev[0] % 2
                for c in range(8):
                    P.op('pe', (lambda bk, c, t: lambda e: e.matmul(PS[bk][:, :], hT[:, c, t * 128:(t + 1) * 128], WP[wv][:, c, :], start=(c == 0), stop=(c == 7)))(bk, c, t),
                         reads=[('wp', wv), ('hT', c, t)], writes=[psr(bk)])
                eng = 'act' if ev[0] % 2 == 0 else 'dve'
                if eng == 'act':
                    P.op('act', (lambda bk, t: lambda e: e.activation(out=vaug[:, t, :, 0:128], in_=PS[bk][:, :].rearrange("p (h d) -> p h d", h=4), func=AF.Copy))(bk, t),
                         reads=[psr(bk)], writes=[('v', t)])
                else:
                    P.op('dve', (lambda bk, t: lambda e: e.tensor_copy(vaug[:, t, :, 0:128], PS[bk][:, :].rearrange("p (h d) -> p h d", h=4)))(bk, t),
                         reads=[psr(bk)], writes=[('v', t)])
                ev[0] += 1

            for Q in range(4):
                for dname, wi in (('qT', wq), ('kT', wk)):
                    proj_unit(dname, wi, 0, Q, ev[0] % 2, 'act' if ev[0] % 2 == 0 else 'dve')
                    ev[0] += 1
            pending_proj = {h_: [(dname, wi, h_, Q) for Q in range(4) for dname, wi in (('qT', wq), ('kT', wk))] for h_ in range(1, 4)}

            ACCB = (2, 3, 4)
            steps = [(h, Q, j, m) for h in range(4) for Q in range(4) for j in range(NT) for m in range(2)]
            LA = 3
            SBK = (1, 5, 6, 7)

            def cats_of(Q, j):
                cats = []
                for qi in range(4):
                    d = j - (4 * Q + qi)
                    cats.append('n' if abs(d) <= 1 else ('lo' if d < 0 else 'hi'))
                return cats

            def emit_qk(idx):
                h, Q, j, m = steps[idx]
                sbk = SBK[idx % 4]
                cats = cats_of(Q, j)
                nnear = sum(1 for cc in cats if cc == 'n')
                P.op('pe', (lambda sbk, m, h, j, Q, nnear: lambda e: e.matmul(PS[sbk][:, :], kT[:, h, j * 128:(j + 1) * 128], qT[:, m, h, Q * 512:(Q + 1) * 512], start=True, stop=(nnear == 0)))(sbk, m, h, j, Q, nnear),
                     reads=[('kT', h, j // 4), ('qT', h, Q, m), 'qTz0', 'qTz1'], writes=[psr(sbk)])
                k = 0
                for qi in range(4):
                    if cats[qi] == 'n':
                        k += 1
                        d = j - (4 * Q + qi)
                        P.op('pe', (lambda sbk, qi, d, h, last: lambda e: e.matmul(PS[sbk][:, qi * 128:(qi + 1) * 128], ident_b[:], biasb[:, d + 1, h, :], start=False, stop=last))(sbk, qi, d, h, k == nnear),
                             reads=['biasb', 'ident_b'], writes=[psr(sbk)])

            def emit_exp_pv(idx):
                h, Q, j, m = steps[idx]
                sbk = SBK[idx % 4]
                pb = idx % 4
                cats = cats_of(Q, j)
                r0 = 0
                ri = 0
                while r0 < 4:
                    r1 = r0
                    while r1 < 4 and cats[r1] == cats[r0]:
                        r1 += 1
                    cat = cats[r0]
                    if cat == 'n':
                        P.op('act', (lambda pb, sbk, r0, r1: lambda e: e.activation(out=PT[pb][:, r0 * 128:r1 * 128], in_=PS[sbk][:, r0 * 128:r1 * 128], func=AF.Exp))(pb, sbk, r0, r1),
                             reads=[psr(sbk)], writes=[('PT', pb, ri)])
                    else:
                        ci = h if cat == 'lo' else 4 + h
                        P.op('act', (lambda pb, sbk, r0, r1, ci: lambda e: e.activation(out=PT[pb][:, r0 * 128:r1 * 128], in_=PS[sbk][:, r0 * 128:r1 * 128], func=AF.Exp, bias=cfar[:, ci:ci + 1]))(pb, sbk, r0, r1, ci),
                             reads=[psr(sbk), 'cfar'], writes=[('PT', pb, ri)])
                    r0 = r1
                    ri += 1
                if j == 0 and m == 0:
                    for bi_, bkk_ in enumerate(ACCB):
                        n_ = 3 if bi_ < 2 else 2
                        P.op('pe', (lambda bkk_, n_: lambda e: e.matmul(PS[bkk_][:, 0:n_ * 129], zeros_b[:, 0:128], zeros_b[:, 0:n_ * 129], start=True, stop=False))(bkk_, n_),
                             reads=['zeros_b'], writes=[psr(bkk_)])
                for qi in range(4):
                    a = m * 4 + qi
                    ab_, ao = ACCB[a // 3], (a % 3) * 129
                    P.op('pe', (lambda ab_, ao, pb, qi, j, h, a: lambda e: e.matmul(PS[ab_][:, ao:ao + 129], PT[pb][:, qi * 128:(qi + 1) * 128], vaug[:, j, h, 0:129], start=False, stop=(j == NT - 1 and (a % 3 == 2 or a == 7))))(ab_, ao, pb, qi, j, h, a),
                         reads=[('PT', pb, 0), ('PT', pb, 1), ('PT', pb, 2), ('v', j), 'vones'], writes=[psr(ab_)])
                if j == NT - 1 and m == 1:
                    epilogue(h, Q, (h * 4 + Q))

            deferred = {}
            cur_idx = [0]

            def defer(at, fn):
                deferred.setdefault(at, []).append(fn)

            def epilogue(h, Q, it):
                ai = it % 2
                cur = cur_idx[0]
                accr = [('accsb', ai, bi) for bi in range(3)]
                for bi, bkk in enumerate(ACCB):
                    n = 3 if bi < 2 else 2
                    P.op('dve', (lambda ai, bi, bkk, n: lambda e: e.tensor_copy(accsb[ai][:, bi * 3:bi * 3 + n, :], PS[bkk][:, 0:n * 129].rearrange("p (a d) -> p a d", a=n)))(ai, bi, bkk, n),
                         reads=[psr(bkk)], writes=[('accsb', ai, bi)])
                P.op('dve', (lambda ai: lambda e: e.reciprocal(rr[ai][:, 0:8], accsb[ai][:, :, 128]))(ai), reads=accr, writes=[('rr', ai)])
                P.op('dve', (lambda ai: lambda e: e.tensor_scalar(rr[ai][:, 8:12], rr[ai][:, 4:8], lamneg[:, 0:1], None, ALU.mult))(ai), reads=[('rr', ai), 'lamneg'], writes=[('rr', ai)])
                for qi in range(4):
                    col = it * 4 + qi
                    P.op('dve', (lambda ai, qi: lambda e: e.tensor_scalar(t0b[qi % 2][:], accsb[ai][:, qi, 0:128], rr[ai][:, qi:qi + 1], None, ALU.mult))(ai, qi),
                         reads=accr + [('rr', ai)], writes=[('t0b', qi % 2)])
                    P.op('dve', (lambda ai, qi: lambda e: e.scalar_tensor_tensor(attb[qi][:], accsb[ai][:, 4 + qi, 0:128], rr[ai][:, 8 + qi:9 + qi], t0b[qi % 2][:], ALU.mult, ALU.add))(ai, qi),
                         reads=accr + [('rr', ai), ('t0b', qi % 2)], writes=[('attb', qi)])
                    P.op('dve', (lambda qi: lambda e: e.tensor_tensor(junk[:], attb[qi][:], attb[qi][:], ALU.mult))(qi), reads=[('attb', qi)], writes=['junkb'])
                    P.op('dve', (lambda col: lambda e: e.reduce_sum(ssa[:, col:col + 1], junk[:], axis=AX.X))(col), reads=['junkb'], writes=[('ssa', col)])
                sl = slice(it * 4, it * 4 + 4)
                P.op('dve', (lambda sl: lambda e: e.tensor_scalar(msa[:, sl], ssa[:, sl], 1.0 / 128, EPS, ALU.mult, ALU.add))(sl), reads=[('ssa', it * 4 + q_) for q_ in range(4)], writes=[('msa', it)])

                def st2():
                    P.op('act', (lambda sl: lambda e: e.activation(out=sqa[:, sl], in_=msa[:, sl], func=AF.Ln))(sl), reads=[('msa', it)], writes=[('sqa', it)])
                    P.op('act', (lambda sl: lambda e: e.activation(out=rsa[:, sl], in_=sqa[:, sl], func=AF.Exp, scale=-0.5))(sl), reads=[('sqa', it)], writes=[('rsa', it)])

                def st3():
                    for qi in range(4):
                        col = it * 4 + qi
                        P.op('dve', (lambda qi, col: lambda e: e.scalar_tensor_tensor(attn[qi][:], attb[qi][:], rsa[:, col:col + 1], g08[:], ALU.mult, ALU.mult))(qi, col),
                             reads=[('attb', qi), ('rsa', it), 'g08'], writes=[('attn', qi)])

                def st4():
                    tb = 0
                    for qi in range(4):
                        P.op('pe', (lambda tb, qi: lambda e: e.transpose(PS[tb][:, qi * 128:(qi + 1) * 128], attn[qi][:], ident_f[:]))(tb, qi),
                             reads=[('attn', qi), 'ident_f'], writes=[psr(tb)])

                def st5():
                    tb = 0
                    P.op('dve', (lambda tb, h, Q: lambda e: e.tensor_copy(mixT[:, h, Q * 512:(Q + 1) * 512], PS[tb][:, :]))(tb, h, Q),
                         reads=[psr(tb)], writes=[('mixT', h, 4 * Q + q_) for q_ in range(4)])
                defer(cur + 14, st2)
                defer(cur + 18, st3)
                defer(cur + 22, st4)
                defer(cur + 26, st5)

            for idx in range(len(steps) + LA + 32):
                cur_idx[0] = idx
                if idx < len(steps):
                    emit_qk(idx)
                    h_cur = steps[idx][0]
                    if (idx % 128) % 16 == 8 and h_cur + 1 < 4 and pending_proj[h_cur + 1]:
                        proj_unit(*pending_proj[h_cur + 1].pop(0), 0, 'dve')
                if LA <= idx < len(steps) + LA:
                    emit_exp_pv(idx - LA)
                for fn in deferred.pop(idx, []):
                    fn()
            assert not deferred
            P.emit_phase()

        if stop_after == 'B':
            with contextlib.ExitStack() as ph:
                tmp = ph.enter_context(nc.sbuf_tensor('dbgtmp', [128, S], F32, side='left'))
                for c in range(4):
                    P.op('dve', (lambda c: lambda e: e.tensor_copy(tmp[:], mixT[:, c, :]))(c), reads=[('mixT', c, t) for t in range(NT)], writes=['dbgtmp'])
                    P.op('sp', (lambda c: lambda e: e.dma_start(out=dbg_d['mixT'][c], in_=tmp[:]))(c), reads=['dbgtmp'], dma='dbg')
                P.emit_phase()
            P.final_wait_all_dma()
            hstack.close()
            mixer.close()
            bcst.close()
            return nc

        cst = contextlib.ExitStack()

        def sbC(name, shape, dt):
            return cst.enter_context(nc.sbuf_tensor('s_' + name, list(shape), dt, side='right'))
        BTm = sbC('BTm', [128, 2, S], BF16)
        CTm = sbC('CTm', [128, 2, S], BF16)
        xs_tok = sbC('xs_tok', [128, NT, 512], BF16)
        B_tok = sbC('B_tok', [128, NT, 128], BF16)
        zs = sbC('zs', [128, NT, 512], BF16)
        dt = sbC('dt', [128, 256], F32)
        lndt = sbC('lndt', [128, 256], F32)
        dA = sbC('dA', [128, 256], F32)
        csall = sbC('csall', [128, 512], F32)
        expT = sbC('expT', [128, 256], F32)
        bias_fb = sbC('bias_fb', [128, 256], F32)
        sdec = sbC('sdec', [128, 256], F32)
        eoff = sbC('eoff', [128, 256], F32)
        dskip = sbC('dskip', [128, 8], F32)
        ssmg = sbC('ssmg', [128, 512], F32)

        def v3(ap, n):
            return ap.rearrange("p (t k) -> p t k", t=NT)

        with contextlib.ExitStack() as ph:
            def sb(name, shape, dt_):
                return ph.enter_context(nc.sbuf_tensor('s_' + name, list(shape), dt_, side='left'))
            raw2 = [sb('raw%d' % i, [128, S + 4], F32) for i in range(2)]
            cv = sb('cv', [128, S], F32)
            scr4 = sb('scr4', [128, 1024], F32)
            xsT = [scr4[:, :].bitcast(BF16)] * 2
            convw = sb('convw', [128, 6, 5], F32)
            convb = sb('convb', [128, 6], F32)
            dtb = sb('dtb', [128, 256], F32)
            alog = sb('alog', [128, 256], F32)
            dtx = scr4[:, 0:256]
            tA = scr4[:, 256:512]
            tB = scr4[:, 512:768]
            tC = scr4[:, 768:1024]
            csx = cv[:, 0:512]

            for dst, src, key in ((convw, convw_d, 'convw'), (convb, convb_d, 'convb'), (dtb, dtb_d, 'dtb'), (alog, alog_d, 'alog'),
                                  (dskip, dskip_d, 'dskip'), (ssmg, ssmg_d, 'ssmg')):
                P.op('sp', (lambda dst, src: lambda e: e.dma_start(out=dst[:], in_=src))(dst, src), writes=[key], dma='cst')
            wz, wx, wb5 = wp_next(), wp_next(), wp_next()
            load_w(wz, win_d[:, 1536:2048], 512)
            load_w(wx, win_d[:, 2048:2560], 512)
            load_w(wb5, win_d[:, 2560:2832], 272)
            for rb_ in range(2):
                P.op('dve', (lambda rb_: lambda e: e.memset(raw2[rb_][:, 0:2], 0.0))(rb_), writes=[('rawpadL', rb_)])
                P.op('dve', (lambda rb_: lambda e: e.memset(raw2[rb_][:, S + 2:S + 4], 0.0))(rb_), writes=[('rawpadR', rb_)])
            ev = [0]
            for t in range(NT):
                bk = ev[0] % 2
                ev[0] += 1
                for c in range(8):
                    P.op('pe', (lambda bk, c, t: lambda e: e.matmul(PS[bk][:, :], hT[:, c, t * 128:(t + 1) * 128], WP[wz][:, c, :], start=(c == 0), stop=(c == 7)))(bk, c, t),
                         reads=[('wp', wz), ('hT', c, t)], writes=[psr(bk)])
                P.op('act', (lambda bk, t: lambda e: e.activation(out=zs[:, t, :], in_=PS[bk][:, :], func=AF.Silu))(bk, t), reads=[psr(bk)], writes=[('zs', t)])
            for t in range(NT):
                for c in range(8):
                    P.op('pe', (lambda c, t: lambda e: e.matmul(PS[2][:, t * 16:(t + 1) * 16], hT[:, c, t * 128:(t + 1) * 128], WP[wb5][:, c, 256:272], start=(c == 0), stop=(c == 7)))(c, t),
                         reads=[('wp', wb5), ('hT', c, t)], writes=[psr(2)])
            P.op('dve', lambda e: e.tensor_tensor(dtx[:], PS[2][:, 0:256], dtb[:], ALU.add), reads=[psr(2), 'dtb'], writes=['dtx'])
            P.op('act', lambda e: e.activation(out=tA[:], in_=dtx[:], func=AF.Abs), reads=['dtx'], writes=['tA'])
            P.op('act', lambda e: e.activation(out=tB[:], in_=tA[:], func=AF.Exp, scale=-1.0), reads=['tA'], writes=['tB'])
            P.op('act', lambda e: e.activation(out=tC[:], in_=tB[:], func=AF.Ln, bias=1.0), reads=['tB'], writes=['tC'])
            P.op('dve', lambda e: e.scalar_tensor_tensor(dt[:], dtx[:], 0.0, tC[:], ALU.max, ALU.add), reads=['dtx', 'tC'], writes=['dt'])
            P.op('act', lambda e: e.activation(out=lndt[:], in_=dt[:], func=AF.Ln), reads=['dt'], writes=['lndt'])
            P.op('act', lambda e: e.activation(out=tA[:], in_=alog[:], func=AF.Exp), reads=['alog', 'tA'], writes=['tA2'])
            P.op('dve', lambda e: e.scalar_tensor_tensor(dA[:], tA[:], -1.0, dt[:], ALU.mult, ALU.mult), reads=['tA2', 'dt'], writes=['dA'])
            dA3 = dA[:].rearrange("p (t k) -> p t k", t=NT)
            for t in range(NT):
                for kind, (tri, key, off) in enumerate(((tri_ip, 'tri_ip', 0), (tri_es, 'tri_es', 0), (tri_is, 'tri_is', 8), (tri_ep, 'tri_ep', 8))):
                    P.op('pe', (lambda t, kind, tri, off: lambda e: e.matmul(PS[3][:, t * 32 + kind * 8:t * 32 + kind * 8 + 8], tri[:], dA3[:, t, off:off + 8], start=True, stop=True))(t, kind, tri, off),
                         reads=['dA', key], writes=[psr(3)])
                P.op('pe', (lambda t: lambda e: e.matmul(PS[4][:, t * 16:(t + 1) * 16], ones_f[:], dA3[:, t, :], start=True, stop=True))(t),
                     reads=['dA', 'ones_f'], writes=[psr(4)])
            P.op('dve', lambda e: e.tensor_copy(csall[:], PS[3][:, :]), reads=[psr(3)], writes=['csall'])
            P.op('act', lambda e: e.activation(out=expT[:], in_=PS[4][:, 0:256], func=AF.Exp), reads=[psr(4)], writes=['expT'])
            cs4 = csall[:].rearrange("p (t k r) -> p t k r", t=NT, k=4)
            dt4 = dt[:].rearrange("p (t d r) -> p t d r", t=NT, d=2)
            ln4 = lndt[:].rearrange("p (t d r) -> p t d r", t=NT, d=2)
            bf4 = bias_fb[:].rearrange("p (t d r) -> p t d r", t=NT, d=2)
            sd4 = sdec[:].rearrange("p (t d r) -> p t d r", t=NT, d=2)
            eo4 = eoff[:].rearrange("p (t d r) -> p t d r", t=NT, d=2)
            cx4 = csx[:].rearrange("p (t k r) -> p t k r", t=NT, k=4)
            P.op('act', lambda e: e.activation(out=csx[:], in_=csall[:], func=AF.Exp), reads=['csall'], writes=['csx'])
            for d_, kcs, kst in ((0, 0, 1), (1, 2, 3)):
                P.op('dve', (lambda d_, kcs: lambda e: e.tensor_tensor(bf4[:, :, d_, :], ln4[:, :, d_, :], cs4[:, :, kcs, :], ALU.subtract))(d_, kcs), reads=['lndt', 'csall'], writes=[('bias_fb', d_)])
                P.op('dve', (lambda d_, kst: lambda e: e.tensor_tensor(sd4[:, :, d_, :], cx4[:, :, kst, :], dt4[:, :, d_, :], ALU.mult))(d_, kst), reads=['csx', 'dt'], writes=[('sdec', d_)])
                P.op('dve', (lambda d_, kcs: lambda e: e.tensor_copy(eo4[:, :, d_, :], cx4[:, :, kcs, :]))(d_, kcs), reads=['csx'], writes=[('eoff', d_)])
            def xbc_p(cc):
                raw = raw2[cc % 2]
                rkey = ('raw', cc % 2)
                wi = wx if cc < 4 else wb5
                off = (cc % 4) * 128 if cc < 4 else (cc - 4) * 128
                for Q in range(4):
                    bk = ev[0] % 2
                    ev[0] += 1
                    for c in range(8):
                        P.op('pe', (lambda bk, wi, off, c, Q: lambda e: e.matmul(PS[bk][:, :], WP[wi][:, c, off:off + 128], hT[:, c, Q * 512:(Q + 1) * 512], start=(c == 0), stop=(c == 7)))(bk, wi, off, c, Q),
                             reads=[('wp', wi)] + hT_reads(c, Q), writes=[psr(bk)])
                    P.op('act', (lambda bk, Q, raw: lambda e: e.activation(out=raw[:, 2 + Q * 512:2 + (Q + 1) * 512], in_=PS[bk][:, :], func=AF.Copy))(bk, Q, raw), reads=[psr(bk)], writes=[rkey])

            def xbc_c(cc):
                raw = raw2[cc % 2]
                rkey = ('raw', cc % 2)
                wi = wx if cc < 4 else wb5
                off = (cc % 4) * 128 if cc < 4 else (cc - 4) * 128
                P.op('dve', (lambda cc, raw: lambda e: e.tensor_scalar(cv[:], raw[:, 0:S], convw[:, cc, 0:1], None, ALU.mult))(cc, raw), reads=[rkey, ('rawpadL', cc % 2), ('rawpadR', cc % 2), 'convw'], writes=['cv', 'csx'])
                for j in range(1, 5):
                    P.op('dve', (lambda cc, j, raw: lambda e: e.scalar_tensor_tensor(cv[:], raw[:, j:j + S], convw[:, cc, j:j + 1], cv[:], ALU.mult, ALU.add))(cc, j, raw), reads=[rkey, 'cv', 'convw'], writes=['cv'])

            def xbc_s(cc):
                raw = raw2[cc % 2]
                rkey = ('raw', cc % 2)
                wi = wx if cc < 4 else wb5
                off = (cc % 4) * 128 if cc < 4 else (cc - 4) * 128
                if cc < 4:
                    xb = cc % 2
                    P.op('act', (lambda xb, cc: lambda e: e.activation(out=xsT[xb][:], in_=cv[:], func=AF.Silu, bias=convb[:, cc:cc + 1]))(xb, cc), reads=['cv', 'convb'], writes=[('xsT', 0), 'dtx', 'tA', 'tB', 'tC', 'tA2'])
                    for t in range(NT):
                        P.op('pe', (lambda xb, t, cc: lambda e: e.matmul(PS[5 + (t // 4) % 2][:, (t % 4) * 128:(t % 4 + 1) * 128], xsT[xb][:, t * 128:(t + 1) * 128], ident_b[:], start=True, stop=True))(xb, t, cc),
                             reads=[('xsT', 0), 'ident_b'], writes=[psr(5 + (t // 4) % 2)])
                        if t % 4 == 3:
                            t0_ = t - 3
                            P.op('dve', (lambda t0_, t, cc: lambda e: e.tensor_copy(xs_tok[:, t0_:t0_ + 4, cc * 128:(cc + 1) * 128], PS[5 + (t // 4) % 2][:, :].rearrange("p (a d) -> p a d", a=4)))(t0_, t, cc),
                                 reads=[psr(5 + (t // 4) % 2)], writes=[('xs_tok', cc)])
                else:
                    tgt, tkey = (BTm, 'BTm') if cc == 4 else (CTm, 'CTm')
                    P.op('act', (lambda cc, tgt: lambda e: e.activation(out=tgt[:, 0, :], in_=cv[:], func=AF.Silu, bias=convb[:, cc:cc + 1]))(cc, tgt), reads=['cv', 'convb'], writes=[(tkey, 0)])
                    if cc == 4:
                        for t in range(NT):
                            P.op('pe', (lambda t: lambda e: e.matmul(PS[5 + (t // 4) % 2][:, (t % 4) * 128:(t % 4 + 1) * 128], BTm[:, 0, t * 128:(t + 1) * 128], ident_b[:], start=True, stop=True))(t),
                                 reads=[('BTm', 0), 'ident_b'], writes=[psr(5 + (t // 4) % 2)])
                            if t % 4 == 3:
                                t0_ = t - 3
                                P.op('dve', (lambda t0_, t: lambda e: e.tensor_copy(B_tok[:, t0_:t0_ + 4, :], PS[5 + (t // 4) % 2][:, :].rearrange("p (a d) -> p a d", a=4)))(t0_, t),
                                     reads=[psr(5 + (t // 4) % 2)], writes=['B_tok'])
                    P.op('dve', (lambda tgt: lambda e: e.tensor_copy(tgt[64:128, 1, :], tgt[64:128, 0, :]))(tgt), reads=[(tkey, 0)], writes=[(tkey, 1)])
                    P.op('dve', (lambda tgt: lambda e: e.memset(tgt[0:64, 1, :], 0.0))(tgt), reads=[], writes=[(tkey, 1)])
                    P.op('dve', (lambda tgt: lambda e: e.memset(tgt[64:128, 0, :], 0.0))(tgt), reads=[], writes=[(tkey, 0)])

            xbc_p(0)
            for cc in range(6):
                if cc + 1 < 6:
                    xbc_p(cc + 1)
                xbc_c(cc)
                xbc_s(cc)
            P.emit_phase()
        hstack.close()
        if stop_after == 'C1':
            for nm, tl in (('dt', dt), ('dA', dA), ('csall', csall), ('bias_fb', bias_fb), ('eoff', eoff), ('sdec', sdec), ('expT', expT)):
                dump(nm, tl[:], [])
            P.emit_phase()
            P.final_wait_all_dma()
            cst.close()
            mixer.close()
            bcst.close()
            return nc

        with contextlib.ExitStack() as ph:
            def sb(name, shape, dt_):
                return ph.enter_context(nc.sbuf_tensor('s_' + name, list(shape), dt_, side='left'))
            Sin = sb('Sin', [128, 2, NT, 256], BF16)
            Srun = sb('Srun', [128, 2, 256], F32)
            Stmp = sb('Stmp', [128, 256], F32)
            Tdec = sb('Tdec', [128, NT, 2, 4], F32)
            Xd = sb('Xd', [128, 2, 512], BF16)
            DI = sb('DI', [128, 8, 128], BF16)
            dAbc = [sb('dAbc%d' % i, [128, 16, 128], F32) for i in range(2)]
            Lt = [sb('Lt%d' % i, [128, 16, 128], F32) for i in range(2)]
            Gsb = [sb('Gsb%d' % i, [128, 256], F32) for i in range(2)]
            Mt = Xd[:, :, :].rearrange("p a (b d) -> p (a b) d", b=4)
            ysb = sb('ysb', [128, 512], F32)
            ytmp = sb('ytmp', [128, 512], F32)
            junkc = sb('junkc', [128, 256], F32)
            ssdo = sb('ssdo', [128, 512], BF16)
            ssg = sb('ssg', [128, 32], F32)
            msg = sb('msg', [128, 32], F32)
            sqg = sb('sqg', [128, 32], F32)
            rsg = sb('rsg', [128, 32], F32)
            ex4 = expT[:].rearrange("p (t d r) -> p t d r", t=NT, d=2)
            sd3 = sdec[:].rearrange("p (t k) -> p t k", t=NT)
            bf3 = bias_fb[:].rearrange("p (t k) -> p t k", t=NT)
            eo3 = eoff[:].rearrange("p (t k) -> p t k", t=NT)
            dA3 = dA[:].rearrange("p (t k) -> p t k", t=NT)
            for d_ in range(2):
                P.op('dve', (lambda d_: lambda e: e.tensor_copy(Tdec[0:64, :, d_, :], ex4[0:64, :, d_, 0:4]))(d_), reads=['expT'], writes=['Tdec'])
                P.op('dve', (lambda d_: lambda e: e.tensor_copy(Tdec[64:128, :, d_, :], ex4[64:128, :, d_, 4:8]))(d_), reads=['expT'], writes=['Tdec'])
            for r in range(8):
                P.op('dve', (lambda r: lambda e: e.tensor_scalar(DI[:, r, :], ident_f[:], dskip[:, r:r + 1], None, ALU.mult))(r), reads=['ident_f', 'dskip'], writes=['DI'])
            P.op('dve', lambda e: e.memset(Srun[:], 0.0), writes=['Srun'])
            P.op('dve', lambda e: e.memset(Sin[:, 0, 0, :], 0.0), writes=[('Sin', 0, 0)])
            P.op('dve', lambda e: e.memset(Sin[:, 1, NT - 1, :], 0.0), writes=[('Sin', 1, NT - 1)])
            P.op('dve', lambda e: e.memset(ssg[:], 0.0), writes=['ssg'])
            for d_, order in ((0, range(0, NT - 1)), (1, range(NT - 1, 0, -1))):
                for t in order:
                    P.op('dve', (lambda d_, t: lambda e: e.tensor_tensor(Xd[:, d_, :].rearrange("p (r d) -> p r d", r=8), xs_tok[:, t, :].rearrange("p (r d) -> p r d", r=8),
                                                                      sd3[:, t, d_ * 8:(d_ + 1) * 8].unsqueeze(2).to_broadcast([128, 8, 64]), ALU.mult))(d_, t),
                         reads=[('xs_tok', c) for c in range(4)] + [('sdec', d_)], writes=[('Xd', d_)])
                    bk = 6 + d_
                    P.op('pe', (lambda bk, t, d_: lambda e: e.matmul(PS[bk][:, :], B_tok[:, t, :], Xd[:, d_, :], start=True, stop=True))(bk, t, d_),
                         reads=['B_tok', ('Xd', d_)], writes=[psr(bk)])
                    P.op('dve', (lambda d_, t: lambda e: e.tensor_tensor(Stmp[:].rearrange("p (r d) -> p r d", r=4), Srun[:, d_, :].rearrange("p (r d) -> p r d", r=4),
                                                                      Tdec[:, t, d_, :].unsqueeze(2).to_broadcast([128, 4, 64]), ALU.mult))(d_, t),
                         reads=['Srun', 'Tdec'], writes=['Stmp'])
                    P.op('dve', (lambda bk, d_: lambda e: e.tensor_tensor(Srun[0:64, d_, :], Stmp[0:64, :], PS[bk][0:64, 0:256], ALU.add))(bk, d_), reads=['Stmp', psr(bk)], writes=['Srun'])
                    P.op('dve', (lambda bk, d_: lambda e: e.tensor_tensor(Srun[64:128, d_, :], Stmp[64:128, :], PS[bk][64:128, 256:512], ALU.add))(bk, d_), reads=['Stmp', psr(bk)], writes=['Srun'])
                    tn = t + 1 if d_ == 0 else t - 1
                    P.op('act', (lambda d_, tn: lambda e: e.activation(out=Sin[:, d_, tn, :], in_=Srun[:, d_, :], func=AF.Copy))(d_, tn), reads=['Srun'], writes=[('Sin', d_, tn)])
            def stage_xa(t):
                db = t % 2
                P.op('dve', lambda e: e.tensor_copy(dAbc[db][:], dA3[:, t, :].unsqueeze(2).to_broadcast([128, 16, 128])), reads=['dA'], writes=[('dAbc', db)])

            def stage_xd(t, d_):
                db = t % 2
                if d_ == 0:
                    for g in range(2):
                        P.op('pe', (lambda g: lambda e: e.matmul(PS[0][:, g * 128:(g + 1) * 128], BTm[:, g, t * 128:(t + 1) * 128], CTm[:, g, t * 128:(t + 1) * 128], start=True, stop=True))(g),
                             reads=[('BTm', g), ('CTm', g)], writes=[psr(0)])
                    P.op('act', lambda e: e.activation(out=Gsb[db][:], in_=PS[0][:, 0:256], func=AF.Copy), reads=[psr(0)], writes=[('Gsb', db)])
                tri, tkey, ngm, nkey = (tri_ip, 'tri_ip', negm_f, 'negm_f') if d_ == 0 else (tri_is, 'tri_is', negm_b, 'negm_b')
                for r in range(8):
                    bk = 1 + d_ * 2 + r // 4
                    sl = slice((r % 4) * 128, (r % 4 + 1) * 128)
                    P.op('pe', (lambda bk, sl, r, tri: lambda e: e.matmul(PS[bk][:, sl], dAbc[db][:, d_ * 8 + r, :], tri[:], start=True, stop=False))(bk, sl, r, tri),
                         reads=[('dAbc', db), tkey], writes=[psr(bk)])
                    P.op('pe', (lambda bk, sl, ngm: lambda e: e.matmul(PS[bk][:, sl], ident_f[:], ngm[:], start=False, stop=True))(bk, sl, ngm),
                         reads=['ident_f', nkey], writes=[psr(bk)])
                for r in range(8):
                    bk = 1 + d_ * 2 + r // 4
                    sl = slice((r % 4) * 128, (r % 4 + 1) * 128)
                    P.op('act', (lambda bk, sl, r: lambda e: e.activation(out=Lt[db][:, d_ * 8 + r, :], in_=PS[bk][:, sl], func=AF.Exp, bias=bf3[:, t, d_ * 8 + r:d_ * 8 + r + 1]))(bk, sl, r),
                         reads=[psr(bk), ('bias_fb', d_)], writes=[('Lt', db, d_, r)])

            def stage_y1(t):
                db = t % 2
                P.op('dve', lambda e: e.tensor_tensor(Lt[db][:, 0:8, :], Lt[db][:, 0:8, :], Lt[db][:, 8:16, :], ALU.add), reads=[('Lt', db, dd, rr_) for dd in range(2) for rr_ in range(8)], writes=[('Lt', db, 0, rr_) for rr_ in range(8)])
                for g in range(2):
                    P.op('dve', (lambda g: lambda e: e.tensor_tensor(Mt[:, g * 4:(g + 1) * 4, :], Lt[db][:, g * 4:(g + 1) * 4, :], Gsb[db][:, g * 128:(g + 1) * 128].unsqueeze(1).to_broadcast([128, 4, 128]), ALU.mult))(g),
                         reads=[('Lt', db, 0, rr_) for rr_ in range(8)] + [('Gsb', db)], writes=[('Mt', g), ('Xd', 0), ('Xd', 1)])

            def stage_y2(t):
                for r in range(8):
                    P.op('pe', (lambda r: lambda e: e.matmul(PS[5][:, r * 64:(r + 1) * 64], Mt[:, r, :], xs_tok[:, t, r * 64:(r + 1) * 64], start=True, stop=False))(r),
                         reads=[('Mt', r // 4)] + [('xs_tok', c) for c in range(4)], writes=[psr(5)])
                    P.op('pe', (lambda r: lambda e: e.matmul(PS[5][:, r * 64:(r + 1) * 64], DI[:, r, :], xs_tok[:, t, r * 64:(r + 1) * 64], start=False, stop=True))(r),
                         reads=['DI'] + [('xs_tok', c) for c in range(4)], writes=[psr(5)])
                for d_ in range(2):
                    for g in range(2):
                        P.op('pe', (lambda d_, g: lambda e: e.matmul(PS[6 + d_][:, g * 256:(g + 1) * 256], CTm[:, g, t * 128:(t + 1) * 128], Sin[:, d_, t, :], start=True, stop=True))(d_, g),
                             reads=[('CTm', g), ('Sin', d_, t)], writes=[psr(6 + d_)])
                P.op('act', lambda e: e.activation(out=ysb[:], in_=PS[5][:, :], func=AF.Copy), reads=[psr(5)], writes=['ysb'])

            def stage_y3(t):
                for d_ in range(2):
                    P.op('dve', (lambda d_: lambda e: e.tensor_tensor(ytmp[:].rearrange("p (r d) -> p r d", r=8), PS[6 + d_][:, :].rearrange("p (r d) -> p r d", r=8),
                                                                   eo3[:, t, d_ * 8:(d_ + 1) * 8].unsqueeze(2).to_broadcast([128, 8, 64]), ALU.mult))(d_),
                         reads=[psr(6 + d_), ('eoff', d_)], writes=['ytmp'])
                    P.op('dve', lambda e: e.tensor_tensor(ysb[:], ysb[:], ytmp[:], ALU.add), reads=['ysb', 'ytmp'], writes=['ysb'])
                P.op('dve', lambda e: e.tensor_tensor(ysb[:], ysb[:], zs[:, t, :], ALU.mult), reads=['ysb', ('zs', t)], writes=['ysb'])
                for g in range(2):
                    col = t * 2 + g
                    P.op('dve', (lambda g: lambda e: e.tensor_tensor(junkc[:], ysb[:, g * 256:(g + 1) * 256], ysb[:, g * 256:(g + 1) * 256], ALU.mult))(g), reads=['ysb'], writes=['junkc'])
                    P.op('dve', (lambda col: lambda e: e.reduce_sum(ssg[:, col:col + 1], junkc[:], axis=AX.X))(col), reads=['junkc'], writes=[('ssg', col)])
                sl2 = slice(t * 2, t * 2 + 2)
                P.op('dve', lambda e: e.tensor_scalar(msg[:, sl2], ssg[:, sl2], 1.0 / 256, EPS, ALU.mult, ALU.add), reads=[('ssg', t * 2), ('ssg', t * 2 + 1)], writes=[('msg', t)])

            def stage_z1(t):
                sl2 = slice(t * 2, t * 2 + 2)
                P.op('act', lambda e: e.activation(out=sqg[:, sl2], in_=msg[:, sl2], func=AF.Ln), reads=[('msg', t)], writes=[('sqg', t)])
                P.op('act', lambda e: e.activation(out=rsg[:, sl2], in_=sqg[:, sl2], func=AF.Exp, scale=-0.5), reads=[('sqg', t)], writes=[('rsg', t)])
                for g in range(2):
                    col = t * 2 + g
                    P.op('dve', (lambda g, col: lambda e: e.scalar_tensor_tensor(ssdo[:, g * 256:(g + 1) * 256], ysb[:, g * 256:(g + 1) * 256], rsg[:, col:col + 1], ssmg[:, g * 256:(g + 1) * 256], ALU.mult, ALU.mult))(g, col),
                         reads=['ysb', ('rsg', t), 'ssmg'], writes=['ssdo'])

            def stage_z2(t):
                for cc in range(4):
                    P.op('pe', (lambda cc: lambda e: e.matmul(PS[0][:, cc * 128:(cc + 1) * 128], ssdo[:, cc * 128:(cc + 1) * 128], ident_b[:], start=True, stop=True))(cc),
                         reads=['ssdo', 'ident_b'], writes=[psr(0)])
                P.op('dve', lambda e: e.tensor_copy(mixT[:, 4:8, t * 128:(t + 1) * 128], PS[0][:, :].rearrange("p (a d) -> p a d", a=4)),
                     reads=[psr(0)], writes=[('mixT', 4 + c, t) for c in range(4)])

            NTc = NT if stop_after != 'C2a' else 0
            if NTc:
                stage_xa(0)
                stage_xd(0, 0)
                stage_xd(0, 1)
            for i in range(NTc + 1):
                if 1 <= i:
                    stage_z1(i - 1)
                if i + 1 < NTc:
                    stage_xa(i + 1)
                if i < NTc:
                    stage_y1(i)
                if i + 1 < NTc:
                    stage_xd(i + 1, 0)
                if 1 <= i:
                    stage_z2(i - 1)
                if i < NTc:
                    stage_y2(i)
                if i + 1 < NTc:
                    stage_xd(i + 1, 1)
                if i < NTc:
                    stage_y3(i)
            P.emit_phase()
        cst.close()
        if stop_after in ('C', 'C2a'):
            with contextlib.ExitStack() as ph:
                tmp = ph.enter_context(nc.sbuf_tensor('s_dbgtmp', [128, S], F32, side='left'))
                for c in range(8):
                    P.op('dve', (lambda c: lambda e: e.tensor_copy(tmp[:], mixT[:, c, :]))(c), reads=[('mixT', c, t) for t in range(NT)], writes=['dbgtmp'])
                    P.op('sp', (lambda c: lambda e: e.dma_start(out=dbg_d['mixT'][c], in_=tmp[:]))(c), reads=['dbgtmp'], dma='dbg')
                P.emit_phase()
            P.final_wait_all_dma()
            mixer.close()
            bcst.close()
            return nc

        xres = sbT('xres', [128, NT, D], F32)
        h2 = sbT('h2', [128, NT, D], BF16)
        aff = sbT('aff', [128, NT, 16], F32)
        sel = sbT('sel', [128, NT, 16], F32)
        pos = sbT('pos', [128, NT, 16], F32)
        with contextlib.ExitStack() as ph:
            def sb(name, shape, dt_):
                return ph.enter_context(nc.sbuf_tensor('s_' + name, list(shape), dt_, side='left'))
            tmpm = sb('tmpm', [128, 512], F32)
            xn2 = sb('xn2', [128, D], F32)
            xn3 = sb('xn3', [128, D], F32)
            h2f = [sb('h2f%d' % i, [128, D], F32) for i in range(2)]
            h2Tf = sb('h2Tf', [128, 8, 128], F32)
            rw = sb('rw', [128, 8, 16], F32)
            ss2 = sb('ss2', [128, 16], F32)
            ms2 = sb('ms2', [128, 16], F32)
            sq2 = sb('sq2', [128, 16], F32)
            rs2 = sb('rs2', [128, 16], F32)
            mx = sb('mx', [128, 16], F32)
            nmx = sb('nmx', [128, 16], F32)
            sme = sb('sme', [128, 16], F32)
            rsm = sb('rsm', [128, 16], F32)
            eaf = sb('eaf', [128, NT, 16], F32)
            P.op('sp', lambda e: e.dma_start(out=rw[:], in_=rw_d), writes=['rw'], dma='cst')
            wo = [wp_next(), wp_next()]
            for hf in range(2):
                load_w(wo[hf], wout_d[:, hf * 512:(hf + 1) * 512], 512)
            P.op('dve', lambda e: e.memset(ss2[:], 0.0), writes=['ss2'])
            P.op('dve', lambda e: e.memset(sme[:], 0.0), writes=['sme'])
            def d1_xpe(t):
                P.op('sp', (lambda t: lambda e: e.dma_start(out=xres[:, t, :], in_=x_d[t * 128:(t + 1) * 128, :]))(t), writes=[('xres', t)], dma=('xres', t))
                for hf in range(2):
                    bk = hf
                    for c in range(8):
                        P.op('pe', (lambda bk, hf, c: lambda e: e.matmul(PS[bk][:, :], mixT[:, c, t * 128:(t + 1) * 128], WP[wo[hf]][:, c, :], start=(c == 0), stop=(c == 7)))(bk, hf, c),
                             reads=[('wp', wo[hf]), ('mixT', c, t)], writes=[psr(bk)])

            def d1_xch(t):
                hb = t % 2
                for hf in range(2):
                    bk = hf
                    P.op('dve', (lambda bk, hf: lambda e: e.tensor_tensor(tmpm[:], PS[bk][:, :], bc3[:, 0, hf * 512:(hf + 1) * 512], ALU.mult))(bk, hf), reads=[psr(bk), ('bc4', 0)], writes=['tmpm'])
                    P.op('dve', (lambda hf: lambda e: e.tensor_tensor(xres[:, t, hf * 512:(hf + 1) * 512], xres[:, t, hf * 512:(hf + 1) * 512], tmpm[:], ALU.add))(hf), reads=['tmpm', ('xres', t)], writes=[('xres', t)])
                P.op('act', lambda e: e.activation(out=xn2[:], in_=xres[:, t, :], func=AF.Square, accum_out=ss2[:, t:t + 1]), reads=[('xres', t), 'ss2'], writes=['xn2', ('ss2', t)])
                P.op('dve', lambda e: e.tensor_scalar(ms2[:, t:t + 1], ss2[:, t:t + 1], 1.0 / D, EPS, ALU.mult, ALU.add), reads=[('ss2', t)], writes=[('ms2', t)])
                P.op('act', lambda e: e.activation(out=sq2[:, t:t + 1], in_=ms2[:, t:t + 1], func=AF.Sqrt), reads=[('ms2', t)], writes=[('sq2', t)])
                P.op('dve', lambda e: e.reciprocal(rs2[:, t:t + 1], sq2[:, t:t + 1]), reads=[('sq2', t)], writes=[('rs2', t)])
                P.op('act', lambda e: e.activation(out=xn3[:], in_=xres[:, t, :], func=AF.Copy, scale=rs2[:, t:t + 1]), reads=[('xres', t), ('rs2', t)], writes=['xn3'])
                P.op('pool', lambda e: e.tensor_tensor(xn3[:], xn3[:], bc3[:, 2, :], ALU.mult), reads=['xn3', ('bc4', 2)], writes=['xn3'])
                P.op('pool', lambda e: e.tensor_tensor(h2f[hb][:], xn3[:], bc3[:, 1, :], ALU.add), reads=['xn3', ('bc4', 1)], writes=[('h2f', hb)])
                P.op('act', lambda e: e.activation(out=h2[:, t, :], in_=h2f[hb][:], func=AF.Copy), reads=[('h2f', hb)], writes=[('h2', t)])

            def d1_ytr(t):
                hb = t % 2
                for c in range(8):
                    bk = 2 + c // 4
                    P.op('pe', (lambda bk, c: lambda e: e.transpose(PS[bk][:, (c % 4) * 128:(c % 4 + 1) * 128], h2f[hb][:, c * 128:(c + 1) * 128], ident_f[:]))(bk, c), reads=[('h2f', hb), 'ident_f'], writes=[psr(bk)])
                P.op('act', lambda e: e.activation(out=h2Tf[:, 0:4, :], in_=PS[2][:, :].rearrange("p (a d) -> p a d", a=4), func=AF.Copy), reads=[psr(2)], writes=[('h2Tf', 0)])
                P.op('dve', lambda e: e.tensor_copy(h2Tf[:, 4:8, :], PS[3][:, :].rearrange("p (a d) -> p a d", a=4)), reads=[psr(3)], writes=[('h2Tf', 1)])

            def d1_yrt(t):
                for c in range(8):
                    P.op('pe', (lambda c: lambda e: e.matmul(PS[4][:, t * 16:(t + 1) * 16], h2Tf[:, c, :], rw[:, c, :], start=(c == 0), stop=(c == 7)))(c), reads=[('h2Tf', c // 4), 'rw'], writes=[psr(4)])

            d1_xpe(0)
            d1_xch(0)
            for t in range(NT):
                if t + 1 < NT:
                    d1_xpe(t + 1)
                d1_ytr(t)
                if t + 1 < NT:
                    d1_xch(t + 1)
                d1_yrt(t)
            lg3 = PS[4][:, 0:256].rearrange("p (t k) -> p t k", t=NT)
            P.op('dve', lambda e: e.reduce_max(mx[:], lg3, axis=AX.X), reads=[psr(4)], writes=['mx'])
            P.op('dve', lambda e: e.tensor_scalar(nmx[:], mx[:], -1.0, None, ALU.mult), reads=['mx'], writes=['nmx'])
            for t in range(NT):
                P.op('act', (lambda t: lambda e: e.activation(out=eaf[:, t, :], in_=PS[4][:, t * 16:(t + 1) * 16], func=AF.Exp, bias=nmx[:, t:t + 1], accum_out=sme[:, t:t + 1]))(t),
                     reads=[psr(4), 'nmx', 'sme'], writes=[('eaf', t), ('sme', t)])
            P.op('dve', lambda e: e.reciprocal(rsm[:], sme[:]), reads=[('sme', t) for t in range(NT)], writes=['rsm'])
            P.op('dve', lambda e: e.tensor_tensor(aff[:], eaf[:], rsm[:].unsqueeze(2).to_broadcast([128, NT, 16]), ALU.mult), reads=[('eaf', t) for t in range(NT)] + ['rsm'], writes=['aff'])
            dump('x2', xres[:, :, :], [('xres', t) for t in range(NT)])
            dump('aff', aff[:, :, :], ['aff'])
            P.emit_phase()
        mixer.close()
        bcst.close()

        gsel = sbT('gsel', [128, NT, 16], F32)
        wpx_stack = contextlib.ExitStack()
        NWX = 3
        for i_ in range(NWX):
            WP.append(wpx_stack.enter_context(nc.sbuf_tensor('s_wpx%d' % i_, [128, 8, 512], BF16, side='left')))
        with contextlib.ExitStack() as ph:
            def sb(name, shape, dt_):
                return ph.enter_context(nc.sbuf_tensor('s_' + name, list(shape), dt_, side='left'))
            if stop_after in (None, 'E', 'E1'):
                for fb_ in range(len(WP) // 2):
                    for kind_ in ('g', 'u'):
                        prefetched[(0, kind_, fb_)] = issue_w(0, kind_, fb_)
            affT = sb('affT', [16, S], F32)
            work = sb('work', [16, S], F32)
            m8 = sb('m8', [16, 8], F32)
            csel = sb('csel', [128, NT, 16], F32)
            for t in range(NT):
                bk = t // 4
                P.op('pe', (lambda bk, t: lambda e: e.transpose(PS[bk][0:16, (t % 4) * 128:(t % 4 + 1) * 128], aff[:, t, :], ident_f[:]))(bk, t), reads=['aff', 'ident_f'], writes=[psr(bk)])
            for bk in range(4):
                P.op('dve', (lambda bk: lambda e: e.tensor_copy(affT[:, bk * 512:(bk + 1) * 512], PS[bk][0:16, :]))(bk), reads=[psr(bk)], writes=['affT'])
            P.op('dve', lambda e: e.tensor_copy(work[:], affT[:]), reads=['affT'], writes=['work'])
            for it_ in range(CAP // 8):
                P.op('dve', lambda e: e.max(m8[:], work[:]), reads=['work'], writes=['m8'])
                if it_ < CAP // 8 - 1:
                    P.op('dve', lambda e: e.match_replace(work[:], m8[:], work[:], -1.0), reads=['work', 'm8'], writes=['work'])
            P.op('dve', lambda e: e.tensor_scalar(work[:], affT[:], m8[:, 7:8], None, ALU.is_ge), reads=['affT', 'm8', 'work'], writes=['work'])
            for t in range(NT):
                P.op('pe', (lambda t: lambda e: e.transpose(PS[4][:, t * 16:(t + 1) * 16], work[:, t * 128:(t + 1) * 128], ident_f[0:16, 0:16]))(t), reads=['work', 'ident_f'], writes=[psr(4)])
            P.op('dve', lambda e: e.tensor_copy(sel[:], PS[4][:, 0:256].rearrange("p (t k) -> p t k", t=NT)), reads=[psr(4)], writes=['sel'])
            P.op('dve', lambda e: e.memset(csel[:, 0, :], 0.0), writes=[('csel', 0)])
            for t in range(1, NT):
                P.op('dve', (lambda t: lambda e: e.tensor_tensor(csel[:, t, :], csel[:, t - 1, :], sel[:, t - 1, :], ALU.add))(t), reads=[('csel', t - 1), 'sel'], writes=[('csel', t)])
            for t in range(NT):
                P.op('pe', (lambda t: lambda e: e.matmul(PS[5][:, t * 16:(t + 1) * 16], tri_ep[:], sel[:, t, :], start=True, stop=False))(t), reads=['tri_ep', 'sel'], writes=[psr(5)])
                P.op('pe', (lambda t: lambda e: e.matmul(PS[5][:, t * 16:(t + 1) * 16], ones_f[:], csel[:, t, :], start=False, stop=True))(t), reads=['ones_f', ('csel', t)], writes=[psr(5)])
            P.op('dve', lambda e: e.tensor_copy(pos[:], PS[5][:, 0:256].rearrange("p (t k) -> p t k", t=NT)), reads=[psr(5)], writes=['pos'])
            P.op('dve', lambda e: e.tensor_tensor(gsel[:], aff[:], sel[:], ALU.mult), reads=['aff', 'sel'], writes=['gsel'])
            dump('sel', sel[:, :, :], ['sel'])
            dump('pos', pos[:, :, :], ['pos'])
            P.emit_phase()

        with contextlib.ExitStack() as ph:
            def sb(name, shape, dt_):
                return ph.enter_context(nc.sbuf_tensor('s_' + name, list(shape), dt_, side='left'))
            oh = [sb('oh%d' % i, [128, 256], BF16) for i in range(4)]
            ohg = [sb('ohg%d' % i, [128, 256], BF16) for i in range(4)]
            xgT = [sb('xgT0', [128, 8, 256], BF16)] * 2
            hact = sb('hact', [128, NFC, 256], BF16)
            ohT = [sb('ohT%d' % i, [128, 2, S], BF16) for i in range(2)]
            sg = [sb('sg%d' % i, [128, 256], F32) for i in range(2)]
            ye = sb('ye', [128, 2, D], BF16)
            print('E: sbuf bytes remaining after locals', nc.sbuf_bytes_remaining)
            nblk = [(i * 512, min(512, FF - i * 512)) for i in range(6)]
            ohc = [0]
            NE = NEXP if stop_after != 'E1' else 1

            def gather_units(ex):
                xb = ex % 2
                units = []
                obuf = {}

                def mk_oh(t):
                    ob = ohc[0] % 4
                    ohc[0] += 1
                    obuf[t] = ob
                    P.op('dve', (lambda ob: lambda e: e.tensor_scalar(oh[ob][:], iota_j[:], pos[:, t, ex:ex + 1], sel[:, t, ex:ex + 1], ALU.is_equal, ALU.mult))(ob),
                         reads=['iota_j', 'pos', 'sel'], writes=[('oh', ob)])
                    P.op('dve', (lambda ob: lambda e: e.tensor_scalar(ohg[ob][:], iota_j[:], pos[:, t, ex:ex + 1], gsel[:, t, ex:ex + 1], ALU.is_equal, ALU.mult))(ob),
                         reads=['iota_j', 'pos', 'gsel'], writes=[('ohg', ob)])

                def pre():
                    mk_oh(0)
                    mk_oh(1)
                    for b4_ in range(4):
                        P.op('pe', (lambda b4_: lambda e: e.matmul(PS[b4_][:, :], zeros_b[:, 0:128], zeros_b[:, :], start=True, stop=False))(b4_), reads=['zeros_b'], writes=[psr(b4_)])
                units.append(pre)
                for t in range(NT):
                    def u(t=t):
                        if t + 2 < NT:
                            mk_oh(t + 2)
                        ob = obuf[t]
                        for c in range(8):
                            P.op('pe', (lambda ob, c: lambda e: e.matmul(PS[c // 2][:, (c % 2) * 256:(c % 2 + 1) * 256], h2[:, t, c * 128:(c + 1) * 128], oh[ob][:], start=False, stop=(t == NT - 1 and c % 2 == 1)))(ob, c),
                                 reads=[('h2', t), ('oh', ob)], writes=[psr(c // 2)])
                        for jh in range(2):
                            P.op('pe', (lambda ob, jh: lambda e: e.matmul(PS[4 + jh][:, (t % 4) * 128:(t % 4 + 1) * 128], ohg[ob][:, jh * 128:(jh + 1) * 128], ident_b[:], start=True, stop=True))(ob, jh),
                                 reads=[('ohg', ob), 'ident_b'], writes=[psr(4 + jh)])
                        if t % 4 == 3:
                            t0_ = t - 3
                            P.op('act', (lambda t0_: lambda e: e.activation(out=ohT[xb][:, 0, t0_ * 128:(t0_ + 4) * 128], in_=PS[4][:, :], func=AF.Copy))(t0_),
                                 reads=[psr(4)], writes=[('ohT', xb, 0, t0_ // 4)])
                            P.op('dve', (lambda t0_: lambda e: e.tensor_copy(ohT[xb][:, 1, t0_ * 128:(t0_ + 4) * 128], PS[5][:, :]))(t0_),
                                 reads=[psr(5)], writes=[('ohT', xb, 1, t0_ // 4)])
                    units.append(u)

                def fin():
                    for b4 in range(4):
                        if b4 % 2 == 0:
                            P.op('act', (lambda b4: lambda e: e.activation(out=xgT[xb][:, 2 * b4:2 * b4 + 2, :], in_=PS[b4][:, :].rearrange("p (a d) -> p a d", a=2), func=AF.Copy))(b4), reads=[psr(b4)], writes=[('xgT', 0, b4)])
                        else:
                            P.op('dve', (lambda b4: lambda e: e.tensor_copy(xgT[xb][:, 2 * b4:2 * b4 + 2, :], PS[b4][:, :].rearrange("p (a d) -> p a d", a=2)))(b4), reads=[psr(b4)], writes=[('xgT', 0, b4)])
                units.append(fin)
                return units

            def scatter_units(ex):
                xb = ex % 2
                units = []
                for t in range(NT):
                    for dh in range(2):
                        def u(t=t, dh=dh):
                            bk = 4 + (t * 2 + dh) % 2
                            for jh in range(2):
                                P.op('pe', (lambda bk, jh: lambda e: e.matmul(PS[bk][:, :], ohT[xb][:, jh, t * 128:(t + 1) * 128], ye[:, jh, dh * 512:(dh + 1) * 512], start=(jh == 0), stop=(jh == 1)))(bk, jh),
                                     reads=[('ohT', xb, jh, t // 4), ('ye', jh, dh)], writes=[psr(bk)])
                            P.op('dve', (lambda bk: lambda e: e.tensor_tensor(xres[:, t, dh * 512:(dh + 1) * 512], xres[:, t, dh * 512:(dh + 1) * 512], PS[bk][:, :], ALU.add))(bk),
                                 reads=[psr(bk), ('xres', t)], writes=[('xres', t)])
                        units.append(u)
                return units

            def ffn(ex, fillers):
                xb = ex % 2
                fillers = list(fillers)
                nfc_done = [0]
                nfill_total = [len(fillers)]
                nfill_emitted = [0]
                for fb, (f0, fw) in enumerate(nblk):
                    wgi = get_w(ex, 'g', fb)
                    wui = get_w(ex, 'u', fb)
                    for k in range(fw // 128):
                        fi = fb * 4 + k
                        bk = 6 + fi % 2
                        for c in range(8):
                            P.op('pe', (lambda bk, wgi, c, k: lambda e: e.matmul(PS[bk][:, 0:256], WP[wgi][:, c, k * 128:(k + 1) * 128], xgT[xb][:, c, :], start=(c == 0), stop=(c == 7)))(bk, wgi, c, k),
                                 reads=[('wp', wgi), ('xgT', 0, c // 2)], writes=[psr(bk)])
                        for c in range(8):
                            P.op('pe', (lambda bk, wui, c, k: lambda e: e.matmul(PS[bk][:, 256:512], WP[wui][:, c, k * 128:(k + 1) * 128], xgT[xb][:, c, :], start=(c == 0), stop=(c == 7)))(bk, wui, c, k),
                                 reads=[('wp', wui), ('xgT', 0, c // 2)], writes=[psr(bk)])
                        sgi = fi % 2
                        P.op('act', (lambda bk, sgi: lambda e: e.activation(out=sg[sgi][:], in_=PS[bk][:, 0:256], func=AF.Silu))(bk, sgi), reads=[psr(bk)], writes=[('sg', sgi)])
                        P.op('dve', (lambda bk, sgi, fi: lambda e: e.tensor_tensor(hact[:, fi, :], sg[sgi][:], PS[bk][:, 256:512], ALU.mult))(bk, sgi, fi), reads=[psr(bk), ('sg', sgi)], writes=[('hact', fi)])
                        nfc_done[0] += 1
                        want = (len(fillers) * 0 + nfill_total[0] * nfc_done[0] + NFC - 1) // NFC
                        while nfill_emitted[0] < want and fillers:
                            fillers.pop(0)()
                            nfill_emitted[0] += 1
                for fb in range(6):
                    nk = 4 if fb < 5 else 2
                    wdi = get_w(ex, 'd', fb)
                    for k in range(nk):
                        fi = fb * 4 + k
                        for jh in range(2):
                            for dh in range(2):
                                bk = jh * 2 + dh
                                P.op('pe', (lambda bk, wdi, fi, k, jh, dh: lambda e: e.matmul(PS[bk][:, :], hact[:, fi, jh * 128:(jh + 1) * 128], WP[wdi][:, 2 * k + dh, :], start=(fi == 0), stop=(fi == NFC - 1)))(bk, wdi, fi, k, jh, dh),
                                     reads=[('wp', wdi), ('hact', fi)], writes=[psr(bk)])
                for jh in range(2):
                    for dh in range(2):
                        bk = jh * 2 + dh
                        P.op('dve', (lambda bk, jh, dh: lambda e: e.tensor_tensor(ye[:, jh, dh * 512:(dh + 1) * 512], PS[bk][:, :], g2bc[:, dh * 512:(dh + 1) * 512], ALU.mult))(bk, jh, dh),
                             reads=[psr(bk), ('g2bc' if False else ('bc4', 3))], writes=[('ye', jh, dh)])

            for u in gather_units(0):
                u()
            for ex in range(NE):
                ffn(ex, scatter_units(ex - 1) if ex >= 1 else [])
                if ex + 1 < NE:
                    for u in gather_units(ex + 1):
                        u()
            for u in scatter_units(NE - 1):
                u()
            P.emit_phase()

        del WP[3:]
        wpx_stack.close()
        with contextlib.ExitStack() as ph:
            def sb(name, shape, dt_):
                return ph.enter_context(nc.sbuf_tensor('s_' + name, list(shape), dt_, side='left'))
            gfin = sb('gfin', [128, D], F32)
            ob_ = [sb('ob%d' % i, [128, D], F32) for i in range(2)]
            junkf = sb('junkf', [128, D], F32)
            ssf = sb('ssf', [128, 16], F32)
            msf = sb('msf', [128, 16], F32)
            sqf = sb('sqf', [128, 16], F32)
            rsf = sb('rsf', [128, 16], F32)
            P.op('sp', lambda e: e.dma_start(out=gfin[:], in_=gfin_d), writes=['gfin'], dma='cst')
            P.op('dve', lambda e: e.memset(ssf[:], 0.0), writes=['ssf'])
            for t in range(NT):
                b = t % 2
                P.op('act', (lambda t: lambda e: e.activation(out=junkf[:], in_=xres[:, t, :], func=AF.Square, accum_out=ssf[:, t:t + 1]))(t), reads=[('xres', t), 'ssf'], writes=['junkf', ('ssf', t)])
                P.op('dve', (lambda t: lambda e: e.tensor_scalar(msf[:, t:t + 1], ssf[:, t:t + 1], 1.0 / D, EPS, ALU.mult, ALU.add))(t), reads=[('ssf', t)], writes=[('msf', t)])
                P.op('act', (lambda t: lambda e: e.activation(out=sqf[:, t:t + 1], in_=msf[:, t:t + 1], func=AF.Sqrt))(t), reads=[('msf', t)], writes=[('sqf', t)])
                P.op('dve', (lambda t: lambda e: e.reciprocal(rsf[:, t:t + 1], sqf[:, t:t + 1]))(t), reads=[('sqf', t)], writes=[('rsf', t)])
                P.op('dve', (lambda b, t: lambda e: e.scalar_tensor_tensor(ob_[b][:], xres[:, t, :], rsf[:, t:t + 1], gfin[:], ALU.mult, ALU.mult))(b, t), reads=[('xres', t), ('rsf', t), 'gfin'], writes=[('ob', b)])
                P.op('sp', (lambda b, t: lambda e: e.dma_start(out=out_d[t * 128:(t + 1) * 128, :], in_=ob_[b][:]))(b, t), reads=[('ob', b)], dma=('ob', b))
            P.emit_phase()
        P.final_wait_all_dma()
    return nc


def _t5_bucket_static(rel):
    nb = 16
    ret = np.where(rel > 0, nb, 0)
    n = np.abs(rel)
    max_exact = nb // 2
    nf = np.maximum(n, 1).astype(np.float32)
    large = max_exact + (np.log(nf / max_exact) / math.log(128 / max_exact) * (nb - max_exact)).astype(np.int32)
    large = np.minimum(large, nb - 1)
    return ret + np.where(n < max_exact, n, large)


def _col(v, n):
    return np.ascontiguousarray(np.asarray(v, np.float32).reshape(n, 128).T)


def _rep(v):
    v = np.asarray(v, np.float32).reshape(1, -1)
    return np.ascontiguousarray(np.broadcast_to(v, (128, v.shape[1])))


def prep_inputs(inp):
    f = lambda a: np.ascontiguousarray(np.asarray(a, np.float32))
    sh = {}
    sh['ada_w'] = f(inp['ada_w'][0])
    ada_b = f(inp['ada_b'][0])
    sh['ada_brow'] = np.ascontiguousarray(ada_b.reshape(1, -1))
    sh['ada_bg'] = _rep(ada_b[2048:6144])
    sh['gmixT'] = _col(inp['norm_mix_g'][0], 8)
    sh['gffn_bc'] = _rep(inp['norm_ffn_g'][0])
    sh['gfin_bc'] = _rep(inp['norm_final_g'])
    sh['w_in'] = f(inp['w_in'][0])
    sh['lam_qk'] = _rep(np.concatenate([f(inp['lambda_q1'][0]), f(inp['lambda_k1'][0]), f(inp['lambda_q2'][0]), f(inp['lambda_k2'][0])]))
    sh['subln_bc'] = _rep(inp['attn_subln_g'][0])
    tab = f(inp['rel_bias_table'])
    kl = np.arange(128)[:, None]
    ql = np.arange(128)[None, :]
    blk = np.zeros((128, 3, 4, 128), np.float32)
    for d in (-1, 0, 1):
        bidx = _t5_bucket_static(d * 128 + kl - ql)
        for h in range(4):
            blk[:, d + 1, h, :] = tab[bidx, h]
    sh['biasblk'] = blk
    sh['cfar'] = _rep(np.concatenate([tab[15, :], tab[31, :]]))
    cw = f(inp['conv_w'][0])[:, 0, :]
    sh['conv_wT'] = np.ascontiguousarray(cw.reshape(5, 6, 128).transpose(2, 1, 0))
    sh['conv_bT'] = _col(inp['conv_b'][0], 6)
    dtb = np.concatenate([f(inp['dt_bias_f'][0]), f(inp['dt_bias_b'][0])])
    sh['dtb256'] = _rep(np.tile(dtb, 16))
    alog = np.concatenate([f(inp['A_log_f'][0]), f(inp['A_log_b'][0])])
    sh['alog256'] = _rep(np.tile(alog, 16))
    sh['dskip_bc'] = _rep(inp['D_skip'][0])
    sh['ssmg_bc'] = _rep(inp['ssm_norm_g'][0])
    sh['w_out'] = f(inp['w_out'][0])
    sh['router_wT'] = np.ascontiguousarray(f(inp['router_w'][0]).reshape(8, 128, 16).transpose(1, 0, 2))
    sh['w_gate'] = f(inp['w_gate'][0])
    sh['w_up'] = f(inp['w_up'][0])
    sh['w_down'] = f(inp['w_down'][0])
    x = f(inp['x'])
    c = f(inp['c'])
    maps = []
    for b in range(x.shape[0]):
        m = dict(sh)
        m['x'] = x[b]
        m['c_col'] = _col(c[b], 8)
        maps.append(m)
    return maps


_NC_CACHE = {}


def kernel(**inputs):
    maps = prep_inputs(inputs)
    if 'nc' not in _NC_CACHE:
        _NC_CACHE['nc'] = build()
    nc = _NC_CACHE['nc']
    res = run_bass_kernel_spmd(nc, maps, core_ids=list(range(8)))
    return np.stack([np.asarray(r['out'], np.float32) for r in res.results], axis=0)
```

```python
import contextlib
import math
import numpy as np
import concourse.bass as bass
import concourse.mybir as mybir
from concourse.bass_utils import run_bass_kernel_spmd

F32 = mybir.dt.float32
BF16 = mybir.dt.bfloat16
ALU = mybir.AluOpType
AF = mybir.ActivationFunctionType
AX = mybir.AxisListType

S = 2048
D = 1024
NT = 16
EPS = 1e-6
NEXP = 16
FF = 2816
NFC = 22
CAP = 256
LAM_INIT = 0.8 - 0.6 * math.exp(0.0)


class Prog:
    ENGS = ('pe', 'act', 'dve', 'pool', 'sp')

    def __init__(self, nc, esems, dma_sems):
        self.nc = nc
        self.esem = esems
        self.ecnt = {e: 0 for e in self.ENGS}
        self.dsem = {}
        self.free_dsems = list(dma_sems)
        self.ops = []
        self.lastw = {}
        self.readers = {}
        self.known = {e: {} for e in self.ENGS}

    def _dma_token(self, key):
        if key not in self.dsem:
            self.dsem[key] = [self.free_dsems.pop(), 0]
        ent = self.dsem[key]
        ent[1] += 16
        return ('d', key, ent[1])

    def op(self, eng, fn, reads=(), writes=(), dma=None):
        idx = len(self.ops)
        deps = set()
        for r in reads:
            w = self.lastw.get(r)
            if w is not None:
                deps.add(w)
            if isinstance(r, tuple) and r[0] == 'ps':
                for t in self.readers.get(r, ()):
                    if t[0] == 'c' and self.ops[t[1]]['eng'] != eng:
                        deps.add(t)
        for r in writes:
            w = self.lastw.get(r)
            if w is not None:
                deps.add(w)
            for t in self.readers.get(r, ()):
                if t[0] == 'c' and dma is None and self.ops[t[1]]['eng'] == eng and eng == 'pe':
                    continue
                deps.add(t)
        tok = self._dma_token(dma) if dma is not None else ('c', idx)
        fdeps = set()
        for t in deps:
            if t[0] == 'c' and dma is None and eng == 'pe' and self.ops[t[1]]['eng'] == 'pe':
                continue
            fdeps.add(t)
        self.ops.append(dict(eng=eng, fn=fn, deps=fdeps, dma=dma, tok=tok))
        for r in reads:
            self.readers.setdefault(r, []).append(tok)
        for r in writes:
            self.lastw[r] = tok
            self.readers[r] = []
        return tok

    def emit_phase(self):
        self.phase_no = getattr(self, 'phase_no', 0) + 1
        with self.nc.named_scope('ph%d' % self.phase_no):
            self._emit_phase()

    def _emit_phase(self):
        nc = self.nc
        ops = self.ops
        sig = set()
        for o in ops:
            for t in o['deps']:
                if t[0] == 'c':
                    sig.add(t[1])
        cnt = {}
        for i, o in enumerate(ops):
            if i in sig:
                self.ecnt[o['eng']] += 1
                cnt[i] = self.ecnt[o['eng']]
        per = {e: [] for e in self.ENGS}
        for i, o in enumerate(ops):
            per[o['eng']].append(i)

        def run(eng_name, engobj):
            kn = self.known[eng_name]
            for i in per[eng_name]:
                o = ops[i]
                need = {}
                for t in o['deps']:
                    if t[0] == 'c':
                        key = ('e', ops[t[1]]['eng'])
                        val = cnt[t[1]]
                    else:
                        key = ('d', t[1])
                        val = t[2]
                        if t[1] in ('cst', 'cstB'):
                            val = self.dsem[t[1]][1]
                    if kn.get(key, 0) >= val:
                        continue
                    need[key] = max(need.get(key, 0), val)
                for key, val in need.items():
                    sem = self.esem[key[1]] if key[0] == 'e' else self.dsem[key[1]][0]
                    engobj.wait_ge(sem, val)
                    kn[key] = val
                ins = o['fn'](engobj)
                if o['dma'] is not None:
                    ins.then_inc(self.dsem[o['dma']][0], 16)
                elif i in sig:
                    ins.then_inc(self.esem[o['eng']], 1)

        with nc.Block() as block:
            if per['pe']:
                @block.tensor
                def _(e):
                    run('pe', e)
            if per['act']:
                @block.scalar
                def _(e):
                    run('act', e)
            if per['dve']:
                @block.vector
                def _(e):
                    run('dve', e)
            if per['pool']:
                @block.gpsimd
                def _(e):
                    run('pool', e)
            if per['sp']:
                @block.sync
                def _(e):
                    run('sp', e)
        for r in list(self.lastw.keys()):
            if self.lastw[r] is not None and self.lastw[r][0] == 'c':
                self.lastw[r] = None
        for r in list(self.readers.keys()):
            self.readers[r] = [t for t in self.readers[r] if t[0] == 'd']
        self.ops = []

    def final_wait_all_dma(self):
        nc = self.nc
        with nc.Block() as block:
            @block.sync
            def _(e):
                for key, (sem, val) in self.dsem.items():
                    if val > 0:
                        e.wait_ge(sem, val)


def build(stop_after=None, dbg=None):
    dbg = dbg or {}
    nc = bass.Bass("TRN2", target_bir_lowering=False)

    def din(name, shape):
        return nc.dram_tensor(name, list(shape), F32, kind="ExternalInput").ap()

    x_d = din("x", [S, D])
    ccol_d = din("c_col", [128, 8])
    adaw_d = din("ada_w", [D, 6 * D])
    adabrow_d = din("ada_brow", [1, 6 * D])
    adabg_d = din("ada_bg", [128, 4096])
    gmixT_d = din("gmixT", [128, 8])
    gffn_d = din("gffn_bc", [128, D])
    gfin_d = din("gfin_bc", [128, D])
    win_d = din("w_in", [D, 2832])
    lamqk_d = din("lam_qk", [128, 256])
    subln_d = din("subln_bc", [128, 128])
    biasblk_d = din("biasblk", [128, 3, 4, 128])
    cfar_d = din("cfar", [128, 8])
    convw_d = din("conv_wT", [128, 6, 5])
    convb_d = din("conv_bT", [128, 6])
    dtb_d = din("dtb256", [128, 256])
    alog_d = din("alog256", [128, 256])
    dskip_d = din("dskip_bc", [128, 8])
    ssmg_d = din("ssmg_bc", [128, 512])
    wout_d = din("w_out", [D, D])
    rw_d = din("router_wT", [128, 8, 16])
    if stop_after in (None, 'E', 'E1'):
        wg_d = din("w_gate", [NEXP, D, FF])
        wu_d = din("w_up", [NEXP, D, FF])
        wd_d = din("w_down", [NEXP, FF, D])
    out_d = nc.dram_tensor("out", [S, D], F32, kind="ExternalOutput").ap()
    dbg_d = {k: nc.dram_tensor("dbg_" + k, list(shp), F32, kind="ExternalOutput").ap() for k, shp in dbg.items()}

    with contextlib.ExitStack() as top:
        def sbT(name, shape, dt, side='right'):
            return top.enter_context(nc.sbuf_tensor('s_' + name, list(shape), dt, side=side))

        esems = {e: top.enter_context(nc.semaphore('es_' + e)) for e in ('pe', 'act', 'dve', 'pool')}
        dsems = [top.enter_context(nc.semaphore('ds%d' % i)) for i in range(48)]
        P = Prog(nc, esems, dsems)
        PS = [top.enter_context(nc.psum_tensor('psb%d' % i, [128, 512], F32)) for i in range(8)]

        def psr(b):
            return ('ps', b)

        ident_f = sbT('ident_f', [128, 128], F32)
        ident_b = sbT('ident_b', [128, 128], BF16)
        ones_f = sbT('ones_f', [128, 128], F32)
        tri_ip = sbT('tri_ip', [128, 128], F32)
        tri_es = sbT('tri_es', [128, 128], F32)
        tri_is = sbT('tri_is', [128, 128], F32)
        tri_ep = sbT('tri_ep', [128, 128], F32)
        negm_f = sbT('negm_f', [128, 128], F32)
        negm_b = sbT('negm_b', [128, 128], F32)
        iota_j = sbT('iota_j', [128, 256], F32)
        iota_p = sbT('iota_p', [128, 2], F32)
        modT = sbT('modT', [128, 48], F32)
        a1 = sbT('a1', [128, 8], F32)
        g2bc = sbT('g2bc', [128, D], F32)
        WP = [sbT('wp%d' % i, [128, 8, 512], BF16) for i in range(3)]
        wp_ctr = [0]

        def wp_next():
            i = wp_ctr[0] % len(WP)
            wp_ctr[0] += 1
            return i

        def load_w(i, src_ap, ncols, nk=8):
            P.op('pool', lambda e: e.dma_start(out=WP[i][:, 0:nk, 0:ncols],
                                               in_=src_ap.rearrange("(c p) n -> p c n", p=128)),
                 writes=[('wp', i)], dma=('wp', i))


        NBLK = [(i * 512, min(512, FF - i * 512)) for i in range(6)]
        prefetched = {}

        def issue_w(ex, kind, fb):
            i = wp_next()
            if kind in ('g', 'u'):
                f0, fw = NBLK[fb]
                src = (wg_d if kind == 'g' else wu_d)[ex, :, f0:f0 + fw]
                load_w(i, src, fw)
            else:
                nk = 4 if fb < 5 else 2
                P.op('pool', lambda e: e.dma_start(out=WP[i][:, 0:2 * nk, :].rearrange("p (k h) n -> p k h n", h=2),
                                                   in_=wd_d[ex, fb * 512:fb * 512 + nk * 128, :].rearrange("(k p) (h n) -> p k h n", p=128, h=2)),
                     writes=[('wp', i)], dma=('wp', i))
            return i

        def get_w(ex, kind, fb):
            key = (ex, kind, fb)
            if key in prefetched:
                return prefetched.pop(key)
            return issue_w(ex, kind, fb)

        def dump(key, ap, reads):
            if key in dbg_d:
                P.op('sp', lambda e: e.dma_start(out=dbg_d[key], in_=ap), reads=reads, dma='dbg')

        P.op('pool', lambda e: e.memset(ident_f[:], 0.0), writes=['ident_f'])
        P.op('pool', lambda e: e.affine_select(ident_f[:], ident_f[:], [[-1, 128]], ALU.not_equal, 1.0, base=0, channel_multiplier=1),
             reads=['ident_f'], writes=['ident_f'])
        P.op('pool', lambda e: e.tensor_copy(ident_b[:], ident_f[:]), reads=['ident_f'], writes=['ident_b'])
        P.op('pool', lambda e: e.memset(ones_f[:], 1.0), writes=['ones_f'])
        for tl, key, cmp_ in ((tri_ip, 'tri_ip', ALU.is_ge), (tri_ep, 'tri_ep', ALU.is_gt)):
            P.op('pool', (lambda tl, cmp_: lambda e: e.affine_select(tl[:], ones_f[:], [[1, 128]], cmp_, 0.0, base=0, channel_multiplier=-1))(tl, cmp_),
                 reads=['ones_f'], writes=[key])
        for tl, key, cmp_ in ((tri_is, 'tri_is', ALU.is_ge), (tri_es, 'tri_es', ALU.is_gt)):
            P.op('pool', (lambda tl, cmp_: lambda e: e.affine_select(tl[:], ones_f[:], [[-1, 128]], cmp_, 0.0, base=0, channel_multiplier=1))(tl, cmp_),
                 reads=['ones_f'], writes=[key])
        zeros_f = sbT('zeros_f', [128, 128], F32)
        zeros_b = sbT('zeros_b', [128, 512], BF16)
        P.op('pool', lambda e: e.memset(zeros_b[:], 0.0), writes=['zeros_b'])
        P.op('pool', lambda e: e.memset(zeros_f[:], 0.0), writes=['zeros_f'])
        P.op('pool', lambda e: e.affine_select(negm_f[:], zeros_f[:], [[1, 128]], ALU.is_ge, -30000.0, base=0, channel_multiplier=-1),
             reads=['zeros_f'], writes=['negm_f'])
        P.op('pool', lambda e: e.affine_select(negm_b[:], zeros_f[:], [[-1, 128]], ALU.is_ge, -30000.0, base=0, channel_multiplier=1),
             reads=['zeros_f'], writes=['negm_b'])
        P.op('pool', lambda e: e.iota(iota_j[:], [[1, 256]], base=0, channel_multiplier=0, allow_small_or_imprecise_dtypes=True), writes=['iota_j'])
        P.op('pool', lambda e: e.iota(iota_p[:], [[128, 2]], base=0, channel_multiplier=1, allow_small_or_imprecise_dtypes=True), writes=['iota_p'])

        bcst = contextlib.ExitStack()
        bc3 = bcst.enter_context(nc.sbuf_tensor('s_bc3', [128, 3, D], F32, side='left'))

        def bcrow(i):
            return bc3[:, i, :] if i < 3 else g2bc[:, :]

        mixer = contextlib.ExitStack()
        mixT = mixer.enter_context(nc.sbuf_tensor('mixT', [128, 8, S], BF16, side='left'))
        hstack = contextlib.ExitStack()
        hT = hstack.enter_context(nc.sbuf_tensor('hT', [128, 8, S], BF16, side='left'))

        with contextlib.ExitStack() as ph:
            def sb(name, shape, dt):
                return ph.enter_context(nc.sbuf_tensor('s_' + name, list(shape), dt, side='left'))
            adaw = [sb('adaw%d' % i, [128, 8, 512], F32) for i in range(2)]
            xt = [sb('xt%d' % i, [128, D], F32) for i in range(2)]
            xn = [sb('xn%d' % i, [128, D], F32) for i in range(2)]
            ccol = sb('ccol', [128, 8], F32)
            scv = sb('scv', [128, 8], F32)
            scb = sb('scb', [128, 8, 128], F32)
            abr = [sb('abr%d' % i, [1, 512], F32) for i in range(2)]
            rowt = [sb('rowt%d' % i, [1, 512], F32) for i in range(2)]
            abg = sb('abg', [128, 4096], F32)
            gmixT = sb('gmixT', [128, 8], F32)
            gffn = sb('gffn', [128, D], F32)
            ss = sb('ss', [128, 16], F32)
            ms = sb('ms', [128, 16], F32)
            sq = sb('sq', [128, 16], F32)
            rstd = sb('rstd', [128, 16], F32)

            P.op('sp', lambda e: e.dma_start(out=ccol[:], in_=ccol_d), writes=['ccol'], dma='cst')
            P.op('sp', lambda e: e.dma_start(out=gmixT[:], in_=gmixT_d), writes=['gmixT'], dma='cst')
            P.op('sp', lambda e: e.dma_start(out=abg[:], in_=adabg_d), writes=['abg'], dma='cst')
            P.op('sp', lambda e: e.dma_start(out=gffn[:], in_=gffn_d), writes=['gffn'], dma='cst')
            P.op('act', lambda e: e.activation(out=scv[:], in_=ccol[:], func=AF.Silu), reads=['ccol'], writes=['scv'])
            P.op('dve', lambda e: e.tensor_copy(scb[:], scv[:].unsqueeze(2).to_broadcast([128, 8, 128])), reads=['scv'], writes=['scb'])
            adaw_v = adaw_d.rearrange("(c p) n -> p c n", p=128)
            gi_ctr = [0]

            def ada_block(blk):
                ab = blk % 2
                gi = gi_ctr[0]
                bk = 1 + ab
                P.op('sp', (lambda ab, blk: lambda e: e.dma_start(out=adaw[ab][:], in_=adaw_v[:, :, blk * 512:(blk + 1) * 512]))(ab, blk),
                     writes=[('adaw', ab)], dma=('adaw', ab))
                P.op('sp', (lambda ab, blk: lambda e: e.dma_start(out=abr[ab][:], in_=adabrow_d[:, blk * 512:(blk + 1) * 512]))(ab, blk),
                     writes=[('abr', ab)], dma=('abr', ab))
                for c in range(8):
                    P.op('pe', (lambda ab, bk, c: lambda e: e.matmul(PS[bk][:, :], scb[:, c, :], adaw[ab][:, c, :], start=(c == 0), stop=(c == 7)))(ab, bk, c),
                         reads=[('adaw', ab), 'scb'], writes=[psr(bk)])
                P.op('act', (lambda ab, bk, blk: lambda e: e.activation(out=rowt[ab][0:1, :], in_=PS[bk][0:1, :], func=AF.Copy))(ab, bk, blk), reads=[psr(bk)], writes=[('rowt', ab)])
                P.op('dve', (lambda ab, blk: lambda e: e.tensor_tensor(rowt[ab][0:1, :], rowt[ab][0:1, :], abr[ab][0:1, :], ALU.add))(ab, blk), reads=[('rowt', ab), ('abr', ab)], writes=[('rowt', ab)])
                if blk >= 4:
                    P.op('dve', (lambda bk, gi: lambda e: e.tensor_tensor(bcrow(gi // 2)[:, (gi % 2) * 512:(gi % 2 + 1) * 512], PS[bk][:, :], abg[:, gi * 512:(gi + 1) * 512], ALU.add))(bk, gi),
                         reads=[psr(bk), 'abg'], writes=[('bc4', gi // 2)])
                    gi_ctr[0] += 1
                for jj in range(4):
                    j = blk * 4 + jj
                    P.op('pe', (lambda ab, jj, j: lambda e: e.transpose(PS[0][:, j:j + 1], rowt[ab][0:1, jj * 128:(jj + 1) * 128], ident_f[0:1, 0:1]))(ab, jj, j),
                         reads=[('rowt', ab), 'ident_f'], writes=[psr(0)])
            for blk in range(4):
                ada_block(blk)
            P.op('dve', lambda e: e.tensor_copy(modT[:, 0:16], PS[0][:, 0:16]), reads=[psr(0)], writes=[('modT', 0)])
            P.op('dve', lambda e: e.scalar_tensor_tensor(a1[:], modT[:, 8:16], 1.0, gmixT[:], ALU.add, ALU.mult), reads=[('modT', 0), 'gmixT'], writes=['a1'])
            P.op('dve', lambda e: e.memset(ss[:], 0.0), writes=['ss'])
            def norm_tile(t):
                b = t % 2
                P.op('sp', (lambda b, t: lambda e: e.dma_start(out=xt[b][:], in_=x_d[t * 128:(t + 1) * 128, :]))(b, t), writes=[('xt', b)], dma=('xt', b))
                P.op('act', (lambda b, t: lambda e: e.activation(out=xn[b][:], in_=xt[b][:], func=AF.Square, accum_out=ss[:, t:t + 1]))(b, t),
                     reads=[('xt', b), 'ss'], writes=[('xn', b), ('ss', t)])
                P.op('dve', (lambda t: lambda e: e.tensor_scalar(ms[:, t:t + 1], ss[:, t:t + 1], 1.0 / D, EPS, ALU.mult, ALU.add))(t), reads=[('ss', t)], writes=[('ms', t)])
                P.op('act', (lambda t: lambda e: e.activation(out=sq[:, t:t + 1], in_=ms[:, t:t + 1], func=AF.Sqrt))(t), reads=[('ms', t)], writes=[('sq', t)])
                P.op('dve', (lambda t: lambda e: e.reciprocal(rstd[:, t:t + 1], sq[:, t:t + 1]))(t), reads=[('sq', t)], writes=[('rstd', t)])
                P.op('dve', (lambda b, t: lambda e: e.tensor_scalar(xn[b][:], xt[b][:], rstd[:, t:t + 1], None, ALU.mult))(b, t),
                     reads=[('xt', b), ('rstd', t)], writes=[('xn', b)])
                for c in range(8):
                    bk = 3 + 2 * b + c // 4
                    P.op('pe', (lambda b, bk, c: lambda e: e.transpose(PS[bk][:, (c % 4) * 128:(c % 4 + 1) * 128], xn[b][:, c * 128:(c + 1) * 128], ident_f[:]))(b, bk, c),
                         reads=[('xn', b), 'ident_f'], writes=[psr(bk)])
                for c in range(8):
                    bk = 3 + 2 * b + c // 4
                    if c // 4 == 0:
                        P.op('act', (lambda bk, c, t: lambda e: e.activation(out=hT[:, c, t * 128:(t + 1) * 128], in_=PS[bk][:, (c % 4) * 128:(c % 4 + 1) * 128], func=AF.Identity, scale=a1[:, c:c + 1], bias=modT[:, c:c + 1]))(bk, c, t),
                             reads=[psr(bk), 'a1', ('modT', 0)], writes=[('hT', c, t)])
                    else:
                        P.op('dve', (lambda bk, c, t: lambda e: e.tensor_scalar(hT[:, c, t * 128:(t + 1) * 128], PS[bk][:, (c % 4) * 128:(c % 4 + 1) * 128], a1[:, c:c + 1], modT[:, c:c + 1], ALU.mult, ALU.add))(bk, c, t),
                             reads=[psr(bk), 'a1', ('modT', 0)], writes=[('hT', c, t)])
            for i_ in range(8):
                ada_block(4 + i_)
                norm_tile(2 * i_)
                norm_tile(2 * i_ + 1)
            P.op('dve', lambda e: e.tensor_copy(modT[:, 16:48], PS[0][:, 16:48]), reads=[psr(0)], writes=[('modT', 1)])
            P.op('dve', lambda e: e.scalar_tensor_tensor(bc3[:, 2, :], bc3[:, 2, :], 1.0, gffn[:], ALU.add, ALU.mult), reads=[('bc4', 2), 'gffn'], writes=[('bc4', 2)])
            dump('modT', modT[:], [('modT', 0), ('modT', 1)])
            P.emit_phase()

        def hT_reads(c, Q):
            return [('hT', c, 4 * Q + i) for i in range(4)]

        if stop_after == 'A':
            with contextlib.ExitStack() as ph:
                tmp = ph.enter_context(nc.sbuf_tensor('dbgtmp', [128, S], F32, side='left'))
                for c in range(8):
                    P.op('dve', (lambda c: lambda e: e.tensor_copy(tmp[:], hT[:, c, :]))(c), reads=[('hT', c, t) for t in range(NT)], writes=['dbgtmp'])
                    P.op('sp', (lambda c: lambda e: e.dma_start(out=dbg_d['hT'][c], in_=tmp[:]))(c), reads=['dbgtmp'], dma='dbg')
                P.emit_phase()
            P.final_wait_all_dma()
            hstack.close()
            mixer.close()
            bcst.close()
            return nc

        with contextlib.ExitStack() as ph:
            def sb(name, shape, dt):
                return ph.enter_context(nc.sbuf_tensor('s_' + name, list(shape), dt, side='left'))
            qT = sb('qT', [128, 2, 4, S], BF16)
            kT = sb('kT', [128, 4, S], BF16)
            vaug = sb('vaug', [128, NT, 4, 130], BF16)
            biasb = sb('biasb', [128, 3, 4, 128], BF16)
            cfar = sb('cfar', [128, 8], F32)
            constblk = sb('constblk', [128, 2, 4, 128], BF16)
            lamqk = sb('lamqk', [128, 256], F32)
            lprod = sb('lprod', [128, 256], F32)
            lsum = sb('lsum', [128, 4], F32)
            lamneg = sb('lamneg', [128, 1], F32)
            g08 = sb('g08', [128, 128], F32)
            PT = [sb('PT%d' % i, [128, 512], BF16) for i in range(4)]
            accsb = [sb('accsb%d' % i, [128, 8, 129], F32) for i in range(2)]
            rr = [sb('rr%d' % i, [128, 16], F32) for i in range(2)]
            t0b = [sb('t0b%d' % i, [128, 128], F32) for i in range(2)]
            attb = [sb('attb%d' % i, [128, 128], F32) for i in range(4)]
            attn = [sb('attn%d' % i, [128, 128], F32) for i in range(4)]
            junk = sb('junkb', [128, 128], F32)
            ssa = sb('ssa', [128, 64], F32)
            msa = sb('msa', [128, 64], F32)
            sqa = sb('sqa', [128, 64], F32)
            rsa = sb('rsa', [128, 64], F32)

            P.op('pool', lambda e: e.dma_start(out=biasb[:], in_=biasblk_d), writes=['biasb'], dma='cstB')
            P.op('sp', lambda e: e.dma_start(out=cfar[:], in_=cfar_d), writes=['cfar'], dma='cst')
            P.op('sp', lambda e: e.dma_start(out=lamqk[:], in_=lamqk_d), writes=['lamqk'], dma='cst')
            P.op('sp', lambda e: e.dma_start(out=g08[:], in_=subln_d), writes=['g08'], dma='cst')
            P.op('dve', lambda e: e.tensor_tensor(lprod[:, 0:64], lamqk[:, 0:64], lamqk[:, 64:128], ALU.mult), reads=['lamqk'], writes=['lprod'])
            P.op('dve', lambda e: e.tensor_tensor(lprod[:, 64:128], lamqk[:, 128:192], lamqk[:, 192:256], ALU.mult), reads=['lamqk', 'lprod'], writes=['lprod'])
            P.op('dve', lambda e: e.reduce_sum(lsum[:, 0:1], lprod[:, 0:64], axis=AX.X), reads=['lprod'], writes=['lsum'])
            P.op('dve', lambda e: e.reduce_sum(lsum[:, 1:2], lprod[:, 64:128], axis=AX.X), reads=['lprod', 'lsum'], writes=['lsum'])
            P.op('act', lambda e: e.activation(out=lsum[:, 2:4], in_=lsum[:, 0:2], func=AF.Exp), reads=['lsum'], writes=['lsum'])
            P.op('dve', lambda e: e.tensor_tensor(lamneg[:], lsum[:, 3:4], lsum[:, 2:3], ALU.subtract), reads=['lsum'], writes=['lamneg'])
            P.op('dve', lambda e: e.tensor_scalar(lamneg[:], lamneg[:], -LAM_INIT, None, ALU.add), reads=['lamneg'], writes=['lamneg'])
            P.op('dve', lambda e: e.tensor_scalar(g08[:], g08[:], 1.0 - LAM_INIT, None, ALU.mult), reads=['g08'], writes=['g08'])
            P.op('dve', lambda e: e.memset(vaug[:, :, :, 128:130], 1.0), writes=['vones'])
            for k_ in range(2):
                for h_ in range(4):
                    P.op('dve', (lambda k_, h_: lambda e: e.tensor_copy(constblk[:, k_, h_, :], cfar[:, k_ * 4 + h_:k_ * 4 + h_ + 1].to_broadcast([128, 128])))(k_, h_),
                         reads=['cfar'], writes=[('constblk', k_, h_)])
            P.op('dve', lambda e: e.memset(ssa[:], 0.0), writes=[('ssa', c) for c in range(64)])

            wq, wk, wv = wp_next(), wp_next(), wp_next()
            load_w(wq, win_d[:, 0:512], 512)
            load_w(wk, win_d[:, 512:1024], 512)
            load_w(wv, win_d[:, 1024:1536], 512)
            ev = [0]
            P.op('pool', lambda e: e.memset(qT[64:128, 0, :, :], 0.0), writes=['qTz0'])
            P.op('pool', lambda e: e.memset(qT[0:64, 1, :, :], 0.0), writes=['qTz1'])
            def proj_unit(dname, wi, h, Q, bk, eng):
                for c in range(8):
                    P.op('pe', (lambda c: lambda e: e.matmul(PS[bk][:, :], WP[wi][:, c, h * 128:(h + 1) * 128], hT[:, c, Q * 512:(Q + 1) * 512], start=(c == 0), stop=(c == 7)))(c),
                         reads=[('wp', wi)] + hT_reads(c, Q), writes=[psr(bk)])
                if dname == 'kT':
                    if eng == 'act':
                        P.op('act', lambda e: e.activation(out=kT[:, h, Q * 512:(Q + 1) * 512], in_=PS[bk][:, :], func=AF.Copy), reads=[psr(bk)], writes=[('kT', h, Q)])
                    else:
                        P.op('dve', lambda e: e.tensor_copy(kT[:, h, Q * 512:(Q + 1) * 512], PS[bk][:, :]), reads=[psr(bk)], writes=[('kT', h, Q)])
                else:
                    for m in range(2):
                        ps_ = slice(m * 64, (m + 1) * 64)
                        if eng == 'act':
                            P.op('act', (lambda m, ps_: lambda e: e.activation(out=qT[ps_, m, h, Q * 512:(Q + 1) * 512], in_=PS[bk][ps_, :], func=AF.Copy, scale=0.125))(m, ps_),
                                 reads=[psr(bk), 'qTz0', 'qTz1'], writes=[('qT', h, Q, m)])
                        else:
                            P.op('dve', (lambda m, ps_: lambda e: e.tensor_scalar(qT[ps_, m, h, Q * 512:(Q + 1) * 512], PS[bk][ps_, :], 0.125, None, ALU.mult))(m, ps_),
                                 reads=[psr(bk), 'qTz0', 'qTz1'], writes=[('qT', h, Q, m)])

            for t in range(NT):
                bk = ev[0] % 2
                for c in range(8):
                    P.op('pe', (lambda bk, c, t: lambda e: e.matmul(PS[bk][:, :], hT[:, c, t * 128:(t + 1) * 128], WP[wv][:, c, :], start=(c == 0), stop=(c == 7)))(bk, c, t),
                         reads=[('wp', wv), ('hT', c, t)], writes=[psr(bk)])
                eng = 'act' if ev[0] % 2 == 0 else 'dve'
                if eng == 'act':
                    P.op('act', (lambda bk, t: lambda e: e.activation(out=vaug[:, t, :, 0:128], in_=PS[bk][:, :].rearrange("p (h d) -> p h d", h=4), func=AF.Copy))(bk, t),
                         reads=[psr(bk)], writes=[('v', t)])
                else:
                    P.op('dve', (lambda bk, t: lambda e: e.tensor_copy(vaug[:, t, :, 0:128], PS[bk][:, :].rearrange("p (h d) -> p h d", h=4)))(bk, t),
                         reads=[psr(bk)], writes=[('v', t)])
                ev[0] += 1

            for Q in range(4):
                for dname, wi in (('qT', wq), ('kT', wk)):
                    proj_unit(dname, wi, 0, Q, ev[0] % 2, 'act' if ev[0] % 2 == 0 else 'dve')
                    ev[0] += 1
            pending_proj = {h_: [(dname, wi, h_, Q) for Q in range(4) for dname, wi in (('qT', wq), ('kT', wk))] for h_ in range(1, 4)}

            ACCB = (2, 3, 4)
            steps = [(h, Q, j, m) for h in range(4) for Q in range(4) for j in range(NT) for m in range(2)]
            LA = 3
            SBK = (1, 5, 6, 7)

            def cats_of(Q, j):
                cats = []
                for qi in range(4):
                    d = j - (4 * Q + qi)
                    cats.append('n' if abs(d) <= 1 else ('lo' if d < 0 else 'hi'))
                return cats

            def emit_qk(idx):
                h, Q, j, m = steps[idx]
                sbk = SBK[idx % 4]
                cats = cats_of(Q, j)
                nnear = sum(1 for cc in cats if cc == 'n')
                P.op('pe', (lambda sbk, m, h, j, Q, nnear: lambda e: e.matmul(PS[sbk][:, :], kT[:, h, j * 128:(j + 1) * 128], qT[:, m, h, Q * 512:(Q + 1) * 512], start=True, stop=(nnear == 0)))(sbk, m, h, j, Q, nnear),
                     reads=[('kT', h, j // 4), ('qT', h, Q, m), 'qTz0', 'qTz1'], writes=[psr(sbk)])
                if nnear:
                    for qi in range(4):
                        last = (qi == 3)
                        if cats[qi] == 'n':
                            d = j - (4 * Q + qi)
                            P.op('pe', (lambda sbk, qi, d, h, last: lambda e: e.matmul(PS[sbk][:, qi * 128:(qi + 1) * 128], ident_b[:], biasb[:, d + 1, h, :], start=False, stop=last))(sbk, qi, d, h, last),
                                 reads=['biasb', 'ident_b'], writes=[psr(sbk)])
                        else:
                            k_ = 0 if cats[qi] == 'lo' else 1
                            P.op('pe', (lambda sbk, qi, k_, h, last: lambda e: e.matmul(PS[sbk][:, qi * 128:(qi + 1) * 128], ident_b[:], constblk[:, k_, h, :], start=False, stop=last))(sbk, qi, k_, h, last),
                                 reads=[('constblk', k_, h), 'ident_b'], writes=[psr(sbk)])

            def emit_exp_pv(idx):
                h, Q, j, m = steps[idx]
                sbk = SBK[idx % 4]
                pb = idx % 4
                cats = cats_of(Q, j)
                if any(cc == 'n' for cc in cats):
                    cats = ['n'] * 4
                r0 = 0
                ri = 0
                while r0 < 4:
                    r1 = r0
                    while r1 < 4 and cats[r1] == cats[r0]:
                        r1 += 1
                    cat = cats[r0]
                    if cat == 'n':
                        P.op('act', (lambda pb, sbk, r0, r1: lambda e: e.activation(out=PT[pb][:, r0 * 128:r1 * 128], in_=PS[sbk][:, r0 * 128:r1 * 128], func=AF.Exp))(pb, sbk, r0, r1),
                             reads=[psr(sbk)], writes=[('PT', pb, ri)])
                    else:
                        ci = h if cat == 'lo' else 4 + h
                        P.op('act', (lambda pb, sbk, r0, r1, ci: lambda e: e.activation(out=PT[pb][:, r0 * 128:r1 * 128], in_=PS[sbk][:, r0 * 128:r1 * 128], func=AF.Exp, bias=cfar[:, ci:ci + 1]))(pb, sbk, r0, r1, ci),
                             reads=[psr(sbk), 'cfar'], writes=[('PT', pb, ri)])
                    r0 = r1
                    ri += 1
                if j == 0 and m == 0:
                    for bi_, bkk_ in enumerate(ACCB):
                        n_ = 3 if bi_ < 2 else 2
                        P.op('pe', (lambda bkk_, n_: lambda e: e.matmul(PS[bkk_][:, 0:n_ * 129], zeros_b[:, 0:128], zeros_b[:, 0:n_ * 129], start=True, stop=False))(bkk_, n_),
                             reads=['zeros_b'], writes=[psr(bkk_)])
                for qi in range(4):
                    a = m * 4 + qi
                    ab_, ao = ACCB[a // 3], (a % 3) * 129
                    P.op('pe', (lambda ab_, ao, pb, qi, j, h, a: lambda e: e.matmul(PS[ab_][:, ao:ao + 129], PT[pb][:, qi * 128:(qi + 1) * 128], vaug[:, j, h, 0:129], start=False, stop=(j == NT - 1 and (a % 3 == 2 or a == 7))))(ab_, ao, pb, qi, j, h, a),
                         reads=[('PT', pb, 0), ('PT', pb, 1), ('PT', pb, 2), ('v', j), 'vones'], writes=[psr(ab_)])
                if j == NT - 1 and m == 1:
                    epilogue(h, Q, (h * 4 + Q))

            deferred = {}
            cur_idx = [0]

            def defer(at, fn):
                deferred.setdefault(at, []).append(fn)

            def epilogue(h, Q, it):
                ai = it % 2
                cur = cur_idx[0]
                accr = [('accsb', ai, bi) for bi in range(3)]
                for bi, bkk in enumerate(ACCB):
                    n = 3 if bi < 2 else 2
                    P.op('dve', (lambda ai, bi, bkk, n: lambda e: e.tensor_copy(accsb[ai][:, bi * 3:bi * 3 + n, :], PS[bkk][:, 0:n * 129].rearrange("p (a d) -> p a d", a=n)))(ai, bi, bkk, n),
                         reads=[psr(bkk)], writes=[('accsb', ai, bi)])
                P.op('dve', (lambda ai: lambda e: e.reciprocal(rr[ai][:, 0:8], accsb[ai][:, :, 128]))(ai), reads=accr, writes=[('rr', ai)])
                P.op('dve', (lambda ai: lambda e: e.tensor_scalar(rr[ai][:, 8:12], rr[ai][:, 4:8], lamneg[:, 0:1], None, ALU.mult))(ai), reads=[('rr', ai), 'lamneg'], writes=[('rr', ai)])
                for qi in range(4):
                    col = it * 4 + qi
                    P.op('dve', (lambda ai, qi: lambda e: e.tensor_scalar(t0b[qi % 2][:], accsb[ai][:, qi, 0:128], rr[ai][:, qi:qi + 1], None, ALU.mult))(ai, qi),
                         reads=accr + [('rr', ai)], writes=[('t0b', qi % 2)])
                    P.op('dve', (lambda ai, qi: lambda e: e.scalar_tensor_tensor(attb[qi][:], accsb[ai][:, 4 + qi, 0:128], rr[ai][:, 8 + qi:9 + qi], t0b[qi % 2][:], ALU.mult, ALU.add))(ai, qi),
                         reads=accr + [('rr', ai), ('t0b', qi % 2)], writes=[('attb', qi)])
                    P.op('dve', (lambda qi: lambda e: e.tensor_tensor(junk[:], attb[qi][:], attb[qi][:], ALU.mult))(qi), reads=[('attb', qi)], writes=['junkb'])
                    P.op('dve', (lambda col: lambda e: e.reduce_sum(ssa[:, col:col + 1], junk[:], axis=AX.X))(col), reads=['junkb'], writes=[('ssa', col)])
                sl = slice(it * 4, it * 4 + 4)
                P.op('dve', (lambda sl: lambda e: e.tensor_scalar(msa[:, sl], ssa[:, sl], 1.0 / 128, EPS, ALU.mult, ALU.add))(sl), reads=[('ssa', it * 4 + q_) for q_ in range(4)], writes=[('msa', it)])

                def st2():
                    P.op('act', (lambda sl: lambda e: e.activation(out=sqa[:, sl], in_=msa[:, sl], func=AF.Ln))(sl), reads=[('msa', it)], writes=[('sqa', it)])
                    P.op('act', (lambda sl: lambda e: e.activation(out=rsa[:, sl], in_=sqa[:, sl], func=AF.Exp, scale=-0.5))(sl), reads=[('sqa', it)], writes=[('rsa', it)])

                def st3():
                    for qi in range(4):
                        col = it * 4 + qi
                        P.op('dve', (lambda qi, col: lambda e: e.scalar_tensor_tensor(attn[qi][:], attb[qi][:], rsa[:, col:col + 1], g08[:], ALU.mult, ALU.mult))(qi, col),
                             reads=[('attb', qi), ('rsa', it), 'g08'], writes=[('attn', qi)])

                def st4():
                    tb = 0
                    for qi in range(4):
                        P.op('pe', (lambda tb, qi: lambda e: e.transpose(PS[tb][:, qi * 128:(qi + 1) * 128], attn[qi][:], ident_f[:]))(tb, qi),
                             reads=[('attn', qi), 'ident_f'], writes=[psr(tb)])

                def st5():
                    tb = 0
                    P.op('dve', (lambda tb, h, Q: lambda e: e.tensor_copy(mixT[:, h, Q * 512:(Q + 1) * 512], PS[tb][:, :]))(tb, h, Q),
                         reads=[psr(tb)], writes=[('mixT', h, 4 * Q + q_) for q_ in range(4)])
                defer(cur + 14, st2)
                defer(cur + 18, st3)
                defer(cur + 22, st4)
                defer(cur + 26, st5)

            for idx in range(len(steps) + LA + 32):
                cur_idx[0] = idx
                if idx < len(steps):
                    emit_qk(idx)
                    h_cur = steps[idx][0]
                    if (idx % 128) % 16 == 8 and h_cur + 1 < 4 and pending_proj[h_cur + 1]:
                        proj_unit(*pending_proj[h_cur + 1].pop(0), 0, 'dve')
                if LA <= idx < len(steps) + LA:
                    emit_exp_pv(idx - LA)
                for fn in deferred.pop(idx, []):
                    fn()
            assert not deferred
            P.emit_phase()

        if stop_after == 'B':
            with contextlib.ExitStack() as ph:
                tmp = ph.enter_context(nc.sbuf_tensor('dbgtmp', [128, S], F32, side='left'))
                for c in range(4):
                    P.op('dve', (lambda c: lambda e: e.tensor_copy(tmp[:], mixT[:, c, :]))(c), reads=[('mixT', c, t) for t in range(NT)], writes=['dbgtmp'])
                    P.op('sp', (lambda c: lambda e: e.dma_start(out=dbg_d['mixT'][c], in_=tmp[:]))(c), reads=['dbgtmp'], dma='dbg')
                P.emit_phase()
            P.final_wait_all_dma()
            hstack.close()
            mixer.close()
            bcst.close()
            return nc

        cst = contextlib.ExitStack()

        def sbC(name, shape, dt):
            return cst.enter_context(nc.sbuf_tensor('s_' + name, list(shape), dt, side='right'))
        BTm = sbC('BTm', [128, 2, S], BF16)
        CTm = sbC('CTm', [128, 2, S], BF16)
        xs_tok = sbC('xs_tok', [128, NT, 512], BF16)
        B_tok = sbC('B_tok', [128, NT, 128], BF16)
        zs = sbC('zs', [128, NT, 512], BF16)
        dt = sbC('dt', [128, 256], F32)
        lndt = sbC('lndt', [128, 256], F32)
        dA = sbC('dA', [128, 256], F32)
        csall = sbC('csall', [128, 512], F32)
        expT = sbC('expT', [128, 256], F32)
        bias_fb = sbC('bias_fb', [128, 256], F32)
        sdec = sbC('sdec', [128, 256], F32)
        eoff = sbC('eoff', [128, 256], F32)
        dskip = sbC('dskip', [128, 8], F32)
        ssmg = sbC('ssmg', [128, 512], F32)

        def v3(ap, n):
            return ap.rearrange("p (t k) -> p t k", t=NT)

        with contextlib.ExitStack() as ph:
            def sb(name, shape, dt_):
                return ph.enter_context(nc.sbuf_tensor('s_' + name, list(shape), dt_, side='left'))
            raw2 = [sb('raw%d' % i, [128, S + 4], F32) for i in range(2)]
            cv = sb('cv', [128, S], F32)
            scr4 = sb('scr4', [128, 1024], F32)
            xsT = [scr4[:, :].bitcast(BF16)] * 2
            convw = sb('convw', [128, 6, 5], F32)
            convb = sb('convb', [128, 6], F32)
            dtb = sb('dtb', [128, 256], F32)
            alog = sb('alog', [128, 256], F32)
            dtx = scr4[:, 0:256]
            tA = scr4[:, 256:512]
            tB = scr4[:, 512:768]
            tC = scr4[:, 768:1024]
            csx = cv[:, 0:512]

            for dst, src, key in ((convw, convw_d, 'convw'), (convb, convb_d, 'convb'), (dtb, dtb_d, 'dtb'), (alog, alog_d, 'alog'),
                                  (dskip, dskip_d, 'dskip'), (ssmg, ssmg_d, 'ssmg')):
                P.op('sp', (lambda dst, src: lambda e: e.dma_start(out=dst[:], in_=src))(dst, src), writes=[key], dma='cst')
            wz, wx, wb5 = wp_next(), wp_next(), wp_next()
            load_w(wz, win_d[:, 1536:2048], 512)
            load_w(wx, win_d[:, 2048:2560], 512)
            load_w(wb5, win_d[:, 2560:2832], 272)
            for rb_ in range(2):
                P.op('dve', (lambda rb_: lambda e: e.memset(raw2[rb_][:, 0:2], 0.0))(rb_), writes=[('rawpadL', rb_)])
                P.op('dve', (lambda rb_: lambda e: e.memset(raw2[rb_][:, S + 2:S + 4], 0.0))(rb_), writes=[('rawpadR', rb_)])
            ev = [0]
            for t in range(NT):
                bk = ev[0] % 2
                ev[0] += 1
                for c in range(8):
                    P.op('pe', (lambda bk, c, t: lambda e: e.matmul(PS[bk][:, :], hT[:, c, t * 128:(t + 1) * 128], WP[wz][:, c, :], start=(c == 0), stop=(c == 7)))(bk, c, t),
                         reads=[('wp', wz), ('hT', c, t)], writes=[psr(bk)])
                P.op('act', (lambda bk, t: lambda e: e.activation(out=zs[:, t, :], in_=PS[bk][:, :], func=AF.Silu))(bk, t), reads=[psr(bk)], writes=[('zs', t)])
            for t in range(NT):
                for c in range(8):
                    P.op('pe', (lambda c, t: lambda e: e.matmul(PS[2][:, t * 16:(t + 1) * 16], hT[:, c, t * 128:(t + 1) * 128], WP[wb5][:, c, 256:272], start=(c == 0), stop=(c == 7)))(c, t),
                         reads=[('wp', wb5), ('hT', c, t)], writes=[psr(2)])
            P.op('dve', lambda e: e.tensor_tensor(dtx[:], PS[2][:, 0:256], dtb[:], ALU.add), reads=[psr(2), 'dtb'], writes=['dtx'])
            P.op('act', lambda e: e.activation(out=tA[:], in_=dtx[:], func=AF.Abs), reads=['dtx'], writes=['tA'])
            P.op('act', lambda e: e.activation(out=tB[:], in_=tA[:], func=AF.Exp, scale=-1.0), reads=['tA'], writes=['tB'])
            P.op('act', lambda e: e.activation(out=tC[:], in_=tB[:], func=AF.Ln, bias=1.0), reads=['tB'], writes=['tC'])
            P.op('dve', lambda e: e.scalar_tensor_tensor(dt[:], dtx[:], 0.0, tC[:], ALU.max, ALU.add), reads=['dtx', 'tC'], writes=['dt'])
            P.op('act', lambda e: e.activation(out=lndt[:], in_=dt[:], func=AF.Ln), reads=['dt'], writes=['lndt'])
            P.op('act', lambda e: e.activation(out=tA[:], in_=alog[:], func=AF.Exp), reads=['alog', 'tA'], writes=['tA2'])
            P.op('dve', lambda e: e.scalar_tensor_tensor(dA[:], tA[:], -1.0, dt[:], ALU.mult, ALU.mult), reads=['tA2', 'dt'], writes=['dA'])
            dA3 = dA[:].rearrange("p (t k) -> p t k", t=NT)
            for t in range(NT):
                for kind, (tri, key, off) in enumerate(((tri_ip, 'tri_ip', 0), (tri_es, 'tri_es', 0), (tri_is, 'tri_is', 8), (tri_ep, 'tri_ep', 8))):
                    P.op('pe', (lambda t, kind, tri, off: lambda e: e.matmul(PS[3][:, t * 32 + kind * 8:t * 32 + kind * 8 + 8], tri[:], dA3[:, t, off:off + 8], start=True, stop=True))(t, kind, tri, off),
                         reads=['dA', key], writes=[psr(3)])
                P.op('pe', (lambda t: lambda e: e.matmul(PS[4][:, t * 16:(t + 1) * 16], ones_f[:], dA3[:, t, :], start=True, stop=True))(t),
                     reads=['dA', 'ones_f'], writes=[psr(4)])
            P.op('dve', lambda e: e.tensor_copy(csall[:], PS[3][:, :]), reads=[psr(3)], writes=['csall'])
            P.op('act', lambda e: e.activation(out=expT[:], in_=PS[4][:, 0:256], func=AF.Exp), reads=[psr(4)], writes=['expT'])
            cs4 = csall[:].rearrange("p (t k r) -> p t k r", t=NT, k=4)
            dt4 = dt[:].rearrange("p (t d r) -> p t d r", t=NT, d=2)
            ln4 = lndt[:].rearrange("p (t d r) -> p t d r", t=NT, d=2)
            bf4 = bias_fb[:].rearrange("p (t d r) -> p t d r", t=NT, d=2)
            sd4 = sdec[:].rearrange("p (t d r) -> p t d r", t=NT, d=2)
            eo4 = eoff[:].rearrange("p (t d r) -> p t d r", t=NT, d=2)
            cx4 = csx[:].rearrange("p (t k r) -> p t k r", t=NT, k=4)
            P.op('act', lambda e: e.activation(out=csx[:], in_=csall[:], func=AF.Exp), reads=['csall'], writes=['csx'])
            for d_, kcs, kst in ((0, 0, 1), (1, 2, 3)):
                P.op('dve', (lambda d_, kcs: lambda e: e.tensor_tensor(bf4[:, :, d_, :], ln4[:, :, d_, :], cs4[:, :, kcs, :], ALU.subtract))(d_, kcs), reads=['lndt', 'csall'], writes=[('bias_fb', d_)])
                P.op('dve', (lambda d_, kst: lambda e: e.tensor_tensor(sd4[:, :, d_, :], cx4[:, :, kst, :], dt4[:, :, d_, :], ALU.mult))(d_, kst), reads=['csx', 'dt'], writes=[('sdec', d_)])
                P.op('dve', (lambda d_, kcs: lambda e: e.tensor_copy(eo4[:, :, d_, :], cx4[:, :, kcs, :]))(d_, kcs), reads=['csx'], writes=[('eoff', d_)])
            def xbc_p(cc):
                raw = raw2[cc % 2]
                rkey = ('raw', cc % 2)
                wi = wx if cc < 4 else wb5
                off = (cc % 4) * 128 if cc < 4 else (cc - 4) * 128
                for Q in range(4):
                    bk = ev[0] % 2
                    ev[0] += 1
                    for c in range(8):
                        P.op('pe', (lambda bk, wi, off, c, Q: lambda e: e.matmul(PS[bk][:, :], WP[wi][:, c, off:off + 128], hT[:, c, Q * 512:(Q + 1) * 512], start=(c == 0), stop=(c == 7)))(bk, wi, off, c, Q),
                             reads=[('wp', wi)] + hT_reads(c, Q), writes=[psr(bk)])
                    P.op('act', (lambda bk, Q, raw: lambda e: e.activation(out=raw[:, 2 + Q * 512:2 + (Q + 1) * 512], in_=PS[bk][:, :], func=AF.Copy))(bk, Q, raw), reads=[psr(bk)], writes=[rkey])

            def xbc_c(cc):
                raw = raw2[cc % 2]
                rkey = ('raw', cc % 2)
                wi = wx if cc < 4 else wb5
                off = (cc % 4) * 128 if cc < 4 else (cc - 4) * 128
                P.op('dve', (lambda cc, raw: lambda e: e.tensor_scalar(cv[:], raw[:, 0:S], convw[:, cc, 0:1], None, ALU.mult))(cc, raw), reads=[rkey, ('rawpadL', cc % 2), ('rawpadR', cc % 2), 'convw'], writes=['cv', 'csx'])
                for j in range(1, 5):
                    P.op('dve', (lambda cc, j, raw: lambda e: e.scalar_tensor_tensor(cv[:], raw[:, j:j + S], convw[:, cc, j:j + 1], cv[:], ALU.mult, ALU.add))(cc, j, raw), reads=[rkey, 'cv', 'convw'], writes=['cv'])

            def xbc_s(cc):
                raw = raw2[cc % 2]
                rkey = ('raw', cc % 2)
                wi = wx if cc < 4 else wb5
                off = (cc % 4) * 128 if cc < 4 else (cc - 4) * 128
                if cc < 4:
                    xb = cc % 2
                    P.op('act', (lambda xb, cc: lambda e: e.activation(out=xsT[xb][:], in_=cv[:], func=AF.Silu, bias=convb[:, cc:cc + 1]))(xb, cc), reads=['cv', 'convb'], writes=[('xsT', 0), 'dtx', 'tA', 'tB', 'tC', 'tA2'])
                    for t in range(NT):
                        P.op('pe', (lambda xb, t, cc: lambda e: e.matmul(PS[5 + (t // 4) % 2][:, (t % 4) * 128:(t % 4 + 1) * 128], xsT[xb][:, t * 128:(t + 1) * 128], ident_b[:], start=True, stop=True))(xb, t, cc),
                             reads=[('xsT', 0), 'ident_b'], writes=[psr(5 + (t // 4) % 2)])
                        if t % 4 == 3:
                            t0_ = t - 3
                            P.op('dve', (lambda t0_, t, cc: lambda e: e.tensor_copy(xs_tok[:, t0_:t0_ + 4, cc * 128:(cc + 1) * 128], PS[5 + (t // 4) % 2][:, :].rearrange("p (a d) -> p a d", a=4)))(t0_, t, cc),
                                 reads=[psr(5 + (t // 4) % 2)], writes=[('xs_tok', cc)])
                else:
                    tgt, tkey = (BTm, 'BTm') if cc == 4 else (CTm, 'CTm')
                    P.op('act', (lambda cc, tgt: lambda e: e.activation(out=tgt[:, 0, :], in_=cv[:], func=AF.Silu, bias=convb[:, cc:cc + 1]))(cc, tgt), reads=['cv', 'convb'], writes=[(tkey, 0)])
                    if cc == 4:
                        for t in range(NT):
                            P.op('pe', (lambda t: lambda e: e.matmul(PS[5 + (t // 4) % 2][:, (t % 4) * 128:(t % 4 + 1) * 128], BTm[:, 0, t * 128:(t + 1) * 128], ident_b[:], start=True, stop=True))(t),
                                 reads=[('BTm', 0), 'ident_b'], writes=[psr(5 + (t // 4) % 2)])
                            if t % 4 == 3:
                                t0_ = t - 3
                                P.op('dve', (lambda t0_, t: lambda e: e.tensor_copy(B_tok[:, t0_:t0_ + 4, :], PS[5 + (t // 4) % 2][:, :].rearrange("p (a d) -> p a d", a=4)))(t0_, t),
                                     reads=[psr(5 + (t // 4) % 2)], writes=['B_tok'])
                    P.op('dve', (lambda tgt: lambda e: e.tensor_copy(tgt[64:128, 1, :], tgt[64:128, 0, :]))(tgt), reads=[(tkey, 0)], writes=[(tkey, 1)])
                    P.op('dve', (lambda tgt: lambda e: e.memset(tgt[0:64, 1, :], 0.0))(tgt), reads=[], writes=[(tkey, 1)])
                    P.op('dve', (lambda tgt: lambda e: e.memset(tgt[64:128, 0, :], 0.0))(tgt), reads=[], writes=[(tkey, 0)])

            xbc_p(0)
            for cc in range(6):
                if cc + 1 < 6:
                    xbc_p(cc + 1)
                xbc_c(cc)
                xbc_s(cc)
            P.emit_phase()
        hstack.close()
        if stop_after == 'C1':
            for nm, tl in (('dt', dt), ('dA', dA), ('csall', csall), ('bias_fb', bias_fb), ('eoff', eoff), ('sdec', sdec), ('expT', expT)):
                dump(nm, tl[:], [])
            P.emit_phase()
            P.final_wait_all_dma()
            cst.close()
            mixer.close()
            bcst.close()
            return nc

        with contextlib.ExitStack() as ph:
            def sb(name, shape, dt_):
                return ph.enter_context(nc.sbuf_tensor('s_' + name, list(shape), dt_, side='left'))
            Sin = sb('Sin', [128, 2, NT, 256], BF16)
            Srun = sb('Srun', [128, 2, 256], F32)
            Stmp = sb('Stmp', [128, 256], F32)
            Tdec = sb('Tdec', [128, NT, 2, 4], F32)
            Xd = sb('Xd', [128, 2, 512], BF16)
            DI = sb('DI', [128, 8, 128], BF16)
            dAbc = [sb('dAbc%d' % i, [128, 16, 128], F32) for i in range(2)]
            Lt = [sb('Lt%d' % i, [128, 16, 128], F32) for i in range(2)]
            Gsb = [sb('Gsb%d' % i, [128, 256], F32) for i in range(2)]
            Mt = Xd[:, :, :].rearrange("p a (b d) -> p (a b) d", b=4)
            ysb = sb('ysb', [128, 512], F32)
            ytmp = sb('ytmp', [128, 512], F32)
            junkc = sb('junkc', [128, 256], F32)
            ssdo = sb('ssdo', [128, 512], BF16)
            ssg = sb('ssg', [128, 32], F32)
            msg = sb('msg', [128, 32], F32)
            sqg = sb('sqg', [128, 32], F32)
            rsg = sb('rsg', [128, 32], F32)
            ex4 = expT[:].rearrange("p (t d r) -> p t d r", t=NT, d=2)
            sd3 = sdec[:].rearrange("p (t k) -> p t k", t=NT)
            bf3 = bias_fb[:].rearrange("p (t k) -> p t k", t=NT)
            eo3 = eoff[:].rearrange("p (t k) -> p t k", t=NT)
            dA3 = dA[:].rearrange("p (t k) -> p t k", t=NT)
            for d_ in range(2):
                P.op('dve', (lambda d_: lambda e: e.tensor_copy(Tdec[0:64, :, d_, :], ex4[0:64, :, d_, 0:4]))(d_), reads=['expT'], writes=['Tdec'])
                P.op('dve', (lambda d_: lambda e: e.tensor_copy(Tdec[64:128, :, d_, :], ex4[64:128, :, d_, 4:8]))(d_), reads=['expT'], writes=['Tdec'])
            for r in range(8):
                P.op('dve', (lambda r: lambda e: e.tensor_scalar(DI[:, r, :], ident_f[:], dskip[:, r:r + 1], None, ALU.mult))(r), reads=['ident_f', 'dskip'], writes=['DI'])
            P.op('dve', lambda e: e.memset(Srun[:], 0.0), writes=['Srun'])
            P.op('dve', lambda e: e.memset(Sin[:, 0, 0, :], 0.0), writes=[('Sin', 0, 0)])
            P.op('dve', lambda e: e.memset(Sin[:, 1, NT - 1, :], 0.0), writes=[('Sin', 1, NT - 1)])
            P.op('dve', lambda e: e.memset(ssg[:], 0.0), writes=['ssg'])
            for d_, order in ((0, range(0, NT - 1)), (1, range(NT - 1, 0, -1))):
                for t in order:
                    P.op('dve', (lambda d_, t: lambda e: e.tensor_tensor(Xd[:, d_, :].rearrange("p (r d) -> p r d", r=8), xs_tok[:, t, :].rearrange("p (r d) -> p r d", r=8),
                                                                      sd3[:, t, d_ * 8:(d_ + 1) * 8].unsqueeze(2).to_broadcast([128, 8, 64]), ALU.mult))(d_, t),
                         reads=[('xs_tok', c) for c in range(4)] + [('sdec', d_)], writes=[('Xd', d_)])
                    bk = 6 + d_
                    P.op('pe', (lambda bk, t, d_: lambda e: e.matmul(PS[bk][:, :], B_tok[:, t, :], Xd[:, d_, :], start=True, stop=True))(bk, t, d_),
                         reads=['B_tok', ('Xd', d_)], writes=[psr(bk)])
                    P.op('dve', (lambda d_, t: lambda e: e.tensor_tensor(Stmp[:].rearrange("p (r d) -> p r d", r=4), Srun[:, d_, :].rearrange("p (r d) -> p r d", r=4),
                                                                      Tdec[:, t, d_, :].unsqueeze(2).to_broadcast([128, 4, 64]), ALU.mult))(d_, t),
                         reads=['Srun', 'Tdec'], writes=['Stmp'])
                    P.op('dve', (lambda bk, d_: lambda e: e.tensor_tensor(Srun[0:64, d_, :], Stmp[0:64, :], PS[bk][0:64, 0:256], ALU.add))(bk, d_), reads=['Stmp', psr(bk)], writes=['Srun'])
                    P.op('dve', (lambda bk, d_: lambda e: e.tensor_tensor(Srun[64:128, d_, :], Stmp[64:128, :], PS[bk][64:128, 256:512], ALU.add))(bk, d_), reads=['Stmp', psr(bk)], writes=['Srun'])
                    tn = t + 1 if d_ == 0 else t - 1
                    P.op('act', (lambda d_, tn: lambda e: e.activation(out=Sin[:, d_, tn, :], in_=Srun[:, d_, :], func=AF.Copy))(d_, tn), reads=['Srun'], writes=[('Sin', d_, tn)])
            def stage_xa(t):
                db = t % 2
                P.op('dve', lambda e: e.tensor_copy(dAbc[db][:], dA3[:, t, :].unsqueeze(2).to_broadcast([128, 16, 128])), reads=['dA'], writes=[('dAbc', db)])

            def stage_xd(t, d_):
                db = t % 2
                if d_ == 0:
                    for g in range(2):
                        P.op('pe', (lambda g: lambda e: e.matmul(PS[0][:, g * 128:(g + 1) * 128], BTm[:, g, t * 128:(t + 1) * 128], CTm[:, g, t * 128:(t + 1) * 128], start=True, stop=True))(g),
                             reads=[('BTm', g), ('CTm', g)], writes=[psr(0)])
                    P.op('act', lambda e: e.activation(out=Gsb[db][:], in_=PS[0][:, 0:256], func=AF.Copy), reads=[psr(0)], writes=[('Gsb', db)])
                tri, tkey, ngm, nkey = (tri_ip, 'tri_ip', negm_f, 'negm_f') if d_ == 0 else (tri_is, 'tri_is', negm_b, 'negm_b')
                for r in range(8):
                    bk = 1 + d_ * 2 + r // 4
                    sl = slice((r % 4) * 128, (r % 4 + 1) * 128)
                    P.op('pe', (lambda bk, sl, r, tri: lambda e: e.matmul(PS[bk][:, sl], dAbc[db][:, d_ * 8 + r, :], tri[:], start=True, stop=False))(bk, sl, r, tri),
                         reads=[('dAbc', db), tkey], writes=[psr(bk)])
                    P.op('pe', (lambda bk, sl, ngm: lambda e: e.matmul(PS[bk][:, sl], ident_f[:], ngm[:], start=False, stop=True))(bk, sl, ngm),
                         reads=['ident_f', nkey], writes=[psr(bk)])
                for r in range(8):
                    bk = 1 + d_ * 2 + r // 4
                    sl = slice((r % 4) * 128, (r % 4 + 1) * 128)
                    P.op('act', (lambda bk, sl, r: lambda e: e.activation(out=Lt[db][:, d_ * 8 + r, :], in_=PS[bk][:, sl], func=AF.Exp, bias=bf3[:, t, d_ * 8 + r:d_ * 8 + r + 1]))(bk, sl, r),
                         reads=[psr(bk), ('bias_fb', d_)], writes=[('Lt', db, d_, r)])

            def stage_y1(t):
                db = t % 2
                P.op('dve', lambda e: e.tensor_tensor(Lt[db][:, 0:8, :], Lt[db][:, 0:8, :], Lt[db][:, 8:16, :], ALU.add), reads=[('Lt', db, dd, rr_) for dd in range(2) for rr_ in range(8)], writes=[('Lt', db, 0, rr_) for rr_ in range(8)])
                for g in range(2):
                    P.op('dve', (lambda g: lambda e: e.tensor_tensor(Mt[:, g * 4:(g + 1) * 4, :], Lt[db][:, g * 4:(g + 1) * 4, :], Gsb[db][:, g * 128:(g + 1) * 128].unsqueeze(1).to_broadcast([128, 4, 128]), ALU.mult))(g),
                         reads=[('Lt', db, 0, rr_) for rr_ in range(8)] + [('Gsb', db)], writes=[('Mt', g), ('Xd', 0), ('Xd', 1)])

            def stage_y2(t):
                for r in range(8):
                    P.op('pe', (lambda r: lambda e: e.matmul(PS[5][:, r * 64:(r + 1) * 64], Mt[:, r, :], xs_tok[:, t, r * 64:(r + 1) * 64], start=True, stop=False))(r),
                         reads=[('Mt', r // 4)] + [('xs_tok', c) for c in range(4)], writes=[psr(5)])
                    P.op('pe', (lambda r: lambda e: e.matmul(PS[5][:, r * 64:(r + 1) * 64], DI[:, r, :], xs_tok[:, t, r * 64:(r + 1) * 64], start=False, stop=True))(r),
                         reads=['DI'] + [('xs_tok', c) for c in range(4)], writes=[psr(5)])
                for d_ in range(2):
                    for g in range(2):
                        P.op('pe', (lambda d_, g: lambda e: e.matmul(PS[6 + d_][:, g * 256:(g + 1) * 256], CTm[:, g, t * 128:(t + 1) * 128], Sin[:, d_, t, :], start=True, stop=True))(d_, g),
                             reads=[('CTm', g), ('Sin', d_, t)], writes=[psr(6 + d_)])
                P.op('act', lambda e: e.activation(out=ysb[:], in_=PS[5][:, :], func=AF.Copy), reads=[psr(5)], writes=['ysb'])

            def stage_y3(t):
                for d_ in range(2):
                    P.op('dve', (lambda d_: lambda e: e.tensor_tensor(ytmp[:].rearrange("p (r d) -> p r d", r=8), PS[6 + d_][:, :].rearrange("p (r d) -> p r d", r=8),
                                                                   eo3[:, t, d_ * 8:(d_ + 1) * 8].unsqueeze(2).to_broadcast([128, 8, 64]), ALU.mult))(d_),
                         reads=[psr(6 + d_), ('eoff', d_)], writes=['ytmp'])
                    P.op('dve', lambda e: e.tensor_tensor(ysb[:], ysb[:], ytmp[:], ALU.add), reads=['ysb', 'ytmp'], writes=['ysb'])
                P.op('dve', lambda e: e.tensor_tensor(ysb[:], ysb[:], zs[:, t, :], ALU.mult), reads=['ysb', ('zs', t)], writes=['ysb'])
                for g in range(2):
                    col = t * 2 + g
                    P.op('dve', (lambda g: lambda e: e.tensor_tensor(junkc[:], ysb[:, g * 256:(g + 1) * 256], ysb[:, g * 256:(g + 1) * 256], ALU.mult))(g), reads=['ysb'], writes=['junkc'])
                    P.op('dve', (lambda col: lambda e: e.reduce_sum(ssg[:, col:col + 1], junkc[:], axis=AX.X))(col), reads=['junkc'], writes=[('ssg', col)])
                sl2 = slice(t * 2, t * 2 + 2)
                P.op('dve', lambda e: e.tensor_scalar(msg[:, sl2], ssg[:, sl2], 1.0 / 256, EPS, ALU.mult, ALU.add), reads=[('ssg', t * 2), ('ssg', t * 2 + 1)], writes=[('msg', t)])

            def stage_z1(t):
                sl2 = slice(t * 2, t * 2 + 2)
                P.op('act', lambda e: e.activation(out=sqg[:, sl2], in_=msg[:, sl2], func=AF.Ln), reads=[('msg', t)], writes=[('sqg', t)])
                P.op('act', lambda e: e.activation(out=rsg[:, sl2], in_=sqg[:, sl2], func=AF.Exp, scale=-0.5), reads=[('sqg', t)], writes=[('rsg', t)])
                for g in range(2):
                    col = t * 2 + g
                    P.op('dve', (lambda g, col: lambda e: e.scalar_tensor_tensor(ssdo[:, g * 256:(g + 1) * 256], ysb[:, g * 256:(g + 1) * 256], rsg[:, col:col + 1], ssmg[:, g * 256:(g + 1) * 256], ALU.mult, ALU.mult))(g, col),
                         reads=['ysb', ('rsg', t), 'ssmg'], writes=['ssdo'])

            def stage_z2(t):
                for cc in range(4):
                    P.op('pe', (lambda cc: lambda e: e.matmul(PS[0][:, cc * 128:(cc + 1) * 128], ssdo[:, cc * 128:(cc + 1) * 128], ident_b[:], start=True, stop=True))(cc),
                         reads=['ssdo', 'ident_b'], writes=[psr(0)])
                P.op('dve', lambda e: e.tensor_copy(mixT[:, 4:8, t * 128:(t + 1) * 128], PS[0][:, :].rearrange("p (a d) -> p a d", a=4)),
                     reads=[psr(0)], writes=[('mixT', 4 + c, t) for c in range(4)])

            NTc = NT if stop_after != 'C2a' else 0
            if NTc:
                stage_xa(0)
                stage_xd(0, 0)
                stage_xd(0, 1)
            for i in range(NTc + 1):
                if 1 <= i:
                    stage_z1(i - 1)
                if i + 1 < NTc:
                    stage_xa(i + 1)
                if i < NTc:
                    stage_y1(i)
                if i + 1 < NTc:
                    stage_xd(i + 1, 0)
                if 1 <= i:
                    stage_z2(i - 1)
                if i < NTc:
                    stage_y2(i)
                if i + 1 < NTc:
                    stage_xd(i + 1, 1)
                if i < NTc:
                    stage_y3(i)
            P.emit_phase()
        cst.close()
        if stop_after in ('C', 'C2a'):
            with contextlib.ExitStack() as ph:
                tmp = ph.enter_context(nc.sbuf_tensor('s_dbgtmp', [128, S], F32, side='left'))
                for c in range(8):
                    P.op('dve', (lambda c: lambda e: e.tensor_copy(tmp[:], mixT[:, c, :]))(c), reads=[('mixT', c, t) for t in range(NT)], writes=['dbgtmp'])
                    P.op('sp', (lambda c: lambda e: e.dma_start(out=dbg_d['mixT'][c], in_=tmp[:]))(c), reads=['dbgtmp'], dma='dbg')
                P.emit_phase()
            P.final_wait_all_dma()
            mixer.close()
            bcst.close()
            return nc

        xres = sbT('xres', [128, NT, D], F32)
        h2 = sbT('h2', [128, NT, D], BF16)
        aff = sbT('aff', [128, NT, 16], F32)
        sel = sbT('sel', [128, NT, 16], F32)
        pos = sbT('pos', [128, NT, 16], F32)
        with contextlib.ExitStack() as ph:
            def sb(name, shape, dt_):
                return ph.enter_context(nc.sbuf_tensor('s_' + name, list(shape), dt_, side='left'))
            tmpm = sb('tmpm', [128, 512], F32)
            xn2 = sb('xn2', [128, D], F32)
            h2f = [sb('h2f%d' % i, [128, D], F32) for i in range(2)]
            h2Tf = sb('h2Tf', [128, 8, 128], F32)
            rw = sb('rw', [128, 8, 16], F32)
            ss2 = sb('ss2', [128, 16], F32)
            ms2 = sb('ms2', [128, 16], F32)
            sq2 = sb('sq2', [128, 16], F32)
            rs2 = sb('rs2', [128, 16], F32)
            mx = sb('mx', [128, 16], F32)
            nmx = sb('nmx', [128, 16], F32)
            sme = sb('sme', [128, 16], F32)
            rsm = sb('rsm', [128, 16], F32)
            eaf = sb('eaf', [128, NT, 16], F32)
            P.op('sp', lambda e: e.dma_start(out=rw[:], in_=rw_d), writes=['rw'], dma='cst')
            wo = [wp_next(), wp_next()]
            for hf in range(2):
                load_w(wo[hf], wout_d[:, hf * 512:(hf + 1) * 512], 512)
            P.op('dve', lambda e: e.memset(ss2[:], 0.0), writes=['ss2'])
            P.op('dve', lambda e: e.memset(sme[:], 0.0), writes=['sme'])
            def d1_xpe(t):
                P.op('sp', (lambda t: lambda e: e.dma_start(out=xres[:, t, :], in_=x_d[t * 128:(t + 1) * 128, :]))(t), writes=[('xres', t)], dma=('xres', t))
                for hf in range(2):
                    bk = hf
                    for c in range(8):
                        P.op('pe', (lambda bk, hf, c: lambda e: e.matmul(PS[bk][:, :], mixT[:, c, t * 128:(t + 1) * 128], WP[wo[hf]][:, c, :], start=(c == 0), stop=(c == 7)))(bk, hf, c),
                             reads=[('wp', wo[hf]), ('mixT', c, t)], writes=[psr(bk)])

            def d1_xch(t):
                hb = t % 2
                for hf in range(2):
                    bk = hf
                    P.op('dve', (lambda bk, hf: lambda e: e.tensor_tensor(tmpm[:], PS[bk][:, :], bc3[:, 0, hf * 512:(hf + 1) * 512], ALU.mult))(bk, hf), reads=[psr(bk), ('bc4', 0)], writes=['tmpm'])
                    P.op('dve', (lambda hf: lambda e: e.tensor_tensor(xres[:, t, hf * 512:(hf + 1) * 512], xres[:, t, hf * 512:(hf + 1) * 512], tmpm[:], ALU.add))(hf), reads=['tmpm', ('xres', t)], writes=[('xres', t)])
                P.op('act', lambda e: e.activation(out=xn2[:], in_=xres[:, t, :], func=AF.Square, accum_out=ss2[:, t:t + 1]), reads=[('xres', t), 'ss2'], writes=['xn2', ('ss2', t)])
                P.op('dve', lambda e: e.tensor_scalar(ms2[:, t:t + 1], ss2[:, t:t + 1], 1.0 / D, EPS, ALU.mult, ALU.add), reads=[('ss2', t)], writes=[('ms2', t)])
                P.op('act', lambda e: e.activation(out=sq2[:, t:t + 1], in_=ms2[:, t:t + 1], func=AF.Sqrt), reads=[('ms2', t)], writes=[('sq2', t)])
                P.op('dve', lambda e: e.reciprocal(rs2[:, t:t + 1], sq2[:, t:t + 1]), reads=[('sq2', t)], writes=[('rs2', t)])
                P.op('dve', lambda e: e.scalar_tensor_tensor(xn2[:], xres[:, t, :], rs2[:, t:t + 1], bc3[:, 2, :], ALU.mult, ALU.mult), reads=[('xres', t), ('rs2', t), ('bc4', 2)], writes=['xn2'])
                P.op('dve', lambda e: e.tensor_tensor(h2f[hb][:], xn2[:], bc3[:, 1, :], ALU.add), reads=['xn2', ('bc4', 1)], writes=[('h2f', hb)])
                P.op('act', lambda e: e.activation(out=h2[:, t, :], in_=h2f[hb][:], func=AF.Copy), reads=[('h2f', hb)], writes=[('h2', t)])

            def d1_ytr(t):
                hb = t % 2
                for c in range(8):
                    bk = 2 + c // 4
                    P.op('pe', (lambda bk, c: lambda e: e.transpose(PS[bk][:, (c % 4) * 128:(c % 4 + 1) * 128], h2f[hb][:, c * 128:(c + 1) * 128], ident_f[:]))(bk, c), reads=[('h2f', hb), 'ident_f'], writes=[psr(bk)])
                P.op('act', lambda e: e.activation(out=h2Tf[:, 0:4, :], in_=PS[2][:, :].rearrange("p (a d) -> p a d", a=4), func=AF.Copy), reads=[psr(2)], writes=[('h2Tf', 0)])
                P.op('dve', lambda e: e.tensor_copy(h2Tf[:, 4:8, :], PS[3][:, :].rearrange("p (a d) -> p a d", a=4)), reads=[psr(3)], writes=[('h2Tf', 1)])

            def d1_yrt(t):
                for c in range(8):
                    P.op('pe', (lambda c: lambda e: e.matmul(PS[4][:, t * 16:(t + 1) * 16], h2Tf[:, c, :], rw[:, c, :], start=(c == 0), stop=(c == 7)))(c), reads=[('h2Tf', c // 4), 'rw'], writes=[psr(4)])

            d1_xpe(0)
            d1_xch(0)
            for t in range(NT):
                if t + 1 < NT:
                    d1_xpe(t + 1)
                d1_ytr(t)
                if t + 1 < NT:
                    d1_xch(t + 1)
                d1_yrt(t)
            lg3 = PS[4][:, 0:256].rearrange("p (t k) -> p t k", t=NT)
            P.op('dve', lambda e: e.reduce_max(mx[:], lg3, axis=AX.X), reads=[psr(4)], writes=['mx'])
            P.op('dve', lambda e: e.tensor_scalar(nmx[:], mx[:], -1.0, None, ALU.mult), reads=['mx'], writes=['nmx'])
            for t in range(NT):
                P.op('act', (lambda t: lambda e: e.activation(out=eaf[:, t, :], in_=PS[4][:, t * 16:(t + 1) * 16], func=AF.Exp, bias=nmx[:, t:t + 1], accum_out=sme[:, t:t + 1]))(t),
                     reads=[psr(4), 'nmx', 'sme'], writes=[('eaf', t), ('sme', t)])
            P.op('dve', lambda e: e.reciprocal(rsm[:], sme[:]), reads=[('sme', t) for t in range(NT)], writes=['rsm'])
            P.op('dve', lambda e: e.tensor_tensor(aff[:], eaf[:], rsm[:].unsqueeze(2).to_broadcast([128, NT, 16]), ALU.mult), reads=[('eaf', t) for t in range(NT)] + ['rsm'], writes=['aff'])
            dump('x2', xres[:, :, :], [('xres', t) for t in range(NT)])
            dump('aff', aff[:, :, :], ['aff'])
            P.emit_phase()
        mixer.close()
        bcst.close()

        gsel = sbT('gsel', [128, NT, 16], F32)
        wpx_stack = contextlib.ExitStack()
        NWX = 3
        for i_ in range(NWX):
            WP.append(wpx_stack.enter_context(nc.sbuf_tensor('s_wpx%d' % i_, [128, 8, 512], BF16, side='left')))
        with contextlib.ExitStack() as ph:
            def sb(name, shape, dt_):
                return ph.enter_context(nc.sbuf_tensor('s_' + name, list(shape), dt_, side='left'))
            if stop_after in (None, 'E', 'E1'):
                for fb_ in range(len(WP) // 2):
                    for kind_ in ('g', 'u'):
                        prefetched[(0, kind_, fb_)] = issue_w(0, kind_, fb_)
            affT = sb('affT', [16, S], F32)
            work = sb('work', [16, S], F32)
            m8 = sb('m8', [16, 8], F32)
            csel = sb('csel', [128, NT, 16], F32)
            for t in range(NT):
                bk = t // 4
                P.op('pe', (lambda bk, t: lambda e: e.transpose(PS[bk][0:16, (t % 4) * 128:(t % 4 + 1) * 128], aff[:, t, :], ident_f[:]))(bk, t), reads=['aff', 'ident_f'], writes=[psr(bk)])
            for bk in range(4):
                P.op('dve', (lambda bk: lambda e: e.tensor_copy(affT[:, bk * 512:(bk + 1) * 512], PS[bk][0:16, :]))(bk), reads=[psr(bk)], writes=['affT'])
            P.op('dve', lambda e: e.tensor_copy(work[:], affT[:]), reads=['affT'], writes=['work'])
            for it_ in range(CAP // 8):
                P.op('dve', lambda e: e.max(m8[:], work[:]), reads=['work'], writes=['m8'])
                if it_ < CAP // 8 - 1:
                    P.op('dve', lambda e: e.match_replace(work[:], m8[:], work[:], -1.0), reads=['work', 'm8'], writes=['work'])
            P.op('dve', lambda e: e.tensor_scalar(work[:], affT[:], m8[:, 7:8], None, ALU.is_ge), reads=['affT', 'm8', 'work'], writes=['work'])
            for t in range(NT):
                P.op('pe', (lambda t: lambda e: e.transpose(PS[4][:, t * 16:(t + 1) * 16], work[:, t * 128:(t + 1) * 128], ident_f[0:16, 0:16]))(t), reads=['work', 'ident_f'], writes=[psr(4)])
            P.op('dve', lambda e: e.tensor_copy(sel[:], PS[4][:, 0:256].rearrange("p (t k) -> p t k", t=NT)), reads=[psr(4)], writes=['sel'])
            P.op('dve', lambda e: e.memset(csel[:, 0, :], 0.0), writes=[('csel', 0)])
            for t in range(1, NT):
                P.op('dve', (lambda t: lambda e: e.tensor_tensor(csel[:, t, :], csel[:, t - 1, :], sel[:, t - 1, :], ALU.add))(t), reads=[('csel', t - 1), 'sel'], writes=[('csel', t)])
            for t in range(NT):
                P.op('pe', (lambda t: lambda e: e.matmul(PS[5][:, t * 16:(t + 1) * 16], tri_ep[:], sel[:, t, :], start=True, stop=False))(t), reads=['tri_ep', 'sel'], writes=[psr(5)])
                P.op('pe', (lambda t: lambda e: e.matmul(PS[5][:, t * 16:(t + 1) * 16], ones_f[:], csel[:, t, :], start=False, stop=True))(t), reads=['ones_f', ('csel', t)], writes=[psr(5)])
            P.op('dve', lambda e: e.tensor_copy(pos[:], PS[5][:, 0:256].rearrange("p (t k) -> p t k", t=NT)), reads=[psr(5)], writes=['pos'])
            P.op('dve', lambda e: e.tensor_tensor(gsel[:], aff[:], sel[:], ALU.mult), reads=['aff', 'sel'], writes=['gsel'])
            dump('sel', sel[:, :, :], ['sel'])
            dump('pos', pos[:, :, :], ['pos'])
            P.emit_phase()

        with contextlib.ExitStack() as ph:
            def sb(name, shape, dt_):
                return ph.enter_context(nc.sbuf_tensor('s_' + name, list(shape), dt_, side='left'))
            oh = [sb('oh%d' % i, [128, 256], BF16) for i in range(4)]
            ohg = [sb('ohg%d' % i, [128, 256], BF16) for i in range(4)]
            xgT = [sb('xgT0', [128, 8, 256], BF16)] * 2
            hact = sb('hact', [128, NFC, 256], BF16)
            ohT = [sb('ohT%d' % i, [128, 2, S], BF16) for i in range(2)]
            sg = [sb('sg%d' % i, [128, 256], F32) for i in range(2)]
            ye = sb('ye', [128, 2, D], BF16)
            print('E: sbuf bytes remaining after locals', nc.sbuf_bytes_remaining)
            nblk = [(i * 512, min(512, FF - i * 512)) for i in range(6)]
            ohc = [0]
            NE = NEXP if stop_after != 'E1' else 1

            def gather_units(ex):
                xb = ex % 2
                units = []
                obuf = {}

                def mk_oh(t):
                    ob = ohc[0] % 4
                    ohc[0] += 1
                    obuf[t] = ob
                    P.op('dve', (lambda ob: lambda e: e.tensor_scalar(oh[ob][:], iota_j[:], pos[:, t, ex:ex + 1], sel[:, t, ex:ex + 1], ALU.is_equal, ALU.mult))(ob),
                         reads=['iota_j', 'pos', 'sel'], writes=[('oh', ob)])
                    P.op('dve', (lambda ob: lambda e: e.tensor_scalar(ohg[ob][:], iota_j[:], pos[:, t, ex:ex + 1], gsel[:, t, ex:ex + 1], ALU.is_equal, ALU.mult))(ob),
                         reads=['iota_j', 'pos', 'gsel'], writes=[('ohg', ob)])

                def pre():
                    mk_oh(0)
                    mk_oh(1)
                    for b4_ in range(4):
                        P.op('pe', (lambda b4_: lambda e: e.matmul(PS[b4_][:, :], zeros_b[:, 0:128], zeros_b[:, :], start=True, stop=False))(b4_), reads=['zeros_b'], writes=[psr(b4_)])
                units.append(pre)
                for t in range(NT):
                    def u(t=t):
                        if t + 2 < NT:
                            mk_oh(t + 2)
                        ob = obuf[t]
                        for c in range(8):
                            P.op('pe', (lambda ob, c: lambda e: e.matmul(PS[c // 2][:, (c % 2) * 256:(c % 2 + 1) * 256], h2[:, t, c * 128:(c + 1) * 128], oh[ob][:], start=False, stop=(t == NT - 1 and c % 2 == 1)))(ob, c),
                                 reads=[('h2', t), ('oh', ob)], writes=[psr(c // 2)])
                        for jh in range(2):
                            P.op('pe', (lambda ob, jh: lambda e: e.matmul(PS[4 + jh][:, (t % 4) * 128:(t % 4 + 1) * 128], ohg[ob][:, jh * 128:(jh + 1) * 128], ident_b[:], start=True, stop=True))(ob, jh),
                                 reads=[('ohg', ob), 'ident_b'], writes=[psr(4 + jh)])
                        if t % 4 == 3:
                            t0_ = t - 3
                            P.op('act', (lambda t0_: lambda e: e.activation(out=ohT[xb][:, 0, t0_ * 128:(t0_ + 4) * 128], in_=PS[4][:, :], func=AF.Copy))(t0_),
                                 reads=[psr(4)], writes=[('ohT', xb, 0, t0_ // 4)])
                            P.op('dve', (lambda t0_: lambda e: e.tensor_copy(ohT[xb][:, 1, t0_ * 128:(t0_ + 4) * 128], PS[5][:, :]))(t0_),
                                 reads=[psr(5)], writes=[('ohT', xb, 1, t0_ // 4)])
                    units.append(u)

                def fin():
                    for b4 in range(4):
                        if b4 % 2 == 0:
                            P.op('act', (lambda b4: lambda e: e.activation(out=xgT[xb][:, 2 * b4:2 * b4 + 2, :], in_=PS[b4][:, :].rearrange("p (a d) -> p a d", a=2), func=AF.Copy))(b4), reads=[psr(b4)], writes=[('xgT', 0, b4)])
                        else:
                            P.op('dve', (lambda b4: lambda e: e.tensor_copy(xgT[xb][:, 2 * b4:2 * b4 + 2, :], PS[b4][:, :].rearrange("p (a d) -> p a d", a=2)))(b4), reads=[psr(b4)], writes=[('xgT', 0, b4)])
                units.append(fin)
                return units

            def scatter_units(ex):
                xb = ex % 2
                units = []
                for t in range(NT):
                    for dh in range(2):
                        def u(t=t, dh=dh):
                            bk = 4 + (t * 2 + dh) % 2
                            for jh in range(2):
                                P.op('pe', (lambda bk, jh: lambda e: e.matmul(PS[bk][:, :], ohT[xb][:, jh, t * 128:(t + 1) * 128], ye[:, jh, dh * 512:(dh + 1) * 512], start=(jh == 0), stop=(jh == 1)))(bk, jh),
                                     reads=[('ohT', xb, jh, t // 4), ('ye', jh, dh)], writes=[psr(bk)])
                            P.op('dve', (lambda bk: lambda e: e.tensor_tensor(xres[:, t, dh * 512:(dh + 1) * 512], xres[:, t, dh * 512:(dh + 1) * 512], PS[bk][:, :], ALU.add))(bk),
                                 reads=[psr(bk), ('xres', t)], writes=[('xres', t)])
                        units.append(u)
                return units

            def ffn(ex, fillers):
                xb = ex % 2
                fillers = list(fillers)
                nfc_done = [0]
                nfill_total = [len(fillers)]
                nfill_emitted = [0]
                for fb, (f0, fw) in enumerate(nblk):
                    wgi = get_w(ex, 'g', fb)
                    wui = get_w(ex, 'u', fb)
                    for k in range(fw // 128):
                        fi = fb * 4 + k
                        bk = 6 + fi % 2
                        for c in range(8):
                            P.op('pe', (lambda bk, wgi, c, k: lambda e: e.matmul(PS[bk][:, 0:256], WP[wgi][:, c, k * 128:(k + 1) * 128], xgT[xb][:, c, :], start=(c == 0), stop=(c == 7)))(bk, wgi, c, k),
                                 reads=[('wp', wgi), ('xgT', 0, c // 2)], writes=[psr(bk)])
                        for c in range(8):
                            P.op('pe', (lambda bk, wui, c, k: lambda e: e.matmul(PS[bk][:, 256:512], WP[wui][:, c, k * 128:(k + 1) * 128], xgT[xb][:, c, :], start=(c == 0), stop=(c == 7)))(bk, wui, c, k),
                                 reads=[('wp', wui), ('xgT', 0, c // 2)], writes=[psr(bk)])
                        sgi = fi % 2
                        P.op('act', (lambda bk, sgi: lambda e: e.activation(out=sg[sgi][:], in_=PS[bk][:, 0:256], func=AF.Silu))(bk, sgi), reads=[psr(bk)], writes=[('sg', sgi)])
                        P.op('dve', (lambda bk, sgi, fi: lambda e: e.tensor_tensor(hact[:, fi, :], sg[sgi][:], PS[bk][:, 256:512], ALU.mult))(bk, sgi, fi), reads=[psr(bk), ('sg', sgi)], writes=[('hact', fi)])
                        nfc_done[0] += 1
                        want = (len(fillers) * 0 + nfill_total[0] * nfc_done[0] + NFC - 1) // NFC
                        while nfill_emitted[0] < want and fillers:
                            fillers.pop(0)()
                            nfill_emitted[0] += 1
                for fb in range(6):
                    nk = 4 if fb < 5 else 2
                    wdi = get_w(ex, 'd', fb)
                    for k in range(nk):
                        fi = fb * 4 + k
                        for jh in range(2):
                            for dh in range(2):
                                bk = jh * 2 + dh
                                P.op('pe', (lambda bk, wdi, fi, k, jh, dh: lambda e: e.matmul(PS[bk][:, :], hact[:, fi, jh * 128:(jh + 1) * 128], WP[wdi][:, 2 * k + dh, :], start=(fi == 0), stop=(fi == NFC - 1)))(bk, wdi, fi, k, jh, dh),
                                     reads=[('wp', wdi), ('hact', fi)], writes=[psr(bk)])
                for jh in range(2):
                    for dh in range(2):
                        bk = jh * 2 + dh
                        P.op('dve', (lambda bk, jh, dh: lambda e: e.tensor_tensor(ye[:, jh, dh * 512:(dh + 1) * 512], PS[bk][:, :], g2bc[:, dh * 512:(dh + 1) * 512], ALU.mult))(bk, jh, dh),
                             reads=[psr(bk), ('g2bc' if False else ('bc4', 3))], writes=[('ye', jh, dh)])

            for u in gather_units(0):
                u()
            for ex in range(NE):
                ffn(ex, scatter_units(ex - 1) if ex >= 1 else [])
                if ex + 1 < NE:
                    for u in gather_units(ex + 1):
                        u()
            for u in scatter_units(NE - 1):
                u()
            P.emit_phase()

        del WP[3:]
        wpx_stack.close()
        with contextlib.ExitStack() as ph:
            def sb(name, shape, dt_):
                return ph.enter_context(nc.sbuf_tensor('s_' + name, list(shape), dt_, side='left'))
            gfin = sb('gfin', [128, D], F32)
            ob_ = [sb('ob%d' % i, [128, D], F32) for i in range(2)]
            junkf = sb('junkf', [128, D], F32)
            ssf = sb('ssf', [128, 16], F32)
            msf = sb('msf', [128, 16], F32)
            sqf = sb('sqf', [128, 16], F32)
            rsf = sb('rsf', [128, 16], F32)
            P.op('sp', lambda e: e.dma_start(out=gfin[:], in_=gfin_d), writes=['gfin'], dma='cst')
            P.op('dve', lambda e: e.memset(ssf[:], 0.0), writes=['ssf'])
            for t in range(NT):
                b = t % 2
                P.op('act', (lambda t: lambda e: e.activation(out=junkf[:], in_=xres[:, t, :], func=AF.Square, accum_out=ssf[:, t:t + 1]))(t), reads=[('xres', t), 'ssf'], writes=['junkf', ('ssf', t)])
                P.op('dve', (lambda t: lambda e: e.tensor_scalar(msf[:, t:t + 1], ssf[:, t:t + 1], 1.0 / D, EPS, ALU.mult, ALU.add))(t), reads=[('ssf', t)], writes=[('msf', t)])
                P.op('act', (lambda t: lambda e: e.activation(out=sqf[:, t:t + 1], in_=msf[:, t:t + 1], func=AF.Sqrt))(t), reads=[('msf', t)], writes=[('sqf', t)])
                P.op('dve', (lambda t: lambda e: e.reciprocal(rsf[:, t:t + 1], sqf[:, t:t + 1]))(t), reads=[('sqf', t)], writes=[('rsf', t)])
                P.op('dve', (lambda b, t: lambda e: e.scalar_tensor_tensor(ob_[b][:], xres[:, t, :], rsf[:, t:t + 1], gfin[:], ALU.mult, ALU.mult))(b, t), reads=[('xres', t), ('rsf', t), 'gfin'], writes=[('ob', b)])
                P.op('sp', (lambda b, t: lambda e: e.dma_start(out=out_d[t * 128:(t + 1) * 128, :], in_=ob_[b][:]))(b, t), reads=[('ob', b)], dma=('ob', b))
            P.emit_phase()
        P.final_wait_all_dma()
    return nc


def _t5_bucket_static(rel):
    nb = 16
    ret = np.where(rel > 0, nb, 0)
    n = np.abs(rel)
    max_exact = nb // 2
    nf = np.maximum(n, 1).astype(np.float32)
    large = max_exact + (np.log(nf / max_exact) / math.log(128 / max_exact) * (nb - max_exact)).astype(np.int32)
    large = np.minimum(large, nb - 1)
    return ret + np.where(n < max_exact, n, large)


def _col(v, n):
    return np.ascontiguousarray(np.asarray(v, np.float32).reshape(n, 128).T)


def _rep(v):
    v = np.asarray(v, np.float32).reshape(1, -1)
    return np.ascontiguousarray(np.broadcast_to(v, (128, v.shape[1])))


def prep_inputs(inp):
    f = lambda a: np.ascontiguousarray(np.asarray(a, np.float32))
    sh = {}
    sh['ada_w'] = f(inp['ada_w'][0])
    ada_b = f(inp['ada_b'][0])
    sh['ada_brow'] = np.ascontiguousarray(ada_b.reshape(1, -1))
    sh['ada_bg'] = _rep(ada_b[2048:6144])
    sh['gmixT'] = _col(inp['norm_mix_g'][0], 8)
    sh['gffn_bc'] = _rep(inp['norm_ffn_g'][0])
    sh['gfin_bc'] = _rep(inp['norm_final_g'])
    sh['w_in'] = f(inp['w_in'][0])
    sh['lam_qk'] = _rep(np.concatenate([f(inp['lambda_q1'][0]), f(inp['lambda_k1'][0]), f(inp['lambda_q2'][0]), f(inp['lambda_k2'][0])]))
    sh['subln_bc'] = _rep(inp['attn_subln_g'][0])
    tab = f(inp['rel_bias_table'])
    kl = np.arange(128)[:, None]
    ql = np.arange(128)[None, :]
    blk = np.zeros((128, 3, 4, 128), np.float32)
    for d in (-1, 0, 1):
        bidx = _t5_bucket_static(d * 128 + kl - ql)
        for h in range(4):
            blk[:, d + 1, h, :] = tab[bidx, h]
    sh['biasblk'] = blk
    sh['cfar'] = _rep(np.concatenate([tab[15, :], tab[31, :]]))
    cw = f(inp['conv_w'][0])[:, 0, :]
    sh['conv_wT'] = np.ascontiguousarray(cw.reshape(5, 6, 128).transpose(2, 1, 0))
    sh['conv_bT'] = _col(inp['conv_b'][0], 6)
    dtb = np.concatenate([f(inp['dt_bias_f'][0]), f(inp['dt_bias_b'][0])])
    sh['dtb256'] = _rep(np.tile(dtb, 16))
    alog = np.concatenate([f(inp['A_log_f'][0]), f(inp['A_log_b'][0])])
    sh['alog256'] = _rep(np.tile(alog, 16))
    sh['dskip_bc'] = _rep(inp['D_skip'][0])
    sh['ssmg_bc'] = _rep(inp['ssm_norm_g'][0])
    sh['w_out'] = f(inp['w_out'][0])
    sh['router_wT'] = np.ascontiguousarray(f(inp['router_w'][0]).reshape(8, 128, 16).transpose(1, 0, 2))
    sh['w_gate'] = f(inp['w_gate'][0])
    sh['w_up'] = f(inp['w_up'][0])
    sh['w_down'] = f(inp['w_down'][0])
    x = f(inp['x'])
    c = f(inp['c'])
    maps = []
    for b in range(x.shape[0]):
        m = dict(sh)
        m['x'] = x[b]
        m['c_col'] = _col(c[b], 8)
        maps.append(m)
    return maps


_NC_CACHE = {}


def kernel(**inputs):
    maps = prep_inputs(inputs)
    if 'nc' not in _NC_CACHE:
        _NC_CACHE['nc'] = build()
    nc = _NC_CACHE['nc']
    res = run_bass_kernel_spmd(nc, maps, core_ids=list(range(8)))
    return np.stack([np.asarray(r['out'], np.float32) for r in res.results], axis=0)
```

```python
import contextlib
import math
import numpy as np
import concourse.bass as bass
import concourse.mybir as mybir
from concourse.bass_utils import run_bass_kernel_spmd

F32 = mybir.dt.float32
BF16 = mybir.dt.bfloat16
ALU = mybir.AluOpType
AF = mybir.ActivationFunctionType
AX = mybir.AxisListType

S = 2048
D = 1024
NT = 16
EPS = 1e-6
NEXP = 16
FF = 2816
NFC = 22
CAP = 256
LAM_INIT = 0.8 - 0.6 * math.exp(0.0)


class Prog:
    ENGS = ('pe', 'act', 'dve', 'pool', 'sp')

    def __init__(self, nc, esems, dma_sems):
        self.nc = nc
        self.esem = esems
        self.ecnt = {e: 0 for e in self.ENGS}
        self.dsem = {}
        self.free_dsems = list(dma_sems)
        self.ops = []
        self.lastw = {}
        self.readers = {}
        self.known = {e: {} for e in self.ENGS}

    def _dma_token(self, key):
        if key not in self.dsem:
            self.dsem[key] = [self.free_dsems.pop(), 0]
        ent = self.dsem[key]
        ent[1] += 16
        return ('d', key, ent[1])

    def op(self, eng, fn, reads=(), writes=(), dma=None):
        idx = len(self.ops)
        deps = set()
        for r in reads:
            w = self.lastw.get(r)
            if w is not None:
                deps.add(w)
            if isinstance(r, tuple) and r[0] == 'ps':
                for t in self.readers.get(r, ()):
                    if t[0] == 'c' and self.ops[t[1]]['eng'] != eng:
                        deps.add(t)
        for r in writes:
            w = self.lastw.get(r)
            if w is not None:
                deps.add(w)
            for t in self.readers.get(r, ()):
                if t[0] == 'c' and dma is None and self.ops[t[1]]['eng'] == eng and eng == 'pe':
                    continue
                deps.add(t)
        tok = self._dma_token(dma) if dma is not None else ('c', idx)
        fdeps = set()
        for t in deps:
            if t[0] == 'c' and dma is None and eng == 'pe' and self.ops[t[1]]['eng'] == 'pe':
                continue
            fdeps.add(t)
        self.ops.append(dict(eng=eng, fn=fn, deps=fdeps, dma=dma, tok=tok))
        for r in reads:
            self.readers.setdefault(r, []).append(tok)
        for r in writes:
            self.lastw[r] = tok
            self.readers[r] = []
        return tok

    def emit_phase(self):
        self.phase_no = getattr(self, 'phase_no', 0) + 1
        with self.nc.named_scope('ph%d' % self.phase_no):
            self._emit_phase()

    def _emit_phase(self):
        nc = self.nc
        ops = self.ops
        sig = set()
        for o in ops:
            for t in o['deps']:
                if t[0] == 'c':
                    sig.add(t[1])
        cnt = {}
        for i, o in enumerate(ops):
            if i in sig:
                self.ecnt[o['eng']] += 1
                cnt[i] = self.ecnt[o['eng']]
        per = {e: [] for e in self.ENGS}
        for i, o in enumerate(ops):
            per[o['eng']].append(i)

        def run(eng_name, engobj):
            kn = self.known[eng_name]
            for i in per[eng_name]:
                o = ops[i]
                need = {}
                for t in o['deps']:
                    if t[0] == 'c':
                        key = ('e', ops[t[1]]['eng'])
                        val = cnt[t[1]]
                    else:
                        key = ('d', t[1])
                        val = t[2]
                        if t[1] in ('cst', 'cstB'):
                            val = self.dsem[t[1]][1]
                    if kn.get(key, 0) >= val:
                        continue
                    need[key] = max(need.get(key, 0), val)
                for key, val in need.items():
                    sem = self.esem[key[1]] if key[0] == 'e' else self.dsem[key[1]][0]
                    engobj.wait_ge(sem, val)
                    kn[key] = val
                ins = o['fn'](engobj)
                if o['dma'] is not None:
                    ins.then_inc(self.dsem[o['dma']][0], 16)
                elif i in sig:
                    ins.then_inc(self.esem[o['eng']], 1)

        with nc.Block() as block:
            if per['pe']:
                @block.tensor
                def _(e):
                    run('pe', e)
            if per['act']:
                @block.scalar
                def _(e):
                    run('act', e)
            if per['dve']:
                @block.vector
                def _(e):
                    run('dve', e)
            if per['pool']:
                @block.gpsimd
                def _(e):
                    run('pool', e)
            if per['sp']:
                @block.sync
                def _(e):
                    run('sp', e)
        for r in list(self.lastw.keys()):
            if self.lastw[r] is not None and self.lastw[r][0] == 'c':
                self.lastw[r] = None
        for r in list(self.readers.keys()):
            self.readers[r] = [t for t in self.readers[r] if t[0] == 'd']
        self.ops = []

    def final_wait_all_dma(self):
        nc = self.nc
        with nc.Block() as block:
            @block.sync
            def _(e):
                for key, (sem, val) in self.dsem.items():
                    if val > 0:
                        e.wait_ge(sem, val)


def build(stop_after=None, dbg=None):
    dbg = dbg or {}
    nc = bass.Bass("TRN2", target_bir_lowering=False)

    def din(name, shape):
        return nc.dram_tensor(name, list(shape), F32, kind="ExternalInput").ap()

    x_d = din("x", [S, D])
    ccol_d = din("c_col", [128, 8])
    adaw_d = din("ada_w", [D, 6 * D])
    adabrow_d = din("ada_brow", [1, 6 * D])
    adabg_d = din("ada_bg", [128, 4096])
    gmixT_d = din("gmixT", [128, 8])
    gffn_d = din("gffn_bc", [128, D])
    gfin_d = din("gfin_bc", [128, D])
    win_d = din("w_in", [D, 2832])
    lamqk_d = din("lam_qk", [128, 256])
    subln_d = din("subln_bc", [128, 128])
    biasblk_d = din("biasblk", [128, 3, 4, 128])
    cfar_d = din("cfar", [128, 8])
    convw_d = din("conv_wT", [128, 6, 5])
    convb_d = din("conv_bT", [128, 6])
    dtb_d = din("dtb256", [128, 256])
    alog_d = din("alog256", [128, 256])
    dskip_d = din("dskip_bc", [128, 8])
    ssmg_d = din("ssmg_bc", [128, 512])
    wout_d = din("w_out", [D, D])
    rw_d = din("router_wT", [128, 8, 16])
    if stop_after in (None, 'E', 'E1'):
        wg_d = din("w_gate", [NEXP, D, FF])
        wu_d = din("w_up", [NEXP, D, FF])
        wd_d = din("w_down", [NEXP, FF, D])
    out_d = nc.dram_tensor("out", [S, D], F32, kind="ExternalOutput").ap()
    dbg_d = {k: nc.dram_tensor("dbg_" + k, list(shp), F32, kind="ExternalOutput").ap() for k, shp in dbg.items()}

    with contextlib.ExitStack() as top:
        def sbT(name, shape, dt, side='right'):
            return top.enter_context(nc.sbuf_tensor('s_' + name, list(shape), dt, side=side))

        esems = {e: top.enter_context(nc.semaphore('es_' + e)) for e in ('pe', 'act', 'dve', 'pool')}
        dsems = [top.enter_context(nc.semaphore('ds%d' % i)) for i in range(48)]
        P = Prog(nc, esems, dsems)
        PS = [top.enter_context(nc.psum_tensor('psb%d' % i, [128, 512], F32)) for i in range(8)]

        def psr(b):
            return ('ps', b)

        ident_f = sbT('ident_f', [128, 128], F32)
        ident_b = sbT('ident_b', [128, 128], BF16)
        ones_f = sbT('ones_f', [128, 128], F32)
        tri_ip = sbT('tri_ip', [128, 128], F32)
        tri_es = sbT('tri_es', [128, 128], F32)
        tri_is = sbT('tri_is', [128, 128], F32)
        tri_ep = sbT('tri_ep', [128, 128], F32)
        negm_f = sbT('negm_f', [128, 128], F32)
        negm_b = sbT('negm_b', [128, 128], F32)
        iota_j = sbT('iota_j', [128, 256], F32)
        iota_p = sbT('iota_p', [128, 2], F32)
        modT = sbT('modT', [128, 48], F32)
        a1 = sbT('a1', [128, 8], F32)
        g2bc = sbT('g2bc', [128, D], F32)
        WP = [sbT('wp%d' % i, [128, 8, 512], BF16) for i in range(3)]
        wp_ctr = [0]

        def wp_next():
            i = wp_ctr[0] % len(WP)
            wp_ctr[0] += 1
            return i

        def load_w(i, src_ap, ncols, nk=8):
            P.op('pool', lambda e: e.dma_start(out=WP[i][:, 0:nk, 0:ncols],
                                               in_=src_ap.rearrange("(c p) n -> p c n", p=128)),
                 writes=[('wp', i)], dma=('wp', i))


        NBLK = [(i * 512, min(512, FF - i * 512)) for i in range(6)]
        prefetched = {}

        def issue_w(ex, kind, fb):
            i = wp_next()
            if kind in ('g', 'u'):
                f0, fw = NBLK[fb]
                src = (wg_d if kind == 'g' else wu_d)[ex, :, f0:f0 + fw]
                load_w(i, src, fw)
            else:
                nk = 4 if fb < 5 else 2
                P.op('pool', lambda e: e.dma_start(out=WP[i][:, 0:2 * nk, :].rearrange("p (k h) n -> p k h n", h=2),
                                                   in_=wd_d[ex, fb * 512:fb * 512 + nk * 128, :].rearrange("(k p) (h n) -> p k h n", p=128, h=2)),
                     writes=[('wp', i)], dma=('wp', i))
            return i

        def get_w(ex, kind, fb):
            key = (ex, kind, fb)
            if key in prefetched:
                return prefetched.pop(key)
            return issue_w(ex, kind, fb)

        def dump(key, ap, reads):
            if key in dbg_d:
                P.op('sp', lambda e: e.dma_start(out=dbg_d[key], in_=ap), reads=reads, dma='dbg')

        P.op('pool', lambda e: e.memset(ident_f[:], 0.0), writes=['ident_f'])
        P.op('pool', lambda e: e.affine_select(ident_f[:], ident_f[:], [[-1, 128]], ALU.not_equal, 1.0, base=0, channel_multiplier=1),
             reads=['ident_f'], writes=['ident_f'])
        P.op('pool', lambda e: e.tensor_copy(ident_b[:], ident_f[:]), reads=['ident_f'], writes=['ident_b'])
        P.op('pool', lambda e: e.memset(ones_f[:], 1.0), writes=['ones_f'])
        for tl, key, cmp_ in ((tri_ip, 'tri_ip', ALU.is_ge), (tri_ep, 'tri_ep', ALU.is_gt)):
            P.op('pool', (lambda tl, cmp_: lambda e: e.affine_select(tl[:], ones_f[:], [[1, 128]], cmp_, 0.0, base=0, channel_multiplier=-1))(tl, cmp_),
                 reads=['ones_f'], writes=[key])
        for tl, key, cmp_ in ((tri_is, 'tri_is', ALU.is_ge), (tri_es, 'tri_es', ALU.is_gt)):
            P.op('pool', (lambda tl, cmp_: lambda e: e.affine_select(tl[:], ones_f[:], [[-1, 128]], cmp_, 0.0, base=0, channel_multiplier=1))(tl, cmp_),
                 reads=['ones_f'], writes=[key])
        zeros_f = sbT('zeros_f', [128, 128], F32)
        zeros_b = sbT('zeros_b', [128, 512], BF16)
        P.op('pool', lambda e: e.memset(zeros_b[:], 0.0), writes=['zeros_b'])
        P.op('pool', lambda e: e.memset(zeros_f[:], 0.0), writes=['zeros_f'])
        P.op('pool', lambda e: e.affine_select(negm_f[:], zeros_f[:], [[1, 128]], ALU.is_ge, -30000.0, base=0, channel_multiplier=-1),
             reads=['zeros_f'], writes=['negm_f'])
        P.op('pool', lambda e: e.affine_select(negm_b[:], zeros_f[:], [[-1, 128]], ALU.is_ge, -30000.0, base=0, channel_multiplier=1),
             reads=['zeros_f'], writes=['negm_b'])
        P.op('pool', lambda e: e.iota(iota_j[:], [[1, 256]], base=0, channel_multiplier=0, allow_small_or_imprecise_dtypes=True), writes=['iota_j'])
        P.op('pool', lambda e: e.iota(iota_p[:], [[128, 2]], base=0, channel_multiplier=1, allow_small_or_imprecise_dtypes=True), writes=['iota_p'])

        bcst = contextlib.ExitStack()
        bc3 = bcst.enter_context(nc.sbuf_tensor('s_bc3', [128, 3, D], F32, side='left'))

        def bcrow(i):
            return bc3[:, i, :] if i < 3 else g2bc[:, :]

        mixer = contextlib.ExitStack()
        mixT = mixer.enter_context(nc.sbuf_tensor('mixT', [128, 8, S], BF16, side='left'))
        hstack = contextlib.ExitStack()
        hT = hstack.enter_context(nc.sbuf_tensor('hT', [128, 8, S], BF16, side='left'))

        with contextlib.ExitStack() as ph:
            def sb(name, shape, dt):
                return ph.enter_context(nc.sbuf_tensor('s_' + name, list(shape), dt, side='left'))
            adaw = [sb('adaw%d' % i, [128, 8, 512], F32) for i in range(2)]
            xt = [sb('xt%d' % i, [128, D], F32) for i in range(2)]
            xn = [sb('xn%d' % i, [128, D], F32) for i in range(2)]
            ccol = sb('ccol', [128, 8], F32)
            scv = sb('scv', [128, 8], F32)
            scb = sb('scb', [128, 8, 128], F32)
            abr = [sb('abr%d' % i, [1, 512], F32) for i in range(2)]
            rowt = [sb('rowt%d' % i, [1, 512], F32) for i in range(2)]
            abg = sb('abg', [128, 4096], F32)
            gmixT = sb('gmixT', [128, 8], F32)
            gffn = sb('gffn', [128, D], F32)
            ss = sb('ss', [128, 16], F32)
            ms = sb('ms', [128, 16], F32)
            sq = sb('sq', [128, 16], F32)
            rstd = sb('rstd', [128, 16], F32)

            P.op('sp', lambda e: e.dma_start(out=ccol[:], in_=ccol_d), writes=['ccol'], dma='cst')
            P.op('sp', lambda e: e.dma_start(out=gmixT[:], in_=gmixT_d), writes=['gmixT'], dma='cst')
            P.op('sp', lambda e: e.dma_start(out=abg[:], in_=adabg_d), writes=['abg'], dma='cst')
            P.op('sp', lambda e: e.dma_start(out=gffn[:], in_=gffn_d), writes=['gffn'], dma='cst')
            P.op('act', lambda e: e.activation(out=scv[:], in_=ccol[:], func=AF.Silu), reads=['ccol'], writes=['scv'])
            P.op('dve', lambda e: e.tensor_copy(scb[:], scv[:].unsqueeze(2).to_broadcast([128, 8, 128])), reads=['scv'], writes=['scb'])
            wq, wk, wv = wp_next(), wp_next(), wp_next()
            load_w(wv, win_d[:, 1024:1536], 512)
            load_w(wq, win_d[:, 0:512], 512)
            load_w(wk, win_d[:, 512:1024], 512)
            adaw_v = adaw_d.rearrange("(c p) n -> p c n", p=128)
            gi_ctr = [0]

            def ada_block(blk):
                ab = blk % 2
                gi = gi_ctr[0]
                bk = 1 + ab
                P.op('sp', (lambda ab, blk: lambda e: e.dma_start(out=adaw[ab][:], in_=adaw_v[:, :, blk * 512:(blk + 1) * 512]))(ab, blk),
                     writes=[('adaw', ab)], dma=('adaw', ab))
                P.op('sp', (lambda ab, blk: lambda e: e.dma_start(out=abr[ab][:], in_=adabrow_d[:, blk * 512:(blk + 1) * 512]))(ab, blk),
                     writes=[('abr', ab)], dma=('abr', ab))
                for c in range(8):
                    P.op('pe', (lambda ab, bk, c: lambda e: e.matmul(PS[bk][:, :], scb[:, c, :], adaw[ab][:, c, :], start=(c == 0), stop=(c == 7)))(ab, bk, c),
                         reads=[('adaw', ab), 'scb'], writes=[psr(bk)])
                P.op('act', (lambda ab, bk, blk: lambda e: e.activation(out=rowt[ab][0:1, :], in_=PS[bk][0:1, :], func=AF.Copy))(ab, bk, blk), reads=[psr(bk)], writes=[('rowt', ab)])
                P.op('dve', (lambda ab, blk: lambda e: e.tensor_tensor(rowt[ab][0:1, :], rowt[ab][0:1, :], abr[ab][0:1, :], ALU.add))(ab, blk), reads=[('rowt', ab), ('abr', ab)], writes=[('rowt', ab)])
                if blk >= 4:
                    P.op('dve', (lambda bk, gi: lambda e: e.tensor_tensor(bcrow(gi // 2)[:, (gi % 2) * 512:(gi % 2 + 1) * 512], PS[bk][:, :], abg[:, gi * 512:(gi + 1) * 512], ALU.add))(bk, gi),
                         reads=[psr(bk), 'abg'], writes=[('bc4', gi // 2)])
                    gi_ctr[0] += 1
                for jj in range(4):
                    j = blk * 4 + jj
                    P.op('pe', (lambda ab, jj, j: lambda e: e.transpose(PS[0][:, j:j + 1], rowt[ab][0:1, jj * 128:(jj + 1) * 128], ident_f[0:1, 0:1]))(ab, jj, j),
                         reads=[('rowt', ab), 'ident_f'], writes=[psr(0)])
            for blk in range(4):
                ada_block(blk)
            P.op('dve', lambda e: e.tensor_copy(modT[:, 0:16], PS[0][:, 0:16]), reads=[psr(0)], writes=[('modT', 0)])
            P.op('dve', lambda e: e.scalar_tensor_tensor(a1[:], modT[:, 8:16], 1.0, gmixT[:], ALU.add, ALU.mult), reads=[('modT', 0), 'gmixT'], writes=['a1'])
            P.op('dve', lambda e: e.memset(ss[:], 0.0), writes=['ss'])
            def norm_tile(t):
                b = t % 2
                P.op('sp', (lambda b, t: lambda e: e.dma_start(out=xt[b][:], in_=x_d[t * 128:(t + 1) * 128, :]))(b, t), writes=[('xt', b)], dma=('xt', b))
                P.op('act', (lambda b, t: lambda e: e.activation(out=xn[b][:], in_=xt[b][:], func=AF.Square, accum_out=ss[:, t:t + 1]))(b, t),
                     reads=[('xt', b), 'ss'], writes=[('xn', b), ('ss', t)])
                P.op('dve', (lambda t: lambda e: e.tensor_scalar(ms[:, t:t + 1], ss[:, t:t + 1], 1.0 / D, EPS, ALU.mult, ALU.add))(t), reads=[('ss', t)], writes=[('ms', t)])
                P.op('act', (lambda t: lambda e: e.activation(out=sq[:, t:t + 1], in_=ms[:, t:t + 1], func=AF.Sqrt))(t), reads=[('ms', t)], writes=[('sq', t)])
                P.op('dve', (lambda t: lambda e: e.reciprocal(rstd[:, t:t + 1], sq[:, t:t + 1]))(t), reads=[('sq', t)], writes=[('rstd', t)])
                P.op('dve', (lambda b, t: lambda e: e.tensor_scalar(xn[b][:], xt[b][:], rstd[:, t:t + 1], None, ALU.mult))(b, t),
                     reads=[('xt', b), ('rstd', t)], writes=[('xn', b)])
                for c in range(8):
                    bk = 3 + 2 * b + c // 4
                    P.op('pe', (lambda b, bk, c: lambda e: e.transpose(PS[bk][:, (c % 4) * 128:(c % 4 + 1) * 128], xn[b][:, c * 128:(c + 1) * 128], ident_f[:]))(b, bk, c),
                         reads=[('xn', b), 'ident_f'], writes=[psr(bk)])
                for c in range(8):
                    bk = 3 + 2 * b + c // 4
                    if c // 4 == 0:
                        P.op('act', (lambda bk, c, t: lambda e: e.activation(out=hT[:, c, t * 128:(t + 1) * 128], in_=PS[bk][:, (c % 4) * 128:(c % 4 + 1) * 128], func=AF.Identity, scale=a1[:, c:c + 1], bias=modT[:, c:c + 1]))(bk, c, t),
                             reads=[psr(bk), 'a1', ('modT', 0)], writes=[('hT', c, t)])
                    else:
                        P.op('dve', (lambda bk, c, t: lambda e: e.tensor_scalar(hT[:, c, t * 128:(t + 1) * 128], PS[bk][:, (c % 4) * 128:(c % 4 + 1) * 128], a1[:, c:c + 1], modT[:, c:c + 1], ALU.mult, ALU.add))(bk, c, t),
                             reads=[psr(bk), 'a1', ('modT', 0)], writes=[('hT', c, t)])
            for i_ in range(8):
                ada_block(4 + i_)
                norm_tile(2 * i_)
                norm_tile(2 * i_ + 1)
            P.op('dve', lambda e: e.tensor_copy(modT[:, 16:48], PS[0][:, 16:48]), reads=[psr(0)], writes=[('modT', 1)])
            P.op('dve', lambda e: e.scalar_tensor_tensor(bc3[:, 2, :], bc3[:, 2, :], 1.0, gffn[:], ALU.add, ALU.mult), reads=[('bc4', 2), 'gffn'], writes=[('bc4', 2)])
            dump('modT', modT[:], [('modT', 0), ('modT', 1)])
            P.emit_phase()

        def hT_reads(c, Q):
            return [('hT', c, 4 * Q + i) for i in range(4)]

        if stop_after == 'A':
            with contextlib.ExitStack() as ph:
                tmp = ph.enter_context(nc.sbuf_tensor('dbgtmp', [128, S], F32, side='left'))
                for c in range(8):
                    P.op('dve', (lambda c: lambda e: e.tensor_copy(tmp[:], hT[:, c, :]))(c), reads=[('hT', c, t) for t in range(NT)], writes=['dbgtmp'])
                    P.op('sp', (lambda c: lambda e: e.dma_start(out=dbg_d['hT'][c], in_=tmp[:]))(c), reads=['dbgtmp'], dma='dbg')
                P.emit_phase()
            P.final_wait_all_dma()
            hstack.close()
            mixer.close()
            bcst.close()
            return nc

        with contextlib.ExitStack() as ph:
            def sb(name, shape, dt):
                return ph.enter_context(nc.sbuf_tensor('s_' + name, list(shape), dt, side='left'))
            qT = sb('qT', [128, 2, 4, S], BF16)
            kT = sb('kT', [128, 4, S], BF16)
            vaug = sb('vaug', [128, NT, 4, 130], BF16)
            biasb = sb('biasb', [128, 3, 4, 128], BF16)
            cfar = sb('cfar', [128, 8], F32)
            lamqk = sb('lamqk', [128, 256], F32)
            lprod = sb('lprod', [128, 256], F32)
            lsum = sb('lsum', [128, 4], F32)
            lamneg = sb('lamneg', [128, 1], F32)
            g08 = sb('g08', [128, 128], F32)
            PT = [sb('PT%d' % i, [128, 512], BF16) for i in range(4)]
            accsb = [sb('accsb%d' % i, [128, 8, 129], F32) for i in range(2)]
            rr = [sb('rr%d' % i, [128, 16], F32) for i in range(2)]
            t0b = [sb('t0b%d' % i, [128, 128], F32) for i in range(2)]
            attb = [sb('attb%d' % i, [128, 128], F32) for i in range(4)]
            attn = [sb('attn%d' % i, [128, 128], F32) for i in range(4)]
            junk = sb('junkb', [128, 128], F32)
            ssa = sb('ssa', [128, 64], F32)
            msa = sb('msa', [128, 64], F32)
            sqa = sb('sqa', [128, 64], F32)
            rsa = sb('rsa', [128, 64], F32)

            P.op('pool', lambda e: e.dma_start(out=biasb[:], in_=biasblk_d), writes=['biasb'], dma='cstB')
            P.op('sp', lambda e: e.dma_start(out=cfar[:], in_=cfar_d), writes=['cfar'], dma='cst')
            P.op('sp', lambda e: e.dma_start(out=lamqk[:], in_=lamqk_d), writes=['lamqk'], dma='cst')
            P.op('sp', lambda e: e.dma_start(out=g08[:], in_=subln_d), writes=['g08'], dma='cst')
            P.op('dve', lambda e: e.tensor_tensor(lprod[:, 0:64], lamqk[:, 0:64], lamqk[:, 64:128], ALU.mult), reads=['lamqk'], writes=['lprod'])
            P.op('dve', lambda e: e.tensor_tensor(lprod[:, 64:128], lamqk[:, 128:192], lamqk[:, 192:256], ALU.mult), reads=['lamqk', 'lprod'], writes=['lprod'])
            P.op('dve', lambda e: e.reduce_sum(lsum[:, 0:1], lprod[:, 0:64], axis=AX.X), reads=['lprod'], writes=['lsum'])
            P.op('dve', lambda e: e.reduce_sum(lsum[:, 1:2], lprod[:, 64:128], axis=AX.X), reads=['lprod', 'lsum'], writes=['lsum'])
            P.op('act', lambda e: e.activation(out=lsum[:, 2:4], in_=lsum[:, 0:2], func=AF.Exp), reads=['lsum'], writes=['lsum'])
            P.op('dve', lambda e: e.tensor_tensor(lamneg[:], lsum[:, 3:4], lsum[:, 2:3], ALU.subtract), reads=['lsum'], writes=['lamneg'])
            P.op('dve', lambda e: e.tensor_scalar(lamneg[:], lamneg[:], -LAM_INIT, None, ALU.add), reads=['lamneg'], writes=['lamneg'])
            P.op('dve', lambda e: e.tensor_scalar(g08[:], g08[:], 1.0 - LAM_INIT, None, ALU.mult), reads=['g08'], writes=['g08'])
            P.op('dve', lambda e: e.memset(vaug[:, :, :, 128:130], 1.0), writes=['vones'])
            P.op('dve', lambda e: e.memset(ssa[:], 0.0), writes=[('ssa', c) for c in range(64)])

            ev = [0]
            P.op('pool', lambda e: e.memset(qT[64:128, 0, :, :], 0.0), writes=['qTz0'])
            P.op('pool', lambda e: e.memset(qT[0:64, 1, :, :], 0.0), writes=['qTz1'])
            def proj_unit(dname, wi, h, Q, bk, eng):
                for c in range(8):
                    P.op('pe', (lambda c: lambda e: e.matmul(PS[bk][:, :], WP[wi][:, c, h * 128:(h + 1) * 128], hT[:, c, Q * 512:(Q + 1) * 512], start=(c == 0), stop=(c == 7)))(c),
                         reads=[('wp', wi)] + hT_reads(c, Q), writes=[psr(bk)])
                if dname == 'kT':
                    if eng == 'act':
                        P.op('act', lambda e: e.activation(out=kT[:, h, Q * 512:(Q + 1) * 512], in_=PS[bk][:, :], func=AF.Copy), reads=[psr(bk)], writes=[('kT', h, Q)])
                    else:
                        P.op('dve', lambda e: e.tensor_copy(kT[:, h, Q * 512:(Q + 1) * 512], PS[bk][:, :]), reads=[psr(bk)], writes=[('kT', h, Q)])
                else:
                    for m in range(2):
                        ps_ = slice(m * 64, (m + 1) * 64)
                        if eng == 'act':
                            P.op('act', (lambda m, ps_: lambda e: e.activation(out=qT[ps_, m, h, Q * 512:(Q + 1) * 512], in_=PS[bk][ps_, :], func=AF.Copy, scale=0.125))(m, ps_),
                                 reads=[psr(bk), 'qTz0', 'qTz1'], writes=[('qT', h, Q, m)])
                        else:
                            P.op('dve', (lambda m, ps_: lambda e: e.tensor_scalar(qT[ps_, m, h, Q * 512:(Q + 1) * 512], PS[bk][ps_, :], 0.125, None, ALU.mult))(m, ps_),
                                 reads=[psr(bk), 'qTz0', 'qTz1'], writes=[('qT', h, Q, m)])

            for t in range(NT):
                bk = ev[0] % 2
                for c in range(8):
                    P.op('pe', (lambda bk, c, t: lambda e: e.matmul(PS[bk][:, :], hT[:, c, t * 128:(t + 1) * 128], WP[wv][:, c, :], start=(c == 0), stop=(c == 7)))(bk, c, t),
                         reads=[('wp', wv), ('hT', c, t)], writes=[psr(bk)])
                eng = 'act' if ev[0] % 2 == 0 else 'dve'
                if eng == 'act':
                    P.op('act', (lambda bk, t: lambda e: e.activation(out=vaug[:, t, :, 0:128], in_=PS[bk][:, :].rearrange("p (h d) -> p h d", h=4), func=AF.Copy))(bk, t),
                         reads=[psr(bk)], writes=[('v', t)])
                else:
                    P.op('dve', (lambda bk, t: lambda e: e.tensor_copy(vaug[:, t, :, 0:128], PS[bk][:, :].rearrange("p (h d) -> p h d", h=4)))(bk, t),
                         reads=[psr(bk)], writes=[('v', t)])
                ev[0] += 1

            for Q in range(4):
                for dname, wi in (('qT', wq), ('kT', wk)):
                    proj_unit(dname, wi, 0, Q, ev[0] % 2, 'act' if ev[0] % 2 == 0 else 'dve')
                    ev[0] += 1
            pending_proj = {h_: [(dname, wi, h_, Q) for Q in range(4) for dname, wi in (('qT', wq), ('kT', wk))] for h_ in range(1, 4)}

            ACCB = (2, 3, 4)
            steps = [(h, Q, j, m) for h in range(4) for Q in range(4) for j in range(NT) for m in range(2)]
            LA = 3
            SBK = (1, 5, 6, 7)

            def cats_of(Q, j):
                cats = []
                for qi in range(4):
                    d = j - (4 * Q + qi)
                    cats.append('n' if abs(d) <= 1 else ('lo' if d < 0 else 'hi'))
                return cats

            def emit_qk(idx):
                h, Q, j, m = steps[idx]
                sbk = SBK[idx % 4]
                cats = cats_of(Q, j)
                nnear = sum(1 for cc in cats if cc == 'n')
                P.op('pe', (lambda sbk, m, h, j, Q, nnear: lambda e: e.matmul(PS[sbk][:, :], kT[:, h, j * 128:(j + 1) * 128], qT[:, m, h, Q * 512:(Q + 1) * 512], start=True, stop=(nnear == 0)))(sbk, m, h, j, Q, nnear),
                     reads=[('kT', h, j // 4), ('qT', h, Q, m), 'qTz0', 'qTz1'], writes=[psr(sbk)])
                k = 0
                for qi in range(4):
                    if cats[qi] == 'n':
                        k += 1
                        d = j - (4 * Q + qi)
                        P.op('pe', (lambda sbk, qi, d, h, last: lambda e: e.matmul(PS[sbk][:, qi * 128:(qi + 1) * 128], ident_b[:], biasb[:, d + 1, h, :], start=False, stop=last))(sbk, qi, d, h, k == nnear),
                             reads=['biasb', 'ident_b'], writes=[psr(sbk)])

            def emit_exp_pv(idx):
                h, Q, j, m = steps[idx]
                sbk = SBK[idx % 4]
                pb = idx % 4
                cats = cats_of(Q, j)
                r0 = 0
                ri = 0
                while r0 < 4:
                    r1 = r0
                    while r1 < 4 and cats[r1] == cats[r0]:
                        r1 += 1
                    cat = cats[r0]
                    if cat == 'n':
                        P.op('act', (lambda pb, sbk, r0, r1: lambda e: e.activation(out=PT[pb][:, r0 * 128:r1 * 128], in_=PS[sbk][:, r0 * 128:r1 * 128], func=AF.Exp))(pb, sbk, r0, r1),
                             reads=[psr(sbk)], writes=[('PT', pb, ri)])
                    else:
                        ci = h if cat == 'lo' else 4 + h
                        P.op('act', (lambda pb, sbk, r0, r1, ci: lambda e: e.activation(out=PT[pb][:, r0 * 128:r1 * 128], in_=PS[sbk][:, r0 * 128:r1 * 128], func=AF.Exp, bias=cfar[:, ci:ci + 1]))(pb, sbk, r0, r1, ci),
                             reads=[psr(sbk), 'cfar'], writes=[('PT', pb, ri)])
                    r0 = r1
                    ri += 1
                if j == 0 and m == 0:
                    for bi_, bkk_ in enumerate(ACCB):
                        n_ = 3 if bi_ < 2 else 2
                        P.op('pe', (lambda bkk_, n_: lambda e: e.matmul(PS[bkk_][:, 0:n_ * 129], zeros_b[:, 0:128], zeros_b[:, 0:n_ * 129], start=True, stop=False))(bkk_, n_),
                             reads=['zeros_b'], writes=[psr(bkk_)])
                for qi in range(4):
                    a = m * 4 + qi
                    ab_, ao = ACCB[a // 3], (a % 3) * 129
                    P.op('pe', (lambda ab_, ao, pb, qi, j, h, a: lambda e: e.matmul(PS[ab_][:, ao:ao + 129], PT[pb][:, qi * 128:(qi + 1) * 128], vaug[:, j, h, 0:129], start=False, stop=(j == NT - 1 and (a % 3 == 2 or a == 7))))(ab_, ao, pb, qi, j, h, a),
                         reads=[('PT', pb, 0), ('PT', pb, 1), ('PT', pb, 2), ('v', j), 'vones'], writes=[psr(ab_)])
                if j == NT - 1 and m == 1:
                    epilogue(h, Q, (h * 4 + Q))

            deferred = {}
            cur_idx = [0]

            def defer(at, fn):
                deferred.setdefault(at, []).append(fn)

            def epilogue(h, Q, it):
                ai = it % 2
                cur = cur_idx[0]
                accr = [('accsb', ai, bi) for bi in range(3)]
                for bi, bkk in enumerate(ACCB):
                    n = 3 if bi < 2 else 2
                    P.op('dve', (lambda ai, bi, bkk, n: lambda e: e.tensor_copy(accsb[ai][:, bi * 3:bi * 3 + n, :], PS[bkk][:, 0:n * 129].rearrange("p (a d) -> p a d", a=n)))(ai, bi, bkk, n),
                         reads=[psr(bkk)], writes=[('accsb', ai, bi)])
                P.op('dve', (lambda ai: lambda e: e.reciprocal(rr[ai][:, 0:8], accsb[ai][:, :, 128]))(ai), reads=accr, writes=[('rr', ai)])
                P.op('dve', (lambda ai: lambda e: e.tensor_scalar(rr[ai][:, 8:12], rr[ai][:, 4:8], lamneg[:, 0:1], None, ALU.mult))(ai), reads=[('rr', ai), 'lamneg'], writes=[('rr', ai)])
                for qi in range(4):
                    col = it * 4 + qi
                    P.op('dve', (lambda ai, qi: lambda e: e.tensor_scalar(t0b[qi % 2][:], accsb[ai][:, qi, 0:128], rr[ai][:, qi:qi + 1], None, ALU.mult))(ai, qi),
                         reads=accr + [('rr', ai)], writes=[('t0b', qi % 2)])
                    P.op('dve', (lambda ai, qi: lambda e: e.scalar_tensor_tensor(attb[qi][:], accsb[ai][:, 4 + qi, 0:128], rr[ai][:, 8 + qi:9 + qi], t0b[qi % 2][:], ALU.mult, ALU.add))(ai, qi),
                         reads=accr + [('rr', ai), ('t0b', qi % 2)], writes=[('attb', qi)])
                    P.op('dve', (lambda qi: lambda e: e.tensor_tensor(junk[:], attb[qi][:], attb[qi][:], ALU.mult))(qi), reads=[('attb', qi)], writes=['junkb'])
                    P.op('dve', (lambda col: lambda e: e.reduce_sum(ssa[:, col:col + 1], junk[:], axis=AX.X))(col), reads=['junkb'], writes=[('ssa', col)])
                sl = slice(it * 4, it * 4 + 4)
                P.op('dve', (lambda sl: lambda e: e.tensor_scalar(msa[:, sl], ssa[:, sl], 1.0 / 128, EPS, ALU.mult, ALU.add))(sl), reads=[('ssa', it * 4 + q_) for q_ in range(4)], writes=[('msa', it)])

                def st2():
                    P.op('act', (lambda sl: lambda e: e.activation(out=sqa[:, sl], in_=msa[:, sl], func=AF.Ln))(sl), reads=[('msa', it)], writes=[('sqa', it)])
                    P.op('act', (lambda sl: lambda e: e.activation(out=rsa[:, sl], in_=sqa[:, sl], func=AF.Exp, scale=-0.5))(sl), reads=[('sqa', it)], writes=[('rsa', it)])

                def st3():
                    for qi in range(4):
                        col = it * 4 + qi
                        P.op('dve', (lambda qi, col: lambda e: e.scalar_tensor_tensor(attn[qi][:], attb[qi][:], rsa[:, col:col + 1], g08[:], ALU.mult, ALU.mult))(qi, col),
                             reads=[('attb', qi), ('rsa', it), 'g08'], writes=[('attn', qi)])

                def st4():
                    tb = 0
                    for qi in range(4):
                        P.op('pe', (lambda tb, qi: lambda e: e.transpose(PS[tb][:, qi * 128:(qi + 1) * 128], attn[qi][:], ident_f[:]))(tb, qi),
                             reads=[('attn', qi), 'ident_f'], writes=[psr(tb)])

                def st5():
                    tb = 0
                    P.op('dve', (lambda tb, h, Q: lambda e: e.tensor_copy(mixT[:, h, Q * 512:(Q + 1) * 512], PS[tb][:, :]))(tb, h, Q),
                         reads=[psr(tb)], writes=[('mixT', h, 4 * Q + q_) for q_ in range(4)])
                defer(cur + 14, st2)
                defer(cur + 18, st3)
                defer(cur + 22, st4)
                defer(cur + 26, st5)

            for idx in range(len(steps) + LA + 32):
                cur_idx[0] = idx
                if idx < len(steps):
                    emit_qk(idx)
                    h_cur = steps[idx][0]
                    if (idx % 128) % 16 == 8 and h_cur + 1 < 4 and pending_proj[h_cur + 1]:
                        proj_unit(*pending_proj[h_cur + 1].pop(0), 0, 'dve')
                if LA <= idx < len(steps) + LA:
                    emit_exp_pv(idx - LA)
                for fn in deferred.pop(idx, []):
                    fn()
            assert not deferred
            wz, wx, wb5 = wp_next(), wp_next(), wp_next()
            load_w(wz, win_d[:, 1536:2048], 512)
            load_w(wx, win_d[:, 2048:2560], 512)
            load_w(wb5, win_d[:, 2560:2832], 272)
            P.emit_phase()

        if stop_after == 'B':
            with contextlib.ExitStack() as ph:
                tmp = ph.enter_context(nc.sbuf_tensor('dbgtmp', [128, S], F32, side='left'))
                for c in range(4):
                    P.op('dve', (lambda c: lambda e: e.tensor_copy(tmp[:], mixT[:, c, :]))(c), reads=[('mixT', c, t) for t in range(NT)], writes=['dbgtmp'])
                    P.op('sp', (lambda c: lambda e: e.dma_start(out=dbg_d['mixT'][c], in_=tmp[:]))(c), reads=['dbgtmp'], dma='dbg')
                P.emit_phase()
            P.final_wait_all_dma()
            hstack.close()
            mixer.close()
            bcst.close()
            return nc

        cst = contextlib.ExitStack()

        def sbC(name, shape, dt):
            return cst.enter_context(nc.sbuf_tensor('s_' + name, list(shape), dt, side='right'))
        BTm = sbC('BTm', [128, 2, S], BF16)
        CTm = sbC('CTm', [128, 2, S], BF16)
        xs_tok = sbC('xs_tok', [128, NT, 512], BF16)
        B_tok = sbC('B_tok', [128, NT, 128], BF16)
        zs = sbC('zs', [128, NT, 512], BF16)
        dt = sbC('dt', [128, 256], F32)
        lndt = sbC('lndt', [128, 256], F32)
        dA = sbC('dA', [128, 256], F32)
        csall = sbC('csall', [128, 512], F32)
        expT = sbC('expT', [128, 256], F32)
        bias_fb = sbC('bias_fb', [128, 256], F32)
        sdec = sbC('sdec', [128, 256], F32)
        eoff = sbC('eoff', [128, 256], F32)
        dskip = sbC('dskip', [128, 8], F32)
        ssmg = sbC('ssmg', [128, 512], F32)

        def v3(ap, n):
            return ap.rearrange("p (t k) -> p t k", t=NT)

        with contextlib.ExitStack() as ph:
            def sb(name, shape, dt_):
                return ph.enter_context(nc.sbuf_tensor('s_' + name, list(shape), dt_, side='left'))
            raw2 = [sb('raw%d' % i, [128, S + 4], F32) for i in range(2)]
            cv = sb('cv', [128, S], F32)
            scr4 = sb('scr4', [128, 1024], F32)
            xsT = [scr4[:, :].bitcast(BF16)] * 2
            convw = sb('convw', [128, 6, 5], F32)
            convb = sb('convb', [128, 6], F32)
            dtb = sb('dtb', [128, 256], F32)
            alog = sb('alog', [128, 256], F32)
            dtx = scr4[:, 0:256]
            tA = scr4[:, 256:512]
            tB = scr4[:, 512:768]
            tC = scr4[:, 768:1024]
            csx = cv[:, 0:512]

            for dst, src, key in ((convw, convw_d, 'convw'), (convb, convb_d, 'convb'), (dtb, dtb_d, 'dtb'), (alog, alog_d, 'alog'),
                                  (dskip, dskip_d, 'dskip'), (ssmg, ssmg_d, 'ssmg')):
                P.op('sp', (lambda dst, src: lambda e: e.dma_start(out=dst[:], in_=src))(dst, src), writes=[key], dma='cst')
            for rb_ in range(2):
                P.op('dve', (lambda rb_: lambda e: e.memset(raw2[rb_][:, 0:2], 0.0))(rb_), writes=[('rawpadL', rb_)])
                P.op('dve', (lambda rb_: lambda e: e.memset(raw2[rb_][:, S + 2:S + 4], 0.0))(rb_), writes=[('rawpadR', rb_)])
            ev = [0]
            for t in range(NT):
                bk = ev[0] % 2
                ev[0] += 1
                for c in range(8):
                    P.op('pe', (lambda bk, c, t: lambda e: e.matmul(PS[bk][:, :], hT[:, c, t * 128:(t + 1) * 128], WP[wz][:, c, :], start=(c == 0), stop=(c == 7)))(bk, c, t),
                         reads=[('wp', wz), ('hT', c, t)], writes=[psr(bk)])
                P.op('act', (lambda bk, t: lambda e: e.activation(out=zs[:, t, :], in_=PS[bk][:, :], func=AF.Silu))(bk, t), reads=[psr(bk)], writes=[('zs', t)])
            for t in range(NT):
                for c in range(8):
                    P.op('pe', (lambda c, t: lambda e: e.matmul(PS[2][:, t * 16:(t + 1) * 16], hT[:, c, t * 128:(t + 1) * 128], WP[wb5][:, c, 256:272], start=(c == 0), stop=(c == 7)))(c, t),
                         reads=[('wp', wb5), ('hT', c, t)], writes=[psr(2)])
            P.op('dve', lambda e: e.tensor_tensor(dtx[:], PS[2][:, 0:256], dtb[:], ALU.add), reads=[psr(2), 'dtb'], writes=['dtx'])
            P.op('act', lambda e: e.activation(out=tA[:], in_=dtx[:], func=AF.Abs), reads=['dtx'], writes=['tA'])
            P.op('act', lambda e: e.activation(out=tB[:], in_=tA[:], func=AF.Exp, scale=-1.0), reads=['tA'], writes=['tB'])
            P.op('act', lambda e: e.activation(out=tC[:], in_=tB[:], func=AF.Ln, bias=1.0), reads=['tB'], writes=['tC'])
            P.op('dve', lambda e: e.scalar_tensor_tensor(dt[:], dtx[:], 0.0, tC[:], ALU.max, ALU.add), reads=['dtx', 'tC'], writes=['dt'])
            P.op('act', lambda e: e.activation(out=lndt[:], in_=dt[:], func=AF.Ln), reads=['dt'], writes=['lndt'])
            P.op('act', lambda e: e.activation(out=tA[:], in_=alog[:], func=AF.Exp), reads=['alog', 'tA'], writes=['tA2'])
            P.op('dve', lambda e: e.scalar_tensor_tensor(dA[:], tA[:], -1.0, dt[:], ALU.mult, ALU.mult), reads=['tA2', 'dt'], writes=['dA'])
            dA3 = dA[:].rearrange("p (t k) -> p t k", t=NT)
            for t in range(NT):
                for kind, (tri, key, off) in enumerate(((tri_ip, 'tri_ip', 0), (tri_es, 'tri_es', 0), (tri_is, 'tri_is', 8), (tri_ep, 'tri_ep', 8))):
                    P.op('pe', (lambda t, kind, tri, off: lambda e: e.matmul(PS[3][:, t * 32 + kind * 8:t * 32 + kind * 8 + 8], tri[:], dA3[:, t, off:off + 8], start=True, stop=True))(t, kind, tri, off),
                         reads=['dA', key], writes=[psr(3)])
                P.op('pe', (lambda t: lambda e: e.matmul(PS[4][:, t * 16:(t + 1) * 16], ones_f[:], dA3[:, t, :], start=True, stop=True))(t),
                     reads=['dA', 'ones_f'], writes=[psr(4)])
            P.op('dve', lambda e: e.tensor_copy(csall[:], PS[3][:, :]), reads=[psr(3)], writes=['csall'])
            P.op('act', lambda e: e.activation(out=expT[:], in_=PS[4][:, 0:256], func=AF.Exp), reads=[psr(4)], writes=['expT'])
            cs4 = csall[:].rearrange("p (t k r) -> p t k r", t=NT, k=4)
            dt4 = dt[:].rearrange("p (t d r) -> p t d r", t=NT, d=2)
            ln4 = lndt[:].rearrange("p (t d r) -> p t d r", t=NT, d=2)
            bf4 = bias_fb[:].rearrange("p (t d r) -> p t d r", t=NT, d=2)
            sd4 = sdec[:].rearrange("p (t d r) -> p t d r", t=NT, d=2)
            eo4 = eoff[:].rearrange("p (t d r) -> p t d r", t=NT, d=2)
            cx4 = csx[:].rearrange("p (t k r) -> p t k r", t=NT, k=4)
            P.op('act', lambda e: e.activation(out=csx[:], in_=csall[:], func=AF.Exp), reads=['csall'], writes=['csx'])
            for d_, kcs, kst in ((0, 0, 1), (1, 2, 3)):
                P.op('dve', (lambda d_, kcs: lambda e: e.tensor_tensor(bf4[:, :, d_, :], ln4[:, :, d_, :], cs4[:, :, kcs, :], ALU.subtract))(d_, kcs), reads=['lndt', 'csall'], writes=[('bias_fb', d_)])
                P.op('dve', (lambda d_, kst: lambda e: e.tensor_tensor(sd4[:, :, d_, :], cx4[:, :, kst, :], dt4[:, :, d_, :], ALU.mult))(d_, kst), reads=['csx', 'dt'], writes=[('sdec', d_)])
                P.op('dve', (lambda d_, kcs: lambda e: e.tensor_copy(eo4[:, :, d_, :], cx4[:, :, kcs, :]))(d_, kcs), reads=['csx'], writes=[('eoff', d_)])
            def xbc_p(cc):
                raw = raw2[cc % 2]
                rkey = ('raw', cc % 2)
                wi = wx if cc < 4 else wb5
                off = (cc % 4) * 128 if cc < 4 else (cc - 4) * 128
                for Q in range(4):
                    bk = ev[0] % 2
                    ev[0] += 1
                    for c in range(8):
                        P.op('pe', (lambda bk, wi, off, c, Q: lambda e: e.matmul(PS[bk][:, :], WP[wi][:, c, off:off + 128], hT[:, c, Q * 512:(Q + 1) * 512], start=(c == 0), stop=(c == 7)))(bk, wi, off, c, Q),
                             reads=[('wp', wi)] + hT_reads(c, Q), writes=[psr(bk)])
                    P.op('act', (lambda bk, Q, raw: lambda e: e.activation(out=raw[:, 2 + Q * 512:2 + (Q + 1) * 512], in_=PS[bk][:, :], func=AF.Copy))(bk, Q, raw), reads=[psr(bk)], writes=[rkey])

            def xbc_c(cc):
                raw = raw2[cc % 2]
                rkey = ('raw', cc % 2)
                wi = wx if cc < 4 else wb5
                off = (cc % 4) * 128 if cc < 4 else (cc - 4) * 128
                P.op('dve', (lambda cc, raw: lambda e: e.tensor_scalar(cv[:], raw[:, 0:S], convw[:, cc, 0:1], None, ALU.mult))(cc, raw), reads=[rkey, ('rawpadL', cc % 2), ('rawpadR', cc % 2), 'convw'], writes=['cv', 'csx'])
                for j in range(1, 5):
                    P.op('dve', (lambda cc, j, raw: lambda e: e.scalar_tensor_tensor(cv[:], raw[:, j:j + S], convw[:, cc, j:j + 1], cv[:], ALU.mult, ALU.add))(cc, j, raw), reads=[rkey, 'cv', 'convw'], writes=['cv'])

            def xbc_s(cc):
                raw = raw2[cc % 2]
                rkey = ('raw', cc % 2)
                wi = wx if cc < 4 else wb5
                off = (cc % 4) * 128 if cc < 4 else (cc - 4) * 128
                if cc < 4:
                    xb = cc % 2
                    P.op('act', (lambda xb, cc: lambda e: e.activation(out=xsT[xb][:], in_=cv[:], func=AF.Silu, bias=convb[:, cc:cc + 1]))(xb, cc), reads=['cv', 'convb'], writes=[('xsT', 0), 'dtx', 'tA', 'tB', 'tC', 'tA2'])
                    for t in range(NT):
                        P.op('pe', (lambda xb, t, cc: lambda e: e.matmul(PS[5 + (t // 4) % 2][:, (t % 4) * 128:(t % 4 + 1) * 128], xsT[xb][:, t * 128:(t + 1) * 128], ident_b[:], start=True, stop=True))(xb, t, cc),
                             reads=[('xsT', 0), 'ident_b'], writes=[psr(5 + (t // 4) % 2)])
                        if t % 4 == 3:
                            t0_ = t - 3
                            P.op('dve', (lambda t0_, t, cc: lambda e: e.tensor_copy(xs_tok[:, t0_:t0_ + 4, cc * 128:(cc + 1) * 128], PS[5 + (t // 4) % 2][:, :].rearrange("p (a d) -> p a d", a=4)))(t0_, t, cc),
                                 reads=[psr(5 + (t // 4) % 2)], writes=[('xs_tok', cc)])
                else:
                    tgt, tkey = (BTm, 'BTm') if cc == 4 else (CTm, 'CTm')
                    P.op('act', (lambda cc, tgt: lambda e: e.activation(out=tgt[:, 0, :], in_=cv[:], func=AF.Silu, bias=convb[:, cc:cc + 1]))(cc, tgt), reads=['cv', 'convb'], writes=[(tkey, 0)])
                    if cc == 4:
                        for t in range(NT):
                            P.op('pe', (lambda t: lambda e: e.matmul(PS[5 + (t // 4) % 2][:, (t % 4) * 128:(t % 4 + 1) * 128], BTm[:, 0, t * 128:(t + 1) * 128], ident_b[:], start=True, stop=True))(t),
                                 reads=[('BTm', 0), 'ident_b'], writes=[psr(5 + (t // 4) % 2)])
                            if t % 4 == 3:
                                t0_ = t - 3
                                P.op('dve', (lambda t0_, t: lambda e: e.tensor_copy(B_tok[:, t0_:t0_ + 4, :], PS[5 + (t // 4) % 2][:, :].rearrange("p (a d) -> p a d", a=4)))(t0_, t),
                                     reads=[psr(5 + (t // 4) % 2)], writes=['B_tok'])
                    P.op('dve', (lambda tgt: lambda e: e.tensor_copy(tgt[64:128, 1, :], tgt[64:128, 0, :]))(tgt), reads=[(tkey, 0)], writes=[(tkey, 1)])
                    P.op('dve', (lambda tgt: lambda e: e.memset(tgt[0:64, 1, :], 0.0))(tgt), reads=[], writes=[(tkey, 1)])
                    P.op('dve', (lambda tgt: lambda e: e.memset(tgt[64:128, 0, :], 0.0))(tgt), reads=[], writes=[(tkey, 0)])

            xbc_p(0)
            for cc in range(6):
                if cc + 1 < 6:
                    xbc_p(cc + 1)
                xbc_c(cc)
                xbc_s(cc)
            P.emit_phase()
        hstack.close()
        if stop_after == 'C1':
            for nm, tl in (('dt', dt), ('dA', dA), ('csall', csall), ('bias_fb', bias_fb), ('eoff', eoff), ('sdec', sdec), ('expT', expT)):
                dump(nm, tl[:], [])
            P.emit_phase()
            P.final_wait_all_dma()
            cst.close()
            mixer.close()
            bcst.close()
            return nc

        with contextlib.ExitStack() as ph:
            def sb(name, shape, dt_):
                return ph.enter_context(nc.sbuf_tensor('s_' + name, list(shape), dt_, side='left'))
            wo = [wp_next(), wp_next()]
            for hf in range(2):
                load_w(wo[hf], wout_d[:, hf * 512:(hf + 1) * 512], 512)
            Sin = sb('Sin', [128, 2, NT, 256], BF16)
            Srun = sb('Srun', [128, 2, 256], F32)
            Stmp = sb('Stmp', [128, 256], F32)
            Tdec = sb('Tdec', [128, NT, 2, 4], F32)
            Xd = sb('Xd', [128, 2, 512], BF16)
            DI = sb('DI', [128, 8, 128], BF16)
            dAbc = [sb('dAbc%d' % i, [128, 16, 128], F32) for i in range(2)]
            Lt = [sb('Lt%d' % i, [128, 16, 128], F32) for i in range(2)]
            Gsb = [sb('Gsb%d' % i, [128, 256], F32) for i in range(2)]
            Mt = Xd[:, :, :].rearrange("p a (b d) -> p (a b) d", b=4)
            ysb = sb('ysb', [128, 512], F32)
            ytmp = sb('ytmp', [128, 512], F32)
            junkc = sb('junkc', [128, 256], F32)
            ssdo = sb('ssdo', [128, 512], BF16)
            ssg = sb('ssg', [128, 32], F32)
            msg = sb('msg', [128, 32], F32)
            sqg = sb('sqg', [128, 32], F32)
            rsg = sb('rsg', [128, 32], F32)
            ex4 = expT[:].rearrange("p (t d r) -> p t d r", t=NT, d=2)
            sd3 = sdec[:].rearrange("p (t k) -> p t k", t=NT)
            bf3 = bias_fb[:].rearrange("p (t k) -> p t k", t=NT)
            eo3 = eoff[:].rearrange("p (t k) -> p t k", t=NT)
            dA3 = dA[:].rearrange("p (t k) -> p t k", t=NT)
            for d_ in range(2):
                P.op('dve', (lambda d_: lambda e: e.tensor_copy(Tdec[0:64, :, d_, :], ex4[0:64, :, d_, 0:4]))(d_), reads=['expT'], writes=['Tdec'])
                P.op('dve', (lambda d_: lambda e: e.tensor_copy(Tdec[64:128, :, d_, :], ex4[64:128, :, d_, 4:8]))(d_), reads=['expT'], writes=['Tdec'])
            for r in range(8):
                P.op('dve', (lambda r: lambda e: e.tensor_scalar(DI[:, r, :], ident_f[:], dskip[:, r:r + 1], None, ALU.mult))(r), reads=['ident_f', 'dskip'], writes=['DI'])
            P.op('dve', lambda e: e.memset(Srun[:], 0.0), writes=['Srun'])
            P.op('dve', lambda e: e.memset(Sin[:, 0, 0, :], 0.0), writes=[('Sin', 0, 0)])
            P.op('dve', lambda e: e.memset(Sin[:, 1, NT - 1, :], 0.0), writes=[('Sin', 1, NT - 1)])
            P.op('dve', lambda e: e.memset(ssg[:], 0.0), writes=['ssg'])
            for d_, order in ((0, range(0, NT - 1)), (1, range(NT - 1, 0, -1))):
                for t in order:
                    P.op('dve', (lambda d_, t: lambda e: e.tensor_tensor(Xd[:, d_, :].rearrange("p (r d) -> p r d", r=8), xs_tok[:, t, :].rearrange("p (r d) -> p r d", r=8),
                                                                      sd3[:, t, d_ * 8:(d_ + 1) * 8].unsqueeze(2).to_broadcast([128, 8, 64]), ALU.mult))(d_, t),
                         reads=[('xs_tok', c) for c in range(4)] + [('sdec', d_)], writes=[('Xd', d_)])
                    bk = 6 + d_
                    P.op('pe', (lambda bk, t, d_: lambda e: e.matmul(PS[bk][:, :], B_tok[:, t, :], Xd[:, d_, :], start=True, stop=True))(bk, t, d_),
                         reads=['B_tok', ('Xd', d_)], writes=[psr(bk)])
                    P.op('dve', (lambda d_, t: lambda e: e.tensor_tensor(Stmp[:].rearrange("p (r d) -> p r d", r=4), Srun[:, d_, :].rearrange("p (r d) -> p r d", r=4),
                                                                      Tdec[:, t, d_, :].unsqueeze(2).to_broadcast([128, 4, 64]), ALU.mult))(d_, t),
                         reads=['Srun', 'Tdec'], writes=['Stmp'])
                    P.op('dve', (lambda bk, d_: lambda e: e.tensor_tensor(Srun[0:64, d_, :], Stmp[0:64, :], PS[bk][0:64, 0:256], ALU.add))(bk, d_), reads=['Stmp', psr(bk)], writes=['Srun'])
                    P.op('dve', (lambda bk, d_: lambda e: e.tensor_tensor(Srun[64:128, d_, :], Stmp[64:128, :], PS[bk][64:128, 256:512], ALU.add))(bk, d_), reads=['Stmp', psr(bk)], writes=['Srun'])
                    tn = t + 1 if d_ == 0 else t - 1
                    P.op('act', (lambda d_, tn: lambda e: e.activation(out=Sin[:, d_, tn, :], in_=Srun[:, d_, :], func=AF.Copy))(d_, tn), reads=['Srun'], writes=[('Sin', d_, tn)])
            def stage_xa(t):
                db = t % 2
                P.op('dve', lambda e: e.tensor_copy(dAbc[db][:], dA3[:, t, :].unsqueeze(2).to_broadcast([128, 16, 128])), reads=['dA'], writes=[('dAbc', db)])

            def stage_xd(t, d_):
                db = t % 2
                if d_ == 0:
                    for g in range(2):
                        P.op('pe', (lambda g: lambda e: e.matmul(PS[0][:, g * 128:(g + 1) * 128], BTm[:, g, t * 128:(t + 1) * 128], CTm[:, g, t * 128:(t + 1) * 128], start=True, stop=True))(g),
                             reads=[('BTm', g), ('CTm', g)], writes=[psr(0)])
                    P.op('act', lambda e: e.activation(out=Gsb[db][:], in_=PS[0][:, 0:256], func=AF.Copy), reads=[psr(0)], writes=[('Gsb', db)])
                tri, tkey, ngm, nkey = (tri_ip, 'tri_ip', negm_f, 'negm_f') if d_ == 0 else (tri_is, 'tri_is', negm_b, 'negm_b')
                for r in range(8):
                    bk = 1 + d_ * 2 + r // 4
                    sl = slice((r % 4) * 128, (r % 4 + 1) * 128)
                    P.op('pe', (lambda bk, sl, r, tri: lambda e: e.matmul(PS[bk][:, sl], dAbc[db][:, d_ * 8 + r, :], tri[:], start=True, stop=False))(bk, sl, r, tri),
                         reads=[('dAbc', db), tkey], writes=[psr(bk)])
                    P.op('pe', (lambda bk, sl, ngm: lambda e: e.matmul(PS[bk][:, sl], ident_f[:], ngm[:], start=False, stop=True))(bk, sl, ngm),
                         reads=['ident_f', nkey], writes=[psr(bk)])
                for r in range(8):
                    bk = 1 + d_ * 2 + r // 4
                    sl = slice((r % 4) * 128, (r % 4 + 1) * 128)
                    P.op('act', (lambda bk, sl, r: lambda e: e.activation(out=Lt[db][:, d_ * 8 + r, :], in_=PS[bk][:, sl], func=AF.Exp, bias=bf3[:, t, d_ * 8 + r:d_ * 8 + r + 1]))(bk, sl, r),
                         reads=[psr(bk), ('bias_fb', d_)], writes=[('Lt', db, d_, r)])

            def stage_y1(t):
                db = t % 2
                P.op('dve', lambda e: e.tensor_tensor(Lt[db][:, 0:8, :], Lt[db][:, 0:8, :], Lt[db][:, 8:16, :], ALU.add), reads=[('Lt', db, dd, rr_) for dd in range(2) for rr_ in range(8)], writes=[('Lt', db, 0, rr_) for rr_ in range(8)])
                for g in range(2):
                    P.op('dve', (lambda g: lambda e: e.tensor_tensor(Mt[:, g * 4:(g + 1) * 4, :], Lt[db][:, g * 4:(g + 1) * 4, :], Gsb[db][:, g * 128:(g + 1) * 128].unsqueeze(1).to_broadcast([128, 4, 128]), ALU.mult))(g),
                         reads=[('Lt', db, 0, rr_) for rr_ in range(8)] + [('Gsb', db)], writes=[('Mt', g), ('Xd', 0), ('Xd', 1)])

            def stage_y2(t):
                for r in range(8):
                    P.op('pe', (lambda r: lambda e: e.matmul(PS[5][:, r * 64:(r + 1) * 64], Mt[:, r, :], xs_tok[:, t, r * 64:(r + 1) * 64], start=True, stop=False))(r),
                         reads=[('Mt', r // 4)] + [('xs_tok', c) for c in range(4)], writes=[psr(5)])
                    P.op('pe', (lambda r: lambda e: e.matmul(PS[5][:, r * 64:(r + 1) * 64], DI[:, r, :], xs_tok[:, t, r * 64:(r + 1) * 64], start=False, stop=True))(r),
                         reads=['DI'] + [('xs_tok', c) for c in range(4)], writes=[psr(5)])
                for d_ in range(2):
                    for g in range(2):
                        P.op('pe', (lambda d_, g: lambda e: e.matmul(PS[6 + d_][:, g * 256:(g + 1) * 256], CTm[:, g, t * 128:(t + 1) * 128], Sin[:, d_, t, :], start=True, stop=True))(d_, g),
                             reads=[('CTm', g), ('Sin', d_, t)], writes=[psr(6 + d_)])
                P.op('act', lambda e: e.activation(out=ysb[:], in_=PS[5][:, :], func=AF.Copy), reads=[psr(5)], writes=['ysb'])

            def stage_y3(t):
                for d_ in range(2):
                    P.op('dve', (lambda d_: lambda e: e.tensor_tensor(ytmp[:].rearrange("p (r d) -> p r d", r=8), PS[6 + d_][:, :].rearrange("p (r d) -> p r d", r=8),
                                                                   eo3[:, t, d_ * 8:(d_ + 1) * 8].unsqueeze(2).to_broadcast([128, 8, 64]), ALU.mult))(d_),
                         reads=[psr(6 + d_), ('eoff', d_)], writes=['ytmp'])
                    P.op('dve', lambda e: e.tensor_tensor(ysb[:], ysb[:], ytmp[:], ALU.add), reads=['ysb', 'ytmp'], writes=['ysb'])
                P.op('dve', lambda e: e.tensor_tensor(ysb[:], ysb[:], zs[:, t, :], ALU.mult), reads=['ysb', ('zs', t)], writes=['ysb'])
                for g in range(2):
                    col = t * 2 + g
                    P.op('dve', (lambda g: lambda e: e.tensor_tensor(junkc[:], ysb[:, g * 256:(g + 1) * 256], ysb[:, g * 256:(g + 1) * 256], ALU.mult))(g), reads=['ysb'], writes=['junkc'])
                    P.op('dve', (lambda col: lambda e: e.reduce_sum(ssg[:, col:col + 1], junkc[:], axis=AX.X))(col), reads=['junkc'], writes=[('ssg', col)])
                sl2 = slice(t * 2, t * 2 + 2)
                P.op('dve', lambda e: e.tensor_scalar(msg[:, sl2], ssg[:, sl2], 1.0 / 256, EPS, ALU.mult, ALU.add), reads=[('ssg', t * 2), ('ssg', t * 2 + 1)], writes=[('msg', t)])

            def stage_z1(t):
                sl2 = slice(t * 2, t * 2 + 2)
                P.op('act', lambda e: e.activation(out=sqg[:, sl2], in_=msg[:, sl2], func=AF.Ln), reads=[('msg', t)], writes=[('sqg', t)])
                P.op('act', lambda e: e.activation(out=rsg[:, sl2], in_=sqg[:, sl2], func=AF.Exp, scale=-0.5), reads=[('sqg', t)], writes=[('rsg', t)])
                for g in range(2):
                    col = t * 2 + g
                    P.op('dve', (lambda g, col: lambda e: e.scalar_tensor_tensor(ssdo[:, g * 256:(g + 1) * 256], ysb[:, g * 256:(g + 1) * 256], rsg[:, col:col + 1], ssmg[:, g * 256:(g + 1) * 256], ALU.mult, ALU.mult))(g, col),
                         reads=['ysb', ('rsg', t), 'ssmg'], writes=['ssdo'])

            def stage_z2(t):
                for cc in range(4):
                    P.op('pe', (lambda cc: lambda e: e.matmul(PS[0][:, cc * 128:(cc + 1) * 128], ssdo[:, cc * 128:(cc + 1) * 128], ident_b[:], start=True, stop=True))(cc),
                         reads=['ssdo', 'ident_b'], writes=[psr(0)])
                P.op('dve', lambda e: e.tensor_copy(mixT[:, 4:8, t * 128:(t + 1) * 128], PS[0][:, :].rearrange("p (a d) -> p a d", a=4)),
                     reads=[psr(0)], writes=[('mixT', 4 + c, t) for c in range(4)])

            NTc = NT if stop_after != 'C2a' else 0
            if NTc:
                stage_xa(0)
                stage_xd(0, 0)
                stage_xd(0, 1)
            for i in range(NTc + 1):
                if 1 <= i:
                    stage_z1(i - 1)
                if i + 1 < NTc:
                    stage_xa(i + 1)
                if i < NTc:
                    stage_y1(i)
                if i + 1 < NTc:
                    stage_xd(i + 1, 0)
                if 1 <= i:
                    stage_z2(i - 1)
                if i < NTc:
                    stage_y2(i)
                if i + 1 < NTc:
                    stage_xd(i + 1, 1)
                if i < NTc:
                    stage_y3(i)
            P.emit_phase()
        cst.close()
        if stop_after in ('C', 'C2a'):
            with contextlib.ExitStack() as ph:
                tmp = ph.enter_context(nc.sbuf_tensor('s_dbgtmp', [128, S], F32, side='left'))
                for c in range(8):
                    P.op('dve', (lambda c: lambda e: e.tensor_copy(tmp[:], mixT[:, c, :]))(c), reads=[('mixT', c, t) for t in range(NT)], writes=['dbgtmp'])
                    P.op('sp', (lambda c: lambda e: e.dma_start(out=dbg_d['mixT'][c], in_=tmp[:]))(c), reads=['dbgtmp'], dma='dbg')
                P.emit_phase()
            P.final_wait_all_dma()
            mixer.close()
            bcst.close()
            return nc

        xres = sbT('xres', [128, NT, D], F32)
        h2 = sbT('h2', [128, NT, D], BF16)
        aff = sbT('aff', [128, NT, 16], F32)
        sel = sbT('sel', [128, NT, 16], F32)
        pos = sbT('pos', [128, NT, 16], F32)
        with contextlib.ExitStack() as ph:
            def sb(name, shape, dt_):
                return ph.enter_context(nc.sbuf_tensor('s_' + name, list(shape), dt_, side='left'))
            tmpm = sb('tmpm', [128, 512], F32)
            xn2 = sb('xn2', [128, D], F32)
            h2f = [sb('h2f%d' % i, [128, D], F32) for i in range(2)]
            h2Tf = sb('h2Tf', [128, 8, 128], F32)
            rw = sb('rw', [128, 8, 16], F32)
            ss2 = sb('ss2', [128, 16], F32)
            ms2 = sb('ms2', [128, 16], F32)
            sq2 = sb('sq2', [128, 16], F32)
            rs2 = sb('rs2', [128, 16], F32)
            mx = sb('mx', [128, 16], F32)
            nmx = sb('nmx', [128, 16], F32)
            sme = sb('sme', [128, 16], F32)
            rsm = sb('rsm', [128, 16], F32)
            eaf = sb('eaf', [128, NT, 16], F32)
            P.op('sp', lambda e: e.dma_start(out=rw[:], in_=rw_d), writes=['rw'], dma='cst')
            P.op('dve', lambda e: e.memset(ss2[:], 0.0), writes=['ss2'])
            P.op('dve', lambda e: e.memset(sme[:], 0.0), writes=['sme'])
            def d1_xpe(t):
                P.op('sp', (lambda t: lambda e: e.dma_start(out=xres[:, t, :], in_=x_d[t * 128:(t + 1) * 128, :]))(t), writes=[('xres', t)], dma=('xres', t))
                for hf in range(2):
                    bk = hf
                    for c in range(8):
                        P.op('pe', (lambda bk, hf, c: lambda e: e.matmul(PS[bk][:, :], mixT[:, c, t * 128:(t + 1) * 128], WP[wo[hf]][:, c, :], start=(c == 0), stop=(c == 7)))(bk, hf, c),
                             reads=[('wp', wo[hf]), ('mixT', c, t)], writes=[psr(bk)])

            def d1_xch(t):
                hb = t % 2
                for hf in range(2):
                    bk = hf
                    P.op('dve', (lambda bk, hf: lambda e: e.tensor_tensor(tmpm[:], PS[bk][:, :], bc3[:, 0, hf * 512:(hf + 1) * 512], ALU.mult))(bk, hf), reads=[psr(bk), ('bc4', 0)], writes=['tmpm'])
                    P.op('dve', (lambda hf: lambda e: e.tensor_tensor(xres[:, t, hf * 512:(hf + 1) * 512], xres[:, t, hf * 512:(hf + 1) * 512], tmpm[:], ALU.add))(hf), reads=['tmpm', ('xres', t)], writes=[('xres', t)])
                P.op('act', lambda e: e.activation(out=xn2[:], in_=xres[:, t, :], func=AF.Square, accum_out=ss2[:, t:t + 1]), reads=[('xres', t), 'ss2'], writes=['xn2', ('ss2', t)])
                P.op('dve', lambda e: e.tensor_scalar(ms2[:, t:t + 1], ss2[:, t:t + 1], 1.0 / D, EPS, ALU.mult, ALU.add), reads=[('ss2', t)], writes=[('ms2', t)])
                P.op('act', lambda e: e.activation(out=sq2[:, t:t + 1], in_=ms2[:, t:t + 1], func=AF.Sqrt), reads=[('ms2', t)], writes=[('sq2', t)])
                P.op('dve', lambda e: e.reciprocal(rs2[:, t:t + 1], sq2[:, t:t + 1]), reads=[('sq2', t)], writes=[('rs2', t)])
                P.op('dve', lambda e: e.scalar_tensor_tensor(xn2[:], xres[:, t, :], rs2[:, t:t + 1], bc3[:, 2, :], ALU.mult, ALU.mult), reads=[('xres', t), ('rs2', t), ('bc4', 2)], writes=['xn2'])
                P.op('dve', lambda e: e.tensor_tensor(h2f[hb][:], xn2[:], bc3[:, 1, :], ALU.add), reads=['xn2', ('bc4', 1)], writes=[('h2f', hb)])
                P.op('act', lambda e: e.activation(out=h2[:, t, :], in_=h2f[hb][:], func=AF.Copy), reads=[('h2f', hb)], writes=[('h2', t)])

            def d1_ytr(t):
                hb = t % 2
                for c in range(8):
                    bk = 2 + c // 4
                    P.op('pe', (lambda bk, c: lambda e: e.transpose(PS[bk][:, (c % 4) * 128:(c % 4 + 1) * 128], h2f[hb][:, c * 128:(c + 1) * 128], ident_f[:]))(bk, c), reads=[('h2f', hb), 'ident_f'], writes=[psr(bk)])
                P.op('act', lambda e: e.activation(out=h2Tf[:, 0:4, :], in_=PS[2][:, :].rearrange("p (a d) -> p a d", a=4), func=AF.Copy), reads=[psr(2)], writes=[('h2Tf', 0)])
                P.op('dve', lambda e: e.tensor_copy(h2Tf[:, 4:8, :], PS[3][:, :].rearrange("p (a d) -> p a d", a=4)), reads=[psr(3)], writes=[('h2Tf', 1)])

            def d1_yrt(t):
                for c in range(8):
                    P.op('pe', (lambda c: lambda e: e.matmul(PS[4][:, t * 16:(t + 1) * 16], h2Tf[:, c, :], rw[:, c, :], start=(c == 0), stop=(c == 7)))(c), reads=[('h2Tf', c // 4), 'rw'], writes=[psr(4)])

            d1_xpe(0)
            d1_xch(0)
            for t in range(NT):
                if t + 1 < NT:
                    d1_xpe(t + 1)
                d1_ytr(t)
                if t + 1 < NT:
                    d1_xch(t + 1)
                d1_yrt(t)
            lg3 = PS[4][:, 0:256].rearrange("p (t k) -> p t k", t=NT)
            P.op('dve', lambda e: e.reduce_max(mx[:], lg3, axis=AX.X), reads=[psr(4)], writes=['mx'])
            P.op('dve', lambda e: e.tensor_scalar(nmx[:], mx[:], -1.0, None, ALU.mult), reads=['mx'], writes=['nmx'])
            for t in range(NT):
                P.op('act', (lambda t: lambda e: e.activation(out=eaf[:, t, :], in_=PS[4][:, t * 16:(t + 1) * 16], func=AF.Exp, bias=nmx[:, t:t + 1], accum_out=sme[:, t:t + 1]))(t),
                     reads=[psr(4), 'nmx', 'sme'], writes=[('eaf', t), ('sme', t)])
            P.op('dve', lambda e: e.reciprocal(rsm[:], sme[:]), reads=[('sme', t) for t in range(NT)], writes=['rsm'])
            P.op('dve', lambda e: e.tensor_tensor(aff[:], eaf[:], rsm[:].unsqueeze(2).to_broadcast([128, NT, 16]), ALU.mult), reads=[('eaf', t) for t in range(NT)] + ['rsm'], writes=['aff'])
            dump('x2', xres[:, :, :], [('xres', t) for t in range(NT)])
            dump('aff', aff[:, :, :], ['aff'])
            P.emit_phase()
        mixer.close()
        bcst.close()

        gsel = sbT('gsel', [128, NT, 16], F32)
        wpx_stack = contextlib.ExitStack()
        NWX = 3
        for i_ in range(NWX):
            WP.append(wpx_stack.enter_context(nc.sbuf_tensor('s_wpx%d' % i_, [128, 8, 512], BF16, side='left')))
        with contextlib.ExitStack() as ph:
            def sb(name, shape, dt_):
                return ph.enter_context(nc.sbuf_tensor('s_' + name, list(shape), dt_, side='left'))
            if stop_after in (None, 'E', 'E1'):
                for fb_ in range(len(WP) // 2):
                    for kind_ in ('g', 'u'):
                        prefetched[(0, kind_, fb_)] = issue_w(0, kind_, fb_)
            affT = sb('affT', [16, S], F32)
            work = sb('work', [16, S], F32)
            m8 = sb('m8', [16, 8], F32)
            csel = sb('csel', [128, NT, 16], F32)
            for t in range(NT):
                bk = t // 4
                P.op('pe', (lambda bk, t: lambda e: e.transpose(PS[bk][0:16, (t % 4) * 128:(t % 4 + 1) * 128], aff[:, t, :], ident_f[:]))(bk, t), reads=['aff', 'ident_f'], writes=[psr(bk)])
            for bk in range(4):
                P.op('dve', (lambda bk: lambda e: e.tensor_copy(affT[:, bk * 512:(bk + 1) * 512], PS[bk][0:16, :]))(bk), reads=[psr(bk)], writes=['affT'])
            P.op('dve', lambda e: e.tensor_copy(work[:], affT[:]), reads=['affT'], writes=['work'])
            for it_ in range(CAP // 8):
                P.op('dve', lambda e: e.max(m8[:], work[:]), reads=['work'], writes=['m8'])
                if it_ < CAP // 8 - 1:
                    P.op('dve', lambda e: e.match_replace(work[:], m8[:], work[:], -1.0), reads=['work', 'm8'], writes=['work'])
            P.op('dve', lambda e: e.tensor_scalar(work[:], affT[:], m8[:, 7:8], None, ALU.is_ge), reads=['affT', 'm8', 'work'], writes=['work'])
            for t in range(NT):
                P.op('pe', (lambda t: lambda e: e.transpose(PS[4][:, t * 16:(t + 1) * 16], work[:, t * 128:(t + 1) * 128], ident_f[0:16, 0:16]))(t), reads=['work', 'ident_f'], writes=[psr(4)])
            P.op('dve', lambda e: e.tensor_copy(sel[:], PS[4][:, 0:256].rearrange("p (t k) -> p t k", t=NT)), reads=[psr(4)], writes=['sel'])
            P.op('dve', lambda e: e.memset(csel[:, 0, :], 0.0), writes=[('csel', 0)])
            for t in range(1, NT):
                P.op('dve', (lambda t: lambda e: e.tensor_tensor(csel[:, t, :], csel[:, t - 1, :], sel[:, t - 1, :], ALU.add))(t), reads=[('csel', t - 1), 'sel'], writes=[('csel', t)])
            for t in range(NT):
                P.op('pe', (lambda t: lambda e: e.matmul(PS[5][:, t * 16:(t + 1) * 16], tri_ep[:], sel[:, t, :], start=True, stop=False))(t), reads=['tri_ep', 'sel'], writes=[psr(5)])
                P.op('pe', (lambda t: lambda e: e.matmul(PS[5][:, t * 16:(t + 1) * 16], ones_f[:], csel[:, t, :], start=False, stop=True))(t), reads=['ones_f', ('csel', t)], writes=[psr(5)])
            P.op('dve', lambda e: e.tensor_copy(pos[:], PS[5][:, 0:256].rearrange("p (t k) -> p t k", t=NT)), reads=[psr(5)], writes=['pos'])
            P.op('dve', lambda e: e.tensor_tensor(gsel[:], aff[:], sel[:], ALU.mult), reads=['aff', 'sel'], writes=['gsel'])
            dump('sel', sel[:, :, :], ['sel'])
            dump('pos', pos[:, :, :], ['pos'])
            P.emit_phase()

        with contextlib.ExitStack() as ph:
            def sb(name, shape, dt_):
                return ph.enter_context(nc.sbuf_tensor('s_' + name, list(shape), dt_, side='left'))
            oh = [sb('oh%d' % i, [128, 256], BF16) for i in range(4)]
            ohg = [sb('ohg%d' % i, [128, 256], BF16) for i in range(4)]
            xgT = [sb('xgT0', [128, 8, 256], BF16)] * 2
            hact = sb('hact', [128, NFC, 256], BF16)
            ohT = [sb('ohT%d' % i, [128, 2, S], BF16) for i in range(2)]
            sg = [sb('sg%d' % i, [128, 256], F32) for i in range(2)]
            ye = sb('ye', [128, 2, D], BF16)
            print('E: sbuf bytes remaining after locals', nc.sbuf_bytes_remaining)
            nblk = [(i * 512, min(512, FF - i * 512)) for i in range(6)]
            ohc = [0]
            NE = NEXP if stop_after != 'E1' else 1

            def gather_units(ex):
                xb = ex % 2
                units = []
                obuf = {}

                def mk_oh(t):
                    ob = ohc[0] % 4
                    ohc[0] += 1
                    obuf[t] = ob
                    P.op('dve', (lambda ob: lambda e: e.tensor_scalar(oh[ob][:], iota_j[:], pos[:, t, ex:ex + 1], sel[:, t, ex:ex + 1], ALU.is_equal, ALU.mult))(ob),
                         reads=['iota_j', 'pos', 'sel'], writes=[('oh', ob)])
                    P.op('dve', (lambda ob: lambda e: e.tensor_scalar(ohg[ob][:], iota_j[:], pos[:, t, ex:ex + 1], gsel[:, t, ex:ex + 1], ALU.is_equal, ALU.mult))(ob),
                         reads=['iota_j', 'pos', 'gsel'], writes=[('ohg', ob)])

                def pre():
                    mk_oh(0)
                    mk_oh(1)
                    for b4_ in range(4):
                        P.op('pe', (lambda b4_: lambda e: e.matmul(PS[b4_][:, :], zeros_b[:, 0:128], zeros_b[:, :], start=True, stop=False))(b4_), reads=['zeros_b'], writes=[psr(b4_)])
                units.append(pre)
                for t in range(NT):
                    def u(t=t):
                        if t + 2 < NT:
                            mk_oh(t + 2)
                        ob = obuf[t]
                        for c in range(8):
                            P.op('pe', (lambda ob, c: lambda e: e.matmul(PS[c // 2][:, (c % 2) * 256:(c % 2 + 1) * 256], h2[:, t, c * 128:(c + 1) * 128], oh[ob][:], start=False, stop=(t == NT - 1 and c % 2 == 1)))(ob, c),
                                 reads=[('h2', t), ('oh', ob)], writes=[psr(c // 2)])
                        for jh in range(2):
                            P.op('pe', (lambda ob, jh: lambda e: e.matmul(PS[4 + jh][:, (t % 4) * 128:(t % 4 + 1) * 128], ohg[ob][:, jh * 128:(jh + 1) * 128], ident_b[:], start=True, stop=True))(ob, jh),
                                 reads=[('ohg', ob), 'ident_b'], writes=[psr(4 + jh)])
                        if t % 4 == 3:
                            t0_ = t - 3
                            P.op('act', (lambda t0_: lambda e: e.activation(out=ohT[xb][:, 0, t0_ * 128:(t0_ + 4) * 128], in_=PS[4][:, :], func=AF.Copy))(t0_),
                                 reads=[psr(4)], writes=[('ohT', xb, 0, t0_ // 4)])
                            P.op('dve', (lambda t0_: lambda e: e.tensor_copy(ohT[xb][:, 1, t0_ * 128:(t0_ + 4) * 128], PS[5][:, :]))(t0_),
                                 reads=[psr(5)], writes=[('ohT', xb, 1, t0_ // 4)])
                    units.append(u)

                def fin():
                    for b4 in range(4):
                        if b4 % 2 == 0:
                            P.op('act', (lambda b4: lambda e: e.activation(out=xgT[xb][:, 2 * b4:2 * b4 + 2, :], in_=PS[b4][:, :].rearrange("p (a d) -> p a d", a=2), func=AF.Copy))(b4), reads=[psr(b4)], writes=[('xgT', 0, b4)])
                        else:
                            P.op('dve', (lambda b4: lambda e: e.tensor_copy(xgT[xb][:, 2 * b4:2 * b4 + 2, :], PS[b4][:, :].rearrange("p (a d) -> p a d", a=2)))(b4), reads=[psr(b4)], writes=[('xgT', 0, b4)])
                units.append(fin)
                return units

            def scatter_units(ex):
                xb = ex % 2
                units = []
                for t in range(NT):
                    for dh in range(2):
                        def u(t=t, dh=dh):
                            bk = 4 + (t * 2 + dh) % 2
                            for jh in range(2):
                                P.op('pe', (lambda bk, jh: lambda e: e.matmul(PS[bk][:, :], ohT[xb][:, jh, t * 128:(t + 1) * 128], ye[:, jh, dh * 512:(dh + 1) * 512], start=(jh == 0), stop=(jh == 1)))(bk, jh),
                                     reads=[('ohT', xb, jh, t // 4), ('ye', jh, dh)], writes=[psr(bk)])
                            P.op('dve', (lambda bk: lambda e: e.tensor_tensor(xres[:, t, dh * 512:(dh + 1) * 512], xres[:, t, dh * 512:(dh + 1) * 512], PS[bk][:, :], ALU.add))(bk),
                                 reads=[psr(bk), ('xres', t)], writes=[('xres', t)])
                        units.append(u)
                return units

            def ffn(ex, fillers):
                xb = ex % 2
                fillers = list(fillers)
                nfc_done = [0]
                nfill_total = [len(fillers)]
                nfill_emitted = [0]
                for fb, (f0, fw) in enumerate(nblk):
                    wgi = get_w(ex, 'g', fb)
                    wui = get_w(ex, 'u', fb)
                    for k in range(fw // 128):
                        fi = fb * 4 + k
                        bk = 6 + fi % 2
                        for c in range(8):
                            P.op('pe', (lambda bk, wgi, c, k: lambda e: e.matmul(PS[bk][:, 0:256], WP[wgi][:, c, k * 128:(k + 1) * 128], xgT[xb][:, c, :], start=(c == 0), stop=(c == 7)))(bk, wgi, c, k),
                                 reads=[('wp', wgi), ('xgT', 0, c // 2)], writes=[psr(bk)])
                        for c in range(8):
                            P.op('pe', (lambda bk, wui, c, k: lambda e: e.matmul(PS[bk][:, 256:512], WP[wui][:, c, k * 128:(k + 1) * 128], xgT[xb][:, c, :], start=(c == 0), stop=(c == 7)))(bk, wui, c, k),
                                 reads=[('wp', wui), ('xgT', 0, c // 2)], writes=[psr(bk)])
                        sgi = fi % 2
                        P.op('act', (lambda bk, sgi: lambda e: e.activation(out=sg[sgi][:], in_=PS[bk][:, 0:256], func=AF.Silu))(bk, sgi), reads=[psr(bk)], writes=[('sg', sgi)])
                        P.op('dve', (lambda bk, sgi, fi: lambda e: e.tensor_tensor(hact[:, fi, :], sg[sgi][:], PS[bk][:, 256:512], ALU.mult))(bk, sgi, fi), reads=[psr(bk), ('sg', sgi)], writes=[('hact', fi)])
                        nfc_done[0] += 1
                        want = (len(fillers) * 0 + nfill_total[0] * nfc_done[0] + NFC - 1) // NFC
                        while nfill_emitted[0] < want and fillers:
                            fillers.pop(0)()
                            nfill_emitted[0] += 1
                for fb in range(6):
                    nk = 4 if fb < 5 else 2
                    wdi = get_w(ex, 'd', fb)
                    for k in range(nk):
                        fi = fb * 4 + k
                        for jh in range(2):
                            for dh in range(2):
                                bk = jh * 2 + dh
                                P.op('pe', (lambda bk, wdi, fi, k, jh, dh: lambda e: e.matmul(PS[bk][:, :], hact[:, fi, jh * 128:(jh + 1) * 128], WP[wdi][:, 2 * k + dh, :], start=(fi == 0), stop=(fi == NFC - 1)))(bk, wdi, fi, k, jh, dh),
                                     reads=[('wp', wdi), ('hact', fi)], writes=[psr(bk)])
                for jh in range(2):
                    for dh in range(2):
                        bk = jh * 2 + dh
                        P.op('dve', (lambda bk, jh, dh: lambda e: e.tensor_tensor(ye[:, jh, dh * 512:(dh + 1) * 512], PS[bk][:, :], g2bc[:, dh * 512:(dh + 1) * 512], ALU.mult))(bk, jh, dh),
                             reads=[psr(bk), ('g2bc' if False else ('bc4', 3))], writes=[('ye', jh, dh)])

            for u in gather_units(0):
                u()
            for ex in range(NE):
                ffn(ex, scatter_units(ex - 1) if ex >= 1 else [])
                if ex + 1 < NE:
                    for u in gather_units(ex + 1):
                        u()
            for u in scatter_units(NE - 1):
                u()
            P.emit_phase()

        del WP[3:]
        wpx_stack.close()
        with contextlib.ExitStack() as ph:
            def sb(name, shape, dt_):
                return ph.enter_context(nc.sbuf_tensor('s_' + name, list(shape), dt_, side='left'))
            gfin = sb('gfin', [128, D], F32)
            ob_ = [sb('ob%d' % i, [128, D], F32) for i in range(2)]
            junkf = sb('junkf', [128, D], F32)
            ssf = sb('ssf', [128, 16], F32)
            msf = sb('msf', [128, 16], F32)
            sqf = sb('sqf', [128, 16], F32)
            rsf = sb('rsf', [128, 16], F32)
            P.op('sp', lambda e: e.dma_start(out=gfin[:], in_=gfin_d), writes=['gfin'], dma='cst')
            P.op('dve', lambda e: e.memset(ssf[:], 0.0), writes=['ssf'])
            for t in range(NT):
                b = t % 2
                P.op('act', (lambda t: lambda e: e.activation(out=junkf[:], in_=xres[:, t, :], func=AF.Square, accum_out=ssf[:, t:t + 1]))(t), reads=[('xres', t), 'ssf'], writes=['junkf', ('ssf', t)])
                P.op('dve', (lambda t: lambda e: e.tensor_scalar(msf[:, t:t + 1], ssf[:, t:t + 1], 1.0 / D, EPS, ALU.mult, ALU.add))(t), reads=[('ssf', t)], writes=[('msf', t)])
                P.op('act', (lambda t: lambda e: e.activation(out=sqf[:, t:t + 1], in_=msf[:, t:t + 1], func=AF.Sqrt))(t), reads=[('msf', t)], writes=[('sqf', t)])
                P.op('dve', (lambda t: lambda e: e.reciprocal(rsf[:, t:t + 1], sqf[:, t:t + 1]))(t), reads=[('sqf', t)], writes=[('rsf', t)])
                P.op('dve', (lambda b, t: lambda e: e.scalar_tensor_tensor(ob_[b][:], xres[:, t, :], rsf[:, t:t + 1], gfin[:], ALU.mult, ALU.mult))(b, t), reads=[('xres', t), ('rsf', t), 'gfin'], writes=[('ob', b)])
                P.op('sp', (lambda b, t: lambda e: e.dma_start(out=out_d[t * 128:(t + 1) * 128, :], in_=ob_[b][:]))(b, t), reads=[('ob', b)], dma=('ob', b))
            P.emit_phase()
        P.final_wait_all_dma()
    return nc


def _t5_bucket_static(rel):
    nb = 16
    ret = np.where(rel > 0, nb, 0)
    n = np.abs(rel)
    max_exact = nb // 2
    nf = np.maximum(n, 1).astype(np.float32)
    large = max_exact + (np.log(nf / max_exact) / math.log(128 / max_exact) * (nb - max_exact)).astype(np.int32)
    large = np.minimum(large, nb - 1)
    return ret + np.where(n < max_exact, n, large)


def _col(v, n):
    return np.ascontiguousarray(np.asarray(v, np.float32).reshape(n, 128).T)


def _rep(v):
    v = np.asarray(v, np.float32).reshape(1, -1)
    return np.ascontiguousarray(np.broadcast_to(v, (128, v.shape[1])))


def prep_inputs(inp):
    f = lambda a: np.ascontiguousarray(np.asarray(a, np.float32))
    sh = {}
    sh['ada_w'] = f(inp['ada_w'][0])
    ada_b = f(inp['ada_b'][0])
    sh['ada_brow'] = np.ascontiguousarray(ada_b.reshape(1, -1))
    sh['ada_bg'] = _rep(ada_b[2048:6144])
    sh['gmixT'] = _col(inp['norm_mix_g'][0], 8)
    sh['gffn_bc'] = _rep(inp['norm_ffn_g'][0])
    sh['gfin_bc'] = _rep(inp['norm_final_g'])
    sh['w_in'] = f(inp['w_in'][0])
    sh['lam_qk'] = _rep(np.concatenate([f(inp['lambda_q1'][0]), f(inp['lambda_k1'][0]), f(inp['lambda_q2'][0]), f(inp['lambda_k2'][0])]))
    sh['subln_bc'] = _rep(inp['attn_subln_g'][0])
    tab = f(inp['rel_bias_table'])
    kl = np.arange(128)[:, None]
    ql = np.arange(128)[None, :]
    blk = np.zeros((128, 3, 4, 128), np.float32)
    for d in (-1, 0, 1):
        bidx = _t5_bucket_static(d * 128 + kl - ql)
        for h in range(4):
            blk[:, d + 1, h, :] = tab[bidx, h]
    sh['biasblk'] = blk
    sh['cfar'] = _rep(np.concatenate([tab[15, :], tab[31, :]]))
    cw = f(inp['conv_w'][0])[:, 0, :]
    sh['conv_wT'] = np.ascontiguousarray(cw.reshape(5, 6, 128).transpose(2, 1, 0))
    sh['conv_bT'] = _col(inp['conv_b'][0], 6)
    dtb = np.concatenate([f(inp['dt_bias_f'][0]), f(inp['dt_bias_b'][0])])
    sh['dtb256'] = _rep(np.tile(dtb, 16))
    alog = np.concatenate([f(inp['A_log_f'][0]), f(inp['A_log_b'][0])])
    sh['alog256'] = _rep(np.tile(alog, 16))
    sh['dskip_bc'] = _rep(inp['D_skip'][0])
    sh['ssmg_bc'] = _rep(inp['ssm_norm_g'][0])
    sh['w_out'] = f(inp['w_out'][0])
    sh['router_wT'] = np.ascontiguousarray(f(inp['router_w'][0]).reshape(8, 128, 16).transpose(1, 0, 2))
    sh['w_gate'] = f(inp['w_gate'][0])
    sh['w_up'] = f(inp['w_up'][0])
    sh['w_down'] = f(inp['w_down'][0])
    x = f(inp['x'])
    c = f(inp['c'])
    maps = []
    for b in range(x.shape[0]):
        m = dict(sh)
        m['x'] = x[b]
        m['c_col'] = _col(c[b], 8)
        maps.append(m)
    return maps


_NC_CACHE = {}


def kernel(**inputs):
    maps = prep_inputs(inputs)
    if 'nc' not in _NC_CACHE:
        _NC_CACHE['nc'] = build()
    nc = _NC_CACHE['nc']
    res = run_bass_kernel_spmd(nc, maps, core_ids=list(range(8)))
    return np.stack([np.asarray(r['out'], np.float32) for r in res.results], axis=0)
```
